# Optimizing a Trainium2 kernel written in Bass

```python
import jax
import jax.numpy as jnp
from jax import lax
import numpy as np

D_MODEL = 1024
BATCH = 4
SEQ = 4096
DEPTH = 4

CTX_LEN = 256
GRID_W = 64
HEAD_DIM = 64
CONV_WIDTH = 256
CONV_K = 3
RET_HEADS = 4
RET_WIDTH = RET_HEADS * HEAD_DIM
RET_CHUNK = 128
ATTN_HEADS = 8
ATTN_KV_HEADS = 2
ATTN_GROUP = ATTN_HEADS // ATTN_KV_HEADS
ATTN_WIDTH = ATTN_HEADS * HEAD_DIM
KV_WIDTH = ATTN_KV_HEADS * HEAD_DIM
WINDOW = 128
BLOCK = 128
MIX_WIDTH = CONV_WIDTH + RET_WIDTH + ATTN_WIDTH
SPLIT_SIZES = (CONV_WIDTH,) * 3 + (RET_WIDTH,) * 5 + (ATTN_WIDTH, KV_WIDTH, KV_WIDTH)
SPLIT_IDX = tuple(sum(SPLIT_SIZES[:i + 1]) for i in range(len(SPLIT_SIZES) - 1))
IN_WIDTH = sum(SPLIT_SIZES)
N_EXPERTS = 16
CAPACITY_FACTOR = 2
D_EXPERT = 1024
ROPE_BASE = 10000.0
EPS = 1e-6
NEG_INF = -1e30

kernel_name = 'hybrid_parallel_group_dit'


def rms_norm(x, g):
    xf = x.astype(jnp.float32)
    y = xf * lax.rsqrt(jnp.mean(xf * xf, axis=-1, keepdims=True) + EPS)
    return (y * g.astype(jnp.float32)).astype(x.dtype)


def head_norm(o):
    of = o.astype(jnp.float32)
    mu = jnp.mean(of, axis=-1, keepdims=True)
    var = jnp.mean(jnp.square(of - mu), axis=-1, keepdims=True)
    return ((of - mu) * lax.rsqrt(var + EPS)).astype(o.dtype)


def modulate(h, shift, scale):
    return h * (1.0 + scale) + shift


def axial_rope_tables(n_tokens):
    rows = n_tokens // GRID_W
    row = jnp.repeat(jnp.arange(rows, dtype=jnp.float32), GRID_W)
    col = jnp.tile(jnp.arange(GRID_W, dtype=jnp.float32), rows)
    axis_dim = HEAD_DIM // 2
    inv_freq = ROPE_BASE ** (-jnp.arange(0, axis_dim, 2, dtype=jnp.float32) / axis_dim)
    ang = jnp.stack([row[:, None] * inv_freq, col[:, None] * inv_freq], axis=1)
    return jnp.cos(ang), jnp.sin(ang)


def apply_axial_rope(x, cos, sin):
    shp = x.shape
    xr = x.astype(jnp.float32).reshape(shp[:-1] + (2, 2, HEAD_DIM // 4))
    x1, x2 = xr[..., 0, :], xr[..., 1, :]
    out = jnp.stack([x1 * cos - x2 * sin, x2 * cos + x1 * sin], axis=-2)
    return out.reshape(shp).astype(x.dtype)


def to_heads(t, n_heads):
    b, n, _ = t.shape
    return t.reshape(b, n, n_heads, HEAD_DIM).transpose(0, 2, 1, 3)


def from_heads(t):
    b, h, n, d = t.shape
    return t.transpose(0, 2, 1, 3).reshape(b, n, h * d)


def gated_short_conv(gate_b, gate_c, u, conv_w):
    z = jnp.pad(gate_c * u, ((0, 0), (1, 1), (0, 0)))
    conv = z[:, :-2] * conv_w[0] + z[:, 1:-1] * conv_w[1] + z[:, 2:] * conv_w[2]
    return gate_b * conv


def retention_states(k, v, log_g, s0):
    b, h, n_tok, d = k.shape
    n_chunk = n_tok // RET_CHUNK
    kc = k.reshape(b, h, n_chunk, RET_CHUNK, d)
    vc = v.reshape(b, h, n_chunk, RET_CHUNK, d)
    j = jnp.arange(RET_CHUNK, dtype=jnp.float32)
    w_in = jnp.exp(log_g[:, None] * (RET_CHUNK - 1 - j))
    u = jnp.einsum('bhnld,bhnle,hl->bhnde', kc, vc, w_in).astype(jnp.float32)
    chunk_decay = jnp.exp(log_g * RET_CHUNK)[:, None, None]

    def step(s, u_n):
        return chunk_decay * s + u_n, s

    s_fin, s_prev = lax.scan(step, s0, jnp.moveaxis(u, 2, 0))
    return jnp.moveaxis(s_prev, 0, 2), s_fin


def retention_readout(q, k, v, log_g, s_prev):
    b, h, n_tok, d = q.shape
    n_chunk = n_tok // RET_CHUNK
    qc = q.reshape(b, h, n_chunk, RET_CHUNK, d)
    kc = k.reshape(b, h, n_chunk, RET_CHUNK, d)
    vc = v.reshape(b, h, n_chunk, RET_CHUNK, d)
    i = jnp.arange(RET_CHUNK, dtype=jnp.float32)
    diff = i[:, None] - i[None, :]
    dmask = jnp.where(diff >= 0, jnp.exp(log_g[:, None, None] * jnp.maximum(diff, 0.0)), 0.0)
    scores = jnp.einsum('bhnid,bhnjd->bhnij', qc, kc) * dmask[:, None]
    o = jnp.einsum('bhnij,bhnje->bhnie', scores, vc)
    q_dec = jnp.exp(log_g[:, None] * (i + 1.0))
    o = o + jnp.einsum('bhnid,bhnde,hi->bhnie', qc, s_prev, q_dec)
    return o.reshape(b, h, n_tok, d).astype(q.dtype)


def window_context_attention(q, k, v, k_ctx, v_ctx, sink):
    b, h, g, n_tok, d = q.shape
    nb = n_tok // BLOCK
    scale = d ** -0.5
    qb = q.reshape(b, h, g, nb, BLOCK, d)
    pad = ((0, 0), (0, 0), (BLOCK, BLOCK), (0, 0))
    kp = jnp.pad(k, pad).reshape(b, h, nb + 2, BLOCK, d)
    vp = jnp.pad(v, pad).reshape(b, h, nb + 2, BLOCK, d)
    kw = jnp.concatenate([kp[:, :, :-2], kp[:, :, 1:-1], kp[:, :, 2:]], axis=3)
    vw = jnp.concatenate([vp[:, :, :-2], vp[:, :, 1:-1], vp[:, :, 2:]], axis=3)
    s_win = jnp.einsum('bhgnqd,bhnkd->bhgnqk', qb, kw).astype(jnp.float32) * scale
    q_pos = jnp.arange(n_tok).reshape(nb, BLOCK)
    k_pos = (jnp.arange(nb) * BLOCK - BLOCK)[:, None] + jnp.arange(3 * BLOCK)[None, :]
    rel = q_pos[:, :, None] - k_pos[:, None, :]
    valid = (jnp.abs(rel) <= WINDOW) & (k_pos[:, None, :] >= 0) & (k_pos[:, None, :] < n_tok)
    s_win = jnp.where(valid, s_win, NEG_INF)
    s_ctx = jnp.einsum('bhgnqd,bhkd->bhgnqk', qb, k_ctx).astype(jnp.float32) * scale
    s_sink = jnp.broadcast_to(sink.astype(jnp.float32)[None, :, :, None, None, None], s_ctx.shape[:-1] + (1,))
    p = jax.nn.softmax(jnp.concatenate([s_win, s_ctx, s_sink], axis=-1), axis=-1).astype(v.dtype)
    n_win = 3 * BLOCK
    n_ctx = k_ctx.shape[2]
    o = (jnp.einsum('bhgnqk,bhnkd->bhgnqd', p[..., :n_win], vw)
         + jnp.einsum('bhgnqk,bhkd->bhgnqd', p[..., n_win:n_win + n_ctx], v_ctx))
    return o.reshape(b, h, g, n_tok, d)


def context_attention(q, k, v, sink):
    scale = q.shape[-1] ** -0.5
    s = jnp.einsum('bhgqd,bhkd->bhgqk', q, k).astype(jnp.float32) * scale
    s_sink = jnp.broadcast_to(sink.astype(jnp.float32)[None, :, :, None, None], s.shape[:-1] + (1,))
    p = jax.nn.softmax(jnp.concatenate([s, s_sink], axis=-1), axis=-1)[..., :-1].astype(v.dtype)
    return jnp.einsum('bhgqk,bhkd->bhgqd', p, v)


def expert_choice_ffn(h, w_router, w_gate, w_up, w_down):
    n_tok, d_model = h.shape[1], h.shape[2]
    cap = CAPACITY_FACTOR * n_tok // N_EXPERTS
    aff = jax.nn.softmax((h @ w_router).astype(jnp.float32), axis=-1)
    gate, idx = lax.top_k(jnp.swapaxes(aff, 1, 2), cap)
    xs = jax.vmap(lambda hb, ib: hb[ib])(h, idx)
    a = jnp.einsum('becd,edf->becf', xs, w_gate)
    u = jnp.einsum('becd,edf->becf', xs, w_up)
    y = jnp.einsum('becf,efd->becd', jax.nn.silu(a) * u, w_down) * gate[..., None].astype(h.dtype)

    def scatter(ib, yb):
        return jnp.zeros((n_tok, d_model), yb.dtype).at[ib.reshape(-1)].add(yb.reshape(-1, d_model))

    return jax.vmap(scatter)(idx, y)


def token_mixer(h_lat, h_ctx, w_in, conv_w, ret_logit, sink, w_out, cos, sin, need_ctx):
    b = h_lat.shape[0]
    (cb_l, cc_l, cx_l, rq_l, rk_l, rv_l, rgf_l, rgb_l, aq_l, ak_l, av_l) = jnp.split(h_lat @ w_in, SPLIT_IDX, axis=-1)
    (cb_c, cc_c, cx_c, rq_c, rk_c, rv_c, rgf_c, rgb_c, aq_c, ak_c, av_c) = jnp.split(h_ctx @ w_in, SPLIT_IDX, axis=-1)
    k_scale = HEAD_DIM ** -0.5

    conv_l = gated_short_conv(cb_l, cc_l, cx_l, conv_w)

    q_l = apply_axial_rope(to_heads(rq_l, RET_HEADS), cos, sin)
    k_l = apply_axial_rope(to_heads(rk_l, RET_HEADS) * k_scale, cos, sin)
    v_l = to_heads(rv_l, RET_HEADS)
    k_c = to_heads(rk_c, RET_HEADS) * k_scale
    v_c = to_heads(rv_c, RET_HEADS)
    q_c = to_heads(rq_c, RET_HEADS) if need_ctx else None
    gates_l = (rgf_l, rgb_l)
    gates_c = (rgf_c, rgb_c)
    s_zero = jnp.zeros((b, RET_HEADS, HEAD_DIM, HEAD_DIM), jnp.float32)
    ret_l = 0.0
    ret_c = 0.0
    for d in range(2):
        log_g = jax.nn.log_sigmoid(ret_logit[d].astype(jnp.float32))
        fl = (lambda t: jnp.flip(t, axis=2)) if d == 1 else (lambda t: t)
        sp_c, s_c = retention_states(fl(k_c), fl(v_c), log_g, s_zero)
        sp_l, _ = retention_states(fl(k_l), fl(v_l), log_g, s_c)
        o_l = fl(retention_readout(fl(q_l), fl(k_l), fl(v_l), log_g, sp_l))
        ret_l = ret_l + from_heads(head_norm(o_l)) * jax.nn.silu(gates_l[d])
        if need_ctx:
            o_c = fl(retention_readout(fl(q_c), fl(k_c), fl(v_c), log_g, sp_c))
            ret_c = ret_c + from_heads(head_norm(o_c)) * jax.nn.silu(gates_c[d])

    n_tok = h_lat.shape[1]
    qa_l = apply_axial_rope(to_heads(aq_l, ATTN_HEADS), cos, sin).reshape(b, ATTN_KV_HEADS, ATTN_GROUP, n_tok, HEAD_DIM)
    ka_l = apply_axial_rope(to_heads(ak_l, ATTN_KV_HEADS), cos, sin)
    va_l = to_heads(av_l, ATTN_KV_HEADS)
    ka_c = to_heads(ak_c, ATTN_KV_HEADS)
    va_c = to_heads(av_c, ATTN_KV_HEADS)
    sink_hg = sink.reshape(ATTN_KV_HEADS, ATTN_GROUP)
    attn_l = window_context_attention(qa_l, ka_l, va_l, ka_c, va_c, sink_hg)
    attn_l = from_heads(attn_l.reshape(b, ATTN_HEADS, n_tok, HEAD_DIM))
    y_l = jnp.concatenate([conv_l, ret_l, attn_l], axis=-1) @ w_out
    if not need_ctx:
        return y_l, None

    n_ctx = h_ctx.shape[1]
    conv_c = gated_short_conv(cb_c, cc_c, cx_c, conv_w)
    qa_c = to_heads(aq_c, ATTN_HEADS).reshape(b, ATTN_KV_HEADS, ATTN_GROUP, n_ctx, HEAD_DIM)
    attn_c = context_attention(qa_c, ka_c, va_c, sink_hg)
    attn_c = from_heads(attn_c.reshape(b, ATTN_HEADS, n_ctx, HEAD_DIM))
    y_c = jnp.concatenate([conv_c, ret_c, attn_c], axis=-1) @ w_out
    return y_l, y_c


def setup_inputs(seed: int = 0) -> dict:
    key = jax.random.key(seed)
    ks = jax.random.split(key, 18)
    f32 = jnp.float32

    def nrm(k, shape, s):
        return jax.random.normal(k, shape, f32) * s

    ret_base = jnp.log(jnp.exp2(5.0 + jnp.arange(RET_HEADS, dtype=f32)) - 1.0)
    return {
        'x': nrm(ks[0], (BATCH, SEQ, D_MODEL), 1.0),
        'c': nrm(ks[1], (BATCH, D_MODEL), 1.0),
        'ctx': nrm(ks[2], (BATCH, CTX_LEN, D_MODEL), 1.0),
        'c_ctx': nrm(ks[3], (D_MODEL,), 1.0),
        'w_mod': nrm(ks[4], (DEPTH, D_MODEL, 6 * D_MODEL), 0.5 * D_MODEL ** -0.5),
        'b_mod': nrm(ks[5], (DEPTH, 6 * D_MODEL), 0.02),
        'norm1_g': 1.0 + nrm(ks[6], (DEPTH, D_MODEL), 0.02),
        'norm2_g': 1.0 + nrm(ks[7], (DEPTH, D_MODEL), 0.02),
        'w_in': nrm(ks[8], (DEPTH, D_MODEL, IN_WIDTH), D_MODEL ** -0.5),
        'conv_w': nrm(ks[9], (DEPTH, CONV_K, CONV_WIDTH), CONV_K ** -0.5),
        'ret_decay_logit': ret_base + nrm(ks[10], (DEPTH, 2, RET_HEADS), 0.1),
        'attn_sink': nrm(ks[11], (DEPTH, ATTN_HEADS), 0.5),
        'w_out': nrm(ks[12], (DEPTH, MIX_WIDTH, D_MODEL), MIX_WIDTH ** -0.5),
        'w_router': nrm(ks[13], (DEPTH, D_MODEL, N_EXPERTS), D_MODEL ** -0.5),
        'w_gate': nrm(ks[14], (DEPTH, N_EXPERTS, D_MODEL, D_EXPERT), D_MODEL ** -0.5),
        'w_up': nrm(ks[15], (DEPTH, N_EXPERTS, D_MODEL, D_EXPERT), D_MODEL ** -0.5),
        'w_down': nrm(ks[16], (DEPTH, N_EXPERTS, D_EXPERT, D_MODEL), D_EXPERT ** -0.5),
        'final_g': 1.0 + nrm(ks[17], (D_MODEL,), 0.02),
    }


def reference(x, c, ctx, c_ctx, w_mod, b_mod, norm1_g, norm2_g, w_in, conv_w, ret_decay_logit,
              attn_sink, w_out, w_router, w_gate, w_up, w_down, final_g):
    cos, sin = axial_rope_tables(x.shape[1])
    silu_c = jax.nn.silu(c)
    silu_cc = jax.nn.silu(c_ctx)
    for l in range(DEPTH):
        need_ctx = l < DEPTH - 1
        mod_l = (silu_c @ w_mod[l] + b_mod[l])[:, None, :]
        mod_c = (silu_cc @ w_mod[l] + b_mod[l])[None, None, :]
        sh1, sc1, g1, sh2, sc2, g2 = jnp.split(mod_l, 6, axis=-1)
        csh1, csc1, cg1, csh2, csc2, cg2 = jnp.split(mod_c, 6, axis=-1)

        h_l = modulate(rms_norm(x, norm1_g[l]), sh1, sc1)
        h_c = modulate(rms_norm(ctx, norm1_g[l]), csh1, csc1)
        y_l, y_c = token_mixer(h_l, h_c, w_in[l], conv_w[l], ret_decay_logit[l], attn_sink[l],
                               w_out[l], cos, sin, need_ctx)
        x = x + g1 * y_l
        h_l = modulate(rms_norm(x, norm2_g[l]), sh2, sc2)
        x = x + g2 * expert_choice_ffn(h_l, w_router[l], w_gate[l], w_up[l], w_down[l])
        if need_ctx:
            ctx = ctx + cg1 * y_c
            h_c = modulate(rms_norm(ctx, norm2_g[l]), csh2, csc2)
            ctx = ctx + cg2 * expert_choice_ffn(h_c, w_router[l], w_gate[l], w_up[l], w_down[l])
    return rms_norm(x, final_g)
```

```python
import os
import numpy as np
from contextlib import ExitStack
import concourse.bass as bass
import concourse.mybir as mybir
from concourse.alu_op_type import AluOpType as ALU
from concourse.bass_utils import run_bass_kernel_spmd

F32 = mybir.dt.float32
BF16 = mybir.dt.bfloat16
I32 = mybir.dt.int32
AF = mybir.ActivationFunctionType
AX = mybir.AxisListType

SEM_LIMIT = 4000
NQ = 12

D = 1024
T = 4096
C = 256
TT = T + C
DEPTH = 4
NE = 16
CAP = 512
CAPC = 32
NW = 3968
EPS = 1e-6
ZW = T + C + 4
PE2 = "dve"
SKIP = set(os.environ.get("KSKIP", "").split(","))


class Buf:
    def __init__(self, t, nparts=1, name="", excl=False):
        self.t = t
        self.name = name
        self.excl = excl
        self.lw = [None] * nparts
        self.rd = [[] for _ in range(nparts)]
        self.np_ = nparts

    def __getitem__(self, k):
        return self.t[k]


def _acc(x):
    if isinstance(x, Buf):
        return x, range(x.np_)
    b, p = x
    if isinstance(p, int):
        p = [p]
    return b, p


class KB:
    def __init__(self, nc, es, same_engine_sync=True):
        self.nc = nc
        self.es = es
        self.eng = {"pe": nc.tensor, "dve": nc.vector, "act": nc.scalar, "pool": nc.gpsimd, "sp": nc.sync}
        self.esem = {}
        self.ecnt = {}
        self.eretired = []
        self.nsem = 0
        for e in self.eng:
            self._new_esem(e)
        self.dsem = {}
        self.dcnt = {}
        self.dbase = {}
        self.dretired = []
        for q in ("sp", "pool"):
            self.dsem[q] = [self._sem(f"d_{q}{i}") for i in range(NQ)]
            self.dcnt[q] = 0
            self.dbase[q] = 0
        self.waited = {e: {} for e in self.eng}
        self.prog = {e: [] for e in self.eng}
        self._fz = nc.alloc_sbuf_tensor("fencez", [128, 8], F32)
        self.ses = same_engine_sync
        self.n_instr = 0
        self.fzb = Buf(self._fz, 1, "fencez")
        self.op("dve", lambda e: e.memset(self._fz[:, :], 0.0), (), [self.fzb])

    def _sem(self, name):
        self.nsem += 1
        return self.es.enter_context(self.nc.semaphore(f"{name}_{self.nsem}"))

    def _new_esem(self, e):
        if e in self.esem and self.ecnt[e] > 0:
            self.eretired.append((self.esem[e], self.ecnt[e], e))
        self.esem[e] = self._sem("e_" + e)
        self.ecnt[e] = 0

    def _deps(self, reads, writes):
        deps = []
        for x in reads:
            b, ps = _acc(x)
            for p in ps:
                if b.lw[p] is not None:
                    deps.append(b.lw[p])
                if b.excl:
                    deps.extend(b.rd[p])
        for x in writes:
            b, ps = _acc(x)
            for p in ps:
                if b.lw[p] is not None:
                    deps.append(b.lw[p])
                deps.extend(b.rd[p])
        return deps

    def _wait(self, e, deps):
        w = self.waited[e]
        best = {}
        for (sem, val, src) in deps:
            if src == e and (e == "pe" or not self.ses):
                continue
            k = id(sem)
            if w.get(k, 0) >= val:
                continue
            if k not in best or best[k][1] < val:
                best[k] = (sem, val)
        for k, (sem, val) in best.items():
            self.prog[e].append(("wait", sem, val))
            w[k] = val

    def _record(self, ev, reads, writes):
        for x in reads:
            b, ps = _acc(x)
            for p in ps:
                b.rd[p].append(ev)
        for x in writes:
            b, ps = _acc(x)
            for p in ps:
                b.lw[p] = ev
                b.rd[p] = []

    def op(self, e, fn, reads=(), writes=(), fence=False):
        if self.ecnt[e] >= SEM_LIMIT:
            self._new_esem(e)
        if fence:
            reads = list(reads) + [self.fzb]
        self._wait(e, self._deps(reads, writes))
        self.prog[e].append(("op", fn, fence, self.esem[e], 1))
        self.ecnt[e] += 1
        ev = (self.esem[e], self.ecnt[e], e)
        self._record(ev, reads, writes)
        self.n_instr += 1
        return ev

    def _fence(self, e):
        if e == "dve":
            return self.nc.vector.tensor_copy(self._fz[0:1, 0:1], self._fz[0:1, 1:2])
        if e == "act":
            return self.nc.scalar.copy(self._fz[0:1, 2:3], self._fz[0:1, 3:4])
        raise ValueError(e)

    def dma(self, q, fn, reads=(), writes=()):
        i = self.dcnt[q] - self.dbase[q]
        if 16 * (i // NQ) + 16 > SEM_LIMIT:
            for j, sm in enumerate(self.dsem[q]):
                cnt = (i - j + NQ - 1) // NQ if i > j else 0
                if cnt > 0:
                    self.dretired.append((sm, 16 * cnt, "dma"))
            self.dsem[q] = [self._sem(f"d_{q}{k}") for k in range(NQ)]
            self.dbase[q] = self.dcnt[q]
            i = 0
        sem = self.dsem[q][i % NQ]
        prev = 16 * (i // NQ)
        deps = self._deps(reads, writes)
        if prev > 0:
            deps.append((sem, prev, "dma"))
        self._wait(q, deps)
        self.prog[q].append(("op", fn, False, sem, 16))
        self.dcnt[q] += 1
        ev = (sem, prev + 16, "dma")
        self._record(ev, reads, writes)
        self.n_instr += 1
        return ev

    def all_events(self):
        evs = []
        for e in self.eng:
            if self.ecnt[e] > 0:
                evs.append((self.esem[e], self.ecnt[e], e))
        evs.extend(self.dretired)
        for q in self.dsem:
            n = self.dcnt[q] - self.dbase[q]
            for j, sem in enumerate(self.dsem[q]):
                cnt = (n - j + NQ - 1) // NQ if n > j else 0
                if cnt > 0:
                    evs.append((sem, 16 * cnt, "dma"))
        return evs

    def barrier(self):
        evs = self.all_events()
        for e in self.eng:
            self._wait(e, evs)

    def finish(self):
        self._wait("sp", self.all_events())

    def sb(self, name, shape, dt=F32, nparts=1):
        return Buf(self.nc.alloc_sbuf_tensor(name, list(shape), dt), nparts, name)

    def ps(self, name, shape, dt=F32, nparts=1):
        return Buf(self.nc.alloc_psum_tensor(name, list(shape), dt), nparts, name, excl=True)

    def dram(self, name, shape, dt=F32, kind="Internal", nparts=1):
        return Buf(self.nc.dram_tensor(name, list(shape), dt, kind=kind), nparts, name)

    def emit(self):
        with self.nc.Block() as block:
            for e, dec in (("pe", block.tensor), ("dve", block.vector), ("act", block.scalar),
                           ("pool", block.gpsimd), ("sp", block.sync)):
                prog = self.prog[e]
                if not prog:
                    continue

                def body(eng, prog=prog, e=e):
                    for it in prog:
                        if it[0] == "wait":
                            eng.wait_ge(it[1], it[2])
                        else:
                            _, fn, fence, sem, inc = it
                            ins = fn(eng)
                            if fence:
                                ins = self._fence(e)
                            ins.then_inc(sem, inc)

                dec(body)


class G:
    def __init__(self, kb, arena_words):
        self.kb = kb
        self.nc = kb.nc
        self.arena = kb.nc.alloc_sbuf_tensor("arena", [128, arena_words], F32)
        self.aw = arena_words
        self.ap_ = 0
        self.uid = 0

    def reset(self):
        self.kb.barrier()
        self.ap_ = 0

    def carve(self, shape, dt=F32, parts=128, nparts=1, name="a"):
        n = 1
        for s in shape[1:]:
            n *= s
        words = n if dt in (F32, I32) else (n + 1) // 2
        words = (words + 1) // 2 * 2
        off = self.ap_
        self.ap_ += words
        assert self.ap_ <= self.aw, f"arena overflow {self.ap_} > {self.aw} ({name})"
        v = self.arena[0:shape[0], off:off + words]
        if dt != F32:
            v = v.bitcast(dt)
        if dt == BF16:
            v = v[:, 0:n]
        else:
            v = v[:, 0:n]
        if len(shape) == 3:
            v = v.rearrange("p (a b) -> p a b", a=shape[1])
        elif len(shape) == 4:
            v = v.rearrange("p (a b c) -> p a b c", a=shape[1], b=shape[2])
        self.uid += 1
        return Buf(v, nparts, f"{name}{self.uid}")

    def mm(self, out, lhsT, rhs, st, sp, R, W):
        self.kb.op("pe", lambda e: e.matmul(out, lhsT, rhs, start=st, stop=sp), R, W)

    def tr(self, out, in_, ident, R, W):
        self.kb.op("pe", lambda e: e.transpose(out, in_, ident), R, W)

    def act(self, out, in_, func, R, W, bias=None, scale=None, accum=None):
        kw = {}
        if bias is not None:
            kw["bias"] = bias
        if scale is not None:
            kw["scale"] = scale
        if accum is not None:
            kw["accum_out"] = accum
        self.kb.op("act", lambda e: e.activation(out, in_, func, **kw), R, W, fence=accum is not None)

    def tt(self, eng, out, a, b, op, R, W):
        self.kb.op(eng, lambda e: e.tensor_tensor(out, a, b, op), R, W)

    def ts(self, eng, out, a, s1, s2, op0, op1, R, W, accum=None):
        if accum is not None:
            self.kb.op(eng, lambda e: e.tensor_scalar(out, a, s1, None, op0, op1, accum_out=accum), R, W, fence=True)
        elif op1 is None:
            self.kb.op(eng, lambda e: e.tensor_scalar(out, a, s1, None, op0), R, W)
        else:
            self.kb.op(eng, lambda e: e.tensor_scalar(out, a, s1, s2, op0, op1), R, W)

    def stt(self, out, in0, scalar, in1, op0, op1, R, W):
        self.kb.op("dve", lambda e: e.scalar_tensor_tensor(out, in0, scalar, in1, op0, op1), R, W)

    def cp(self, eng, out, in_, R, W):
        if eng == "act":
            self.kb.op("act", lambda e: e.copy(out, in_), R, W)
        else:
            self.kb.op(eng, lambda e: e.tensor_copy(out, in_), R, W)

    def memset(self, eng, out, val, W):
        self.kb.op(eng, lambda e: e.memset(out, val), (), W)

    def recip(self, out, in_, R, W):
        self.kb.op("dve", lambda e: e.reciprocal(out, in_), R, W)

    def red(self, out, in_, R, W):
        self.kb.op("dve", lambda e: e.tensor_reduce(out, in_, AX.X, ALU.add), R, W)

    def ld(self, out, in_, R, W, q="sp", slow=False):
        if slow:
            self.kb.dma(q, lambda e: e.dma_start(out=out, in_=in_, allow_slow_non_contiguous=True), R, W)
        else:
            self.kb.dma(q, lambda e: e.dma_start(out=out, in_=in_), R, W)

    def gather(self, out, src, idx, R, W, elem_off=0):
        self.kb.dma("pool", lambda e: e.indirect_dma_start(
            out=out, out_offset=None, in_=src,
            in_offset=bass.IndirectOffsetOnAxis(ap=idx, axis=0), element_offset=elem_off), R, W)

    def scatter_add(self, dst, src, idx, R, W):
        self.kb.dma("pool", lambda e: e.indirect_dma_start(
            out=dst, out_offset=bass.IndirectOffsetOnAxis(ap=idx, axis=0),
            in_=src, in_offset=None, compute_op=ALU.add), R, W)


def _win_perm():
    names = ['cb', 'cc', 'cx', 'rq', 'rk', 'rv', 'rgf', 'rgb', 'aq', 'ak', 'av']
    sizes = [256] * 3 + [256] * 5 + [512, 128, 128]
    off = {}
    o = 0
    for n, s in zip(names, sizes):
        off[n] = o
        o += s

    def sw(nh):
        idx = []
        for h in range(nh):
            for i in range(64):
                b4, j = i // 16, i % 16
                idx.append(h * 64 + (b4 ^ 1) * 16 + j)
        return np.array(idx)

    cols = []
    for n in ('cb', 'cc', 'cx'):
        cols += list(off[n] + np.arange(256))
    cols += list(off['rq'] + np.arange(256)) + list(off['rq'] + sw(4))
    cols += list(off['rk'] + np.arange(256)) + list(off['rk'] + sw(4))
    cols += list(off['aq'] + np.arange(512)) + list(off['aq'] + sw(8))
    cols += list(off['ak'] + np.arange(128)) + list(off['ak'] + sw(2))
    cols += list(off['rv'] + np.arange(256)) + list(off['rgf'] + np.arange(256))
    cols += list(off['rgb'] + np.arange(256)) + list(off['av'] + np.arange(128))
    cols = np.array(cols)
    assert cols.shape[0] == NW
    return cols


def _rope_tables():
    t = np.arange(T)
    row = (t // 64).astype(np.float32)
    col = (t % 64).astype(np.float32)
    inv = (np.float32(10000.0) ** (-np.arange(0, 32, 2, dtype=np.float32) / np.float32(32))).astype(np.float32)
    ar = (row[:, None] * inv[None, :]).astype(np.float32)
    ac = (col[:, None] * inv[None, :]).astype(np.float32)
    cr, sr, cc_, sc_ = np.cos(ar).T, np.sin(ar).T, np.cos(ac).T, np.sin(ac).T
    cos64 = np.concatenate([cr, cr, cc_, cc_], 0)
    sin64 = np.concatenate([-sr, sr, -sc_, sc_], 0)
    cos = np.ones((128, TT), np.float32)
    sin = np.zeros((128, TT), np.float32)
    cos[:, :T] = np.concatenate([cos64, cos64], 0)
    sin[:, :T] = np.concatenate([sin64, sin64], 0)
    return cos, sin


def build(n_layers=DEPTH, debug=False, stop_after=None, wl=DEPTH, ne_w=NE):
    nc = bass.Bass("TRN2", target_bir_lowering=False)
    es = ExitStack()
    kb = KB(nc, es)
    skind = "ExternalOutput" if debug else "Internal"

    def din(name, shape, dt=F32):
        return kb.dram(name, shape, dt, kind="ExternalInput")

    x_in = din("x", [T, D]); ctx_in = din("ctx", [C, D]); cvec = din("cvec", [2, D])
    w_mod = din("w_mod", [wl, D, 6 * D]); bm_t = din("bm_t", [128, DEPTH, 48]); b_mod = din("b_mod", [DEPTH, 6 * D])
    g1t_d = din("g1t", [128, DEPTH, 8]); g2t_d = din("g2t", [128, DEPTH, 8])
    w_in = din("w_in", [wl, D, NW]); cw_d = din("cw_t", [128, DEPTH * 6])
    rdl_d = din("rdl", [1, 32]); sink_d = din("sink", [1, 32])
    w_out = din("w_out", [wl, D, D]); w_router = din("w_router", [wl, D, NE])
    w_gate = din("w_gate", [wl, ne_w, D, D]); w_up = din("w_up", [wl, ne_w, D, D]); w_down = din("w_down", [wl, ne_w, D, D])
    fg_d = din("final_g", [1, D]); cos_d = din("rope_cos", [128, TT]); sin_d = din("rope_sin", [128, TT])
    out_d = kb.dram("out", [T, D], F32, kind="ExternalOutput")

    xres = kb.dram("xres", [TT, D], F32, kind=skind)
    Fd = kb.dram("Fd", [18 * 64, TT], BF16, kind=skind)
    TMd = kb.dram("TMd", [TT, 1152], BF16, kind=skind)
    Zd = kb.dram("Zd", [2, 128, ZW], F32, kind=skind)
    CBd = kb.dram("CBd", [2, 128, TT], BF16, kind=skind)
    SPd = kb.dram("SPd", [2, 34, 64, 256], BF16, kind=skind)
    XN2d = kb.dram("XN2d", [TT, D], BF16, kind=skind)
    EXd = kb.dram("EXd", [NE, TT], F32, kind=skind)
    POSd = kb.dram("POSd", [32, T], F32, kind=skind)
    MROWd = kb.dram("MROWd", [2, 2, D], F32, kind=skind)
    DBGd = kb.dram("DBGd", [128, 256], F32, kind=skind)

    identb = kb.sb("identb", [128, 128], BF16); identf = kb.sb("identf", [128, 128])
    Dm = kb.sb("Dm", [128, 128]); Dpos = kb.sb("Dpos", [128, 128]); Dneg = kb.sb("Dneg", [128, 128])
    Mge8 = kb.sb("Mge8", [128, 128]); Mle8 = kb.sb("Mle8", [128, 128])
    Mprev = kb.sb("Mprev", [128, 4, 128], BF16); Mnext = kb.sb("Mnext", [128, 4, 128], BF16)
    ones_bf = kb.sb("ones_bf", [128, 64], BF16); ones16 = kb.sb("ones16", [16, 16])
    pcol = kb.sb("pcol", [128, 1]); p127 = kb.sb("p127", [128, 1]); jval = kb.sb("jval", [128, 4]); jb = kb.sb("jb", [128, 4])
    ip1 = kb.sb("ip1", [64, 128]); rev = kb.sb("rev", [64, 128])
    scv = kb.sb("scv", [128, 8, 2]); bm_f = kb.sb("bm_f", [128, DEPTH, 48])
    g1t = kb.sb("g1t_s", [128, DEPTH, 8]); g2t = kb.sb("g2t_s", [128, DEPTH, 8]); cw = kb.sb("cw", [128, DEPTH * 6])
    rdl_b = kb.sb("rdl_b", [128, 32]); sink_b = kb.sb("sink_b", [128, 32])
    AB = kb.sb("AB", [128, 4, 8, 2])
    lg = kb.sb("lg", [128, 8]); wkc = kb.sb("wkc", [128, 8]); dcol = kb.sb("dcol", [128, 8]); se = kb.sb("se", [128, 8])
    DMf = kb.sb("DMf", [128, 4, 128]); DMb = kb.sb("DMb", [128, 4, 128])
    QD = kb.sb("QD", [64, 2, 4, 128], BF16); WK = kb.sb("WK", [128, 2, 256]); DEC = kb.sb("DEC", [64, 2, 256])
    SINKE = kb.sb("SINKE", [64, 2, 512]); Wr = kb.sb("Wr", [128, 8, NE])
    KC = kb.sb("KC", [64, 2, 256], BF16); VC = kb.sb("VC", [128, 2, 128], BF16)
    Sf = kb.sb("Sf", [64, 256]); Sb = kb.sb("Sb", [64, 256])
    LISTF = kb.sb("LISTF", [128, 64]); LISTI = kb.sb("LISTI", [128, 64], I32)
    LISTCF = kb.sb("LISTCF", [32, 16]); LISTCI = kb.sb("LISTCI", [32, 16], I32)
    SACC = kb.sb("SACC", [128, 64])
    tmp128 = kb.sb("tmp128", [128, 128])
    WBIG = kb.sb("WBIG", [128, 32768], BF16, nparts=4)
    WM = [kb.sb(f"WM{i}", [128, 8, 256]) for i in range(2)]
    PB = [kb.ps(f"PB{i}", [128, 512]) for i in range(6)]
    PT = [kb.ps(f"PT{i}", [128, 1024], BF16) for i in range(2)]

    g = G(kb, 24576)

    def v3(ap, a):
        return ap.rearrange("p (a b) -> p a b", a=a)

    def setup():
        kb.op("pool", lambda e: e.iota(Dm[:, :], [[1, 128]], base=0, channel_multiplier=-1,
                                       allow_small_or_imprecise_dtypes=True), (), [Dm])
        g.ts("dve", identf[:, :], Dm[:, :], 0.0, None, ALU.is_equal, None, [Dm], [identf])
        g.cp("dve", identb[:, :], identf[:, :], [identf], [identb])
        g.ts("dve", Dpos[:, :], Dm[:, :], 0.0, None, ALU.max, None, [Dm], [Dpos])
        g.ts("dve", Dneg[:, :], Dm[:, :], -1.0, 0.0, ALU.mult, ALU.max, [Dm], [Dneg])
        g.ts("dve", Mge8[:, :], Dm[:, :], 0.0, 0.125, ALU.is_ge, ALU.mult, [Dm], [Mge8])
        g.ts("dve", Mle8[:, :], Dm[:, :], 0.0, 0.125, ALU.is_le, ALU.mult, [Dm], [Mle8])
        for gg in range(4):
            g.ts("dve", Mprev[:, gg, :], Dm[:, :], 0.0, None, ALU.is_le, None, [Dm], [Mprev])
            g.ts("dve", Mnext[:, gg, :], Dm[:, :], 0.0, None, ALU.is_ge, None, [Dm], [Mnext])
        g.memset("dve", ones_bf[:, :], 1.0, [ones_bf])
        g.memset("dve", ones16[:, :], 1.0, [ones16])
        kb.op("pool", lambda e: e.iota(pcol[:, :], [[0, 1]], base=0, channel_multiplier=1,
                                       allow_small_or_imprecise_dtypes=True), (), [pcol])
        g.ts("dve", p127[:, :], pcol[:, :], -1.0, 127.0, ALU.mult, ALU.add, [pcol], [p127])
        kb.op("pool", lambda e: e.iota(jval[:, :], [[128, 4]], base=0, channel_multiplier=1,
                                       allow_small_or_imprecise_dtypes=True), (), [jval])
        g.ts("dve", jb[:, :], jval[:, :], 0.5, None, ALU.add, None, [jval], [jb])
        kb.op("pool", lambda e: e.iota(ip1[:, :], [[1, 128]], base=1, channel_multiplier=0,
                                       allow_small_or_imprecise_dtypes=True), (), [ip1])
        kb.op("pool", lambda e: e.iota(rev[:, :], [[-1, 128]], base=128, channel_multiplier=0,
                                       allow_small_or_imprecise_dtypes=True), (), [rev])
        g.memset("dve", LISTF[:, :], 0.0, [LISTF])
        g.memset("dve", LISTCF[:, :], 0.0, [LISTCF])
        g.ld(xres[0:T, :], x_in[:, :], [x_in], [xres])
        g.ld(xres[T:TT, :], ctx_in[:, :], [ctx_in], [xres])
        g.memset("dve", tmp128[:, :], 0.0, [tmp128])
        for c0 in (0, T + 1, T + 2, T + C + 3):
            g.ld(Zd.t.ap()[:, :, c0:c0 + 1].rearrange("c p o -> p c o"), v3(tmp128[:, 0:2], 2), [tmp128], [Zd], slow=True)
        for s_ in range(2):
            g.ld(scv[:, :, s_:s_ + 1], cvec.t.ap()[s_:s_ + 1, :].rearrange("s (kc p) -> p kc s", p=128), [cvec], [scv], slow=True)
        g.act(scv[:, :, :], scv[:, :, :], AF.Silu, [scv], [scv])
        g.ld(bm_f[:, :, :], bm_t[:, :, :], [bm_t], [bm_f])
        g.ld(g1t[:, :, :], g1t_d[:, :, :], [g1t_d], [g1t])
        g.ld(g2t[:, :, :], g2t_d[:, :, :], [g2t_d], [g2t])
        g.ld(cw[:, :], cw_d[:, :], [cw_d], [cw])
        g.ld(rdl_b[:, :], rdl_d[0:1, :].partition_broadcast(128), [rdl_d], [rdl_b])
        g.ld(sink_b[:, :], sink_d[0:1, :].partition_broadcast(128), [sink_d], [sink_b])

    def phase_mod(l):
        g.reset()
        modf_ps = v3(PB[0][:, 0:64], 32)
        rows_ps = PB[1]
        rows_sb = g.carve([2, 2048], name="rows")
        bmr = g.carve([2, 2048], name="bmr")
        modf = g.carve([128, 32, 2], name="modf")
        tmpm = g.carve([128, 8, 2], name="tmpm")
        g.ld(bmr[0:2, 0:1024], b_mod[l:l + 1, 2 * D:3 * D].partition_broadcast(2), [b_mod], [bmr])
        g.ld(bmr[0:2, 1024:2048], b_mod[l:l + 1, 5 * D:6 * D].partition_broadcast(2), [b_mod], [bmr])
        wsrc = w_mod.t.ap()[l].rearrange("(kc p) n -> p kc n", p=128)
        for j in range(24):
            grp = j // 4
            wm = WM[j % 2]
            g.ld(wm[:, :, :], wsrc[:, :, j * 256:(j + 1) * 256], [w_mod], [wm])
            if grp in (0, 1, 3, 4):
                gi = {0: 0, 1: 1, 3: 2, 4: 3}[grp]
                for nb in range(2):
                    fm = gi * 8 + (j % 4) * 2 + nb
                    for kc in range(8):
                        g.mm(modf_ps[:, fm, :], wm[:, kc, nb * 128:(nb + 1) * 128], scv[:, kc, :], kc == 0, kc == 7,
                             [wm, scv], [PB[0]])
            else:
                ri = 0 if grp == 2 else 1
                col = ri * 1024 + (j % 4) * 256
                for kc in range(8):
                    g.mm(rows_ps[0:2, 0:256], scv[:, kc, :], wm[:, kc, :], kc == 0, kc == 7, [wm, scv], [PB[1]])
                g.tt("dve", rows_sb[0:2, col:col + 256], rows_ps[0:2, 0:256], bmr[0:2, col:col + 256], ALU.add,
                     [PB[1], bmr], [rows_sb])
        for gi, Gi in enumerate((0, 1, 3, 4)):
            g.tt("dve", modf[:, gi * 8:(gi + 1) * 8, :], modf_ps[:, gi * 8:(gi + 1) * 8, :],
                 bm_f[:, l, Gi * 8:(Gi + 1) * 8].unsqueeze(2).to_broadcast([128, 8, 2]), ALU.add, [PB[0], bm_f], [modf])
        g.ts("dve", tmpm[:, :, :], modf[:, 8:16, :], 1.0, None, ALU.add, None, [modf], [tmpm])
        g.tt("dve", AB[:, 0, :, :], tmpm[:, :, :], g1t[:, l, :].unsqueeze(2).to_broadcast([128, 8, 2]), ALU.mult,
             [tmpm, g1t], [AB])
        g.cp("dve", AB[:, 1, :, :], modf[:, 0:8, :], [modf], [AB])
        g.ts("dve", tmpm[:, :, :], modf[:, 24:32, :], 1.0, None, ALU.add, None, [modf], [tmpm])
        g.tt("dve", AB[:, 2, :, :], tmpm[:, :, :], g2t[:, l, :].unsqueeze(2).to_broadcast([128, 8, 2]), ALU.mult,
             [tmpm, g2t], [AB])
        g.cp("dve", AB[:, 3, :, :], modf[:, 16:24, :], [modf], [AB])
        g.ld(MROWd.t.ap()[0], rows_sb[0:2, 0:1024], [rows_sb], [MROWd])
        g.ld(MROWd.t.ap()[1], rows_sb[0:2, 1024:2048], [rows_sb], [MROWd])
        g.act(lg[:, :], rdl_b[:, l * 8:(l + 1) * 8], AF.Exp, [rdl_b], [lg], scale=-1.0)
        g.ts("dve", lg[:, :], lg[:, :], 1.0, None, ALU.add, None, [lg], [lg])
        g.act(lg[:, :], lg[:, :], AF.Ln, [lg], [lg])
        g.ts("dve", lg[:, :], lg[:, :], -1.0, None, ALU.mult, None, [lg], [lg])
        for d in range(2):
            for h in range(4):
                dh = d * 4 + h
                g.act(tmp128[:, :], (Dpos if d == 0 else Dneg)[:, :], AF.Exp, [Dpos, Dneg, lg], [tmp128], scale=lg[:, dh:dh + 1])
                g.tt("dve", (DMf if d == 0 else DMb)[:, h, :], tmp128[:, :], (Mge8 if d == 0 else Mle8)[:, :], ALU.mult,
                     [tmp128, Mge8, Mle8], [DMf if d == 0 else DMb])
                g.act(QD[:, d, h, :], (ip1 if d == 0 else rev)[:, :], AF.Exp, [ip1, rev, lg], [QD], scale=lg[0:64, dh:dh + 1])
                g.act(wkc[:, dh:dh + 1], (p127 if d == 0 else pcol)[:, :], AF.Exp, [p127, pcol, lg], [wkc], scale=lg[:, dh:dh + 1])
        g.ts("dve", wkc[:, :], wkc[:, :], 0.125, None, ALU.mult, None, [wkc], [wkc])
        g.act(dcol[:, :], lg[:, :], AF.Exp, [lg], [dcol], scale=128.0)
        g.act(se[:, :], sink_b[:, l * 8:(l + 1) * 8], AF.Exp, [sink_b], [se])
        for d in range(2):
            g.cp("dve", v3(WK[:, d, :], 4), wkc[:, d * 4:(d + 1) * 4].unsqueeze(2).to_broadcast([128, 4, 64]), [wkc], [WK])
            g.cp("dve", v3(DEC[:, d, :], 4), dcol[0:64, d * 4:(d + 1) * 4].unsqueeze(2).to_broadcast([64, 4, 64]), [dcol], [DEC])
            g.cp("dve", v3(SINKE[:, d, :], 4), se[0:64, d * 4:(d + 1) * 4].unsqueeze(2).to_broadcast([64, 4, 128]), [se], [SINKE])
        g.ld(Wr[:, :, :], w_router.t.ap()[l].rearrange("(kc p) e -> p kc e", p=128), [w_router], [Wr])

    FM_PAIRS = [
        (6, 8, 0, False), (7, 9, 2, False),
        (10, 12, 4, True), (11, 13, 6, True),
        (14, 18, 8, False), (15, 19, 10, False), (16, 20, 12, False), (17, 21, 14, False),
        (22, 23, 16, False),
    ]

    def zcol(tt):
        return tt + 1 if tt < T else tt + 3

    def chunk_of(tt):
        return tt // 128 if tt < T else 32 + (tt - T) // 128

    def tiles(with_ctx=True):
        ts_ = []
        if with_ctx:
            ts_.append((1, T, C))
        for i in range(T // 512):
            ts_.append((0, i * 512, 512))
        return ts_

    def rms_stats(xin, nsub, ss, rstd, junk):
        for a in range(nsub):
            g.act(junk[:, :], xin[:, a, :], AF.Square, [xin], [junk, ss], accum=ss[:, a:a + 1])
        g.ts("dve", rstd[:, 0:nsub], ss[:, 0:nsub], 1.0 / D, EPS, ALU.mult, ALU.add, [ss], [rstd])
        g.act(rstd[:, 0:nsub], rstd[:, 0:nsub], AF.Sqrt, [rstd], [rstd])
        g.recip(rstd[:, 0:nsub], rstd[:, 0:nsub], [rstd], [rstd])

    def phase_proj(l):
        g.reset()
        win = WBIG[:, 0:8 * NW].rearrange("p (k n) -> p k n", k=8)
        for kc in range(8):
            for hh in range(2):
                g.ld(win[:, kc, hh * 1984:(hh + 1) * 1984],
                     w_in.t.ap()[l, kc * 128:(kc + 1) * 128, hh * 1984:(hh + 1) * 1984], [w_in], [WBIG], q="pool")
        xin = g.carve([128, 4, D], name="xin")
        xn = g.carve([128, 4, D], BF16, name="xn")
        hT = g.carve([128, 8, 512], BF16, name="hT")
        cosb = g.carve([128, 512], name="cosb"); sinb = g.carve([128, 512], name="sinb")
        ss = g.carve([128, 4], name="ss"); rstd = g.carve([128, 4], name="rstd")
        junk = g.carve([128, D], name="junk")
        tmst = g.carve([128, 4, 1152], BF16, name="tmst")
        t1 = g.carve([128, 512], name="t1"); t2 = g.carve([128, 512], name="t2")
        fst = [g.carve([128, 512], BF16, name="fst") for _ in range(2)]
        kst = [g.carve([128, 512], BF16, name="kst") for _ in range(2)]
        ccs = g.carve([128, 512], name="ccs")
        zst = g.carve([128, 2, 512], name="zst")
        cbst = g.carve([128, 2, 512], BF16, name="cbst")
        kw = g.carve([128, 256], BF16, name="kw")
        spst = [g.carve([64, 256], BF16, name="spst") for _ in range(2)]
        g.memset("dve", Sf[:, :], 0.0, [Sf])
        fcount = 0
        pbi = 0
        Ffull = Fd.t.ap()
        for (s, t0, n) in tiles(True):
            nsub = n // 128
            g.ld(xin[:, 0:nsub, :], xres[t0:t0 + n, :].rearrange("(a p) d -> p a d", p=128), [xres], [xin])
            g.ld(cosb[:, 0:n], cos_d[:, t0:t0 + n], [cos_d], [cosb])
            g.ld(sinb[:, 0:n], sin_d[:, t0:t0 + n], [sin_d], [sinb])
            rms_stats(xin, nsub, ss, rstd, junk)
            for a in range(nsub):
                g.ts("dve", xn[:, a, :], xin[:, a, :], rstd[:, a:a + 1], None, ALU.mult, None, [xin, rstd], [xn])
            for kc in range(8):
                tp = PT[kc % 2]
                for a in range(nsub):
                    g.tr(tp[:, a * 128:(a + 1) * 128], xn[:, a, kc * 128:(kc + 1) * 128], identb[:, :], [xn, identb], [tp])
                g.act(hT[:, kc, 0:n], tp[:, 0:n], AF.Identity, [tp, AB], [hT],
                      bias=AB[:, 1, kc, s:s + 1], scale=AB[:, 0, kc, s:s + 1])

            def fm_block(fb):
                nonlocal pbi
                pb = PB[pbi % 4]
                pbi += 1
                for kc in range(8):
                    g.mm(pb[:, 0:n], win[:, kc, fb * 128:(fb + 1) * 128], hT[:, kc, 0:n], kc == 0, kc == 7, [WBIG, hT], [pb])
                return pb

            for c in range(2):
                pb = fm_block(c)
                g.cp("act", cbst[:, c, 0:n], pb[:, 0:n], [pb], [cbst])
            for c in range(2):
                pcc = fm_block(2 + c)
                g.cp("act", ccs[:, 0:n], pcc[:, 0:n], [pcc], [ccs])
                pcx = fm_block(4 + c)
                g.tt("dve", zst[:, c, 0:n], pcx[:, 0:n], ccs[:, 0:n], ALU.mult, [pcx, ccs], [zst])
            g.ld(CBd.t.ap()[:, :, t0:t0 + n].rearrange("c p t -> p c t"), cbst[:, :, 0:n], [cbst], [CBd])
            g.ld(Zd.t.ap()[:, :, zcol(t0):zcol(t0) + n].rearrange("c p t -> p c t"), zst[:, :, 0:n], [zst], [Zd])
            for (fb, fbs, hidx, isk) in FM_PAIRS:
                px = fm_block(fb)
                g.tt("dve", t1[:, 0:n], px[:, 0:n], cosb[:, 0:n], ALU.mult, [px, cosb], [t1])
                psw = fm_block(fbs)
                g.tt("dve", t2[:, 0:n], psw[:, 0:n], sinb[:, 0:n], ALU.mult, [psw, sinb], [t2])
                if isk:
                    dst = kst[(hidx - 4) // 2]
                else:
                    dst = fst[fcount % 2]
                    fcount += 1
                g.tt("pool", dst[:, 0:n], t1[:, 0:n], t2[:, 0:n], ALU.add, [t1, t2], [dst])
                g.ld(Ffull[hidx * 64:hidx * 64 + 128, t0:t0 + n], dst[:, 0:n], [dst], [Fd])
            ktp = PT[0]
            for a in range(nsub):
                for hp in range(2):
                    g.tr(ktp[:, a * 256 + hp * 128:a * 256 + (hp + 1) * 128], kst[hp][:, a * 128:(a + 1) * 128], identb[:, :],
                         [kst[hp], identb], [ktp])
            g.cp("act", tmst[:, 0:nsub, 0:256], v3(ktp[:, 0:nsub * 256], nsub), [ktp], [tmst])
            for a in range(nsub):
                pa, pbb = PB[4], PB[5]
                for kc in range(8):
                    g.mm(pa[:, 0:512], hT[:, kc, a * 128:(a + 1) * 128], win[:, kc, 3072:3584], kc == 0, kc == 7, [WBIG, hT], [pa])
                for kc in range(8):
                    g.mm(pbb[:, 0:384], hT[:, kc, a * 128:(a + 1) * 128], win[:, kc, 3584:3968], kc == 0, kc == 7, [WBIG, hT], [pbb])
                g.cp("dve", tmst[:, a, 256:512], pa[:, 0:256], [pa], [tmst])
                g.act(tmst[:, a, 512:768], pa[:, 256:512], AF.Silu, [pa], [tmst])
                g.act(tmst[:, a, 768:1024], pbb[:, 0:256], AF.Silu, [pbb], [tmst])
                g.cp("dve", tmst[:, a, 1024:1152], pbb[:, 256:384], [pbb], [tmst])
            g.ld(TMd[t0:t0 + n, :].rearrange("(a p) c -> p a c", p=128), tmst[:, 0:nsub, :], [tmst], [TMd])
            for a in range(nsub):
                ch = chunk_of(t0 + a * 128)
                g.tt("dve", kw[:, :], tmst[:, a, 0:256], WK[:, 0, :], ALU.mult, [tmst, WK], [kw])
                ups = PB[4]
                for h in range(4):
                    g.mm(ups[0:64, h * 64:(h + 1) * 64], kw[:, h * 64:(h + 1) * 64], tmst[:, a, 256 + h * 64:256 + (h + 1) * 64],
                         True, True, [kw, tmst], [ups])
                sp = spst[ch % 2]
                g.cp("act", sp[:, :], Sf[:, :], [Sf], [sp])
                g.ld(SPd.t.ap()[0, ch], sp[:, :], [sp], [SPd])
                g.tt("pool", Sf[:, :], Sf[:, :], DEC[:, 0, :], ALU.mult, [Sf, DEC], [Sf])
                g.tt("dve", Sf[:, :], Sf[:, :], ups[0:64, 0:256], ALU.add, [Sf, ups], [Sf])

    def phase_bwd(l):
        g.reset()
        kvb = [g.carve([128, 512], BF16, name="kvb") for _ in range(2)]
        kw = g.carve([128, 256], BF16, name="kwb")
        spst = [g.carve([64, 256], BF16, name="spstb") for _ in range(2)]
        g.memset("dve", Sb[:, :], 0.0, [Sb])
        order = [33, 32] + list(range(31, -1, -1))
        for i, ch in enumerate(order):
            r0 = ch * 128 if ch < 32 else T + (ch - 32) * 128
            kv = kvb[i % 2]
            g.ld(kv[:, :], TMd[r0:r0 + 128, 0:512], [TMd], [kv])
            g.tt("dve", kw[:, :], kv[:, 0:256], WK[:, 1, :], ALU.mult, [kv, WK], [kw])
            ups = PB[i % 2]
            for h in range(4):
                g.mm(ups[0:64, h * 64:(h + 1) * 64], kw[:, h * 64:(h + 1) * 64], kv[:, 256 + h * 64:256 + (h + 1) * 64],
                     True, True, [kw, kv], [ups])
            sp = spst[i % 2]
            g.cp("act", sp[:, :], Sb[:, :], [Sb], [sp])
            g.ld(SPd.t.ap()[1, ch], sp[:, :], [sp], [SPd])
            g.tt("pool", Sb[:, :], Sb[:, :], DEC[:, 1, :], ALU.mult, [Sb, DEC], [Sb])
            g.tt("dve", Sb[:, :], Sb[:, :], ups[0:64, 0:256], ALU.add, [Sb, ups], [Sb])

    def phase_mix(l, need_ctx):
        g.reset()
        wo_cr = WBIG[:, 0:4096].rearrange("p (c n) -> p c n", c=4)
        wo_at = WBIG[0:64, 8192:16384].rearrange("p (h n) -> p h n", h=8)
        g.ld(wo_cr, w_out.t.ap()[l, 0:512, :].rearrange("(c p) n -> p c n", p=128), [w_out], [(WBIG, 0)], q="pool")
        g.ld(wo_at, w_out.t.ap()[l, 512:1024, :].rearrange("(h p) n -> p h n", p=64), [w_out], [(WBIG, 1)], q="pool")
        Fv = Fd.t.ap().rearrange("(h p) t -> p h t", p=64)
        g.ld(KC[:, :, :], Fv[:, 16:18, T:TT], [Fd], [KC])
        g.ld(VC[:, :, :], TMd[T:TT, 1024:1152].rearrange("(a p) c -> p a c", p=128), [TMd], [VC])
        RQK = g.carve([64, 8, 512], BF16, name="RQK")
        AQ = g.carve([64, 8, 512], BF16, name="AQ")
        AK = g.carve([64, 2, 768], BF16, name="AK")
        TMt = g.carve([128, 4, 768], BF16, name="TMt")
        AV = g.carve([128, 6, 128], BF16, name="AV")
        Zt = g.carve([128, 2, 514], name="Zt")
        CBt = g.carve([128, 2, 512], BF16, name="CBt")
        xin = g.carve([128, 4, D], name="xin2")
        G1r = [g.carve([128, D], name="G1r") for _ in range(2)]
        SPt = g.carve([64, 2, 4, 256], BF16, name="SPt")
        mconv = g.carve([128, 2, 512], BF16, name="mconv")
        ctmp = g.carve([128, 512], name="ctmp")
        STf = g.carve([128, 4, 128], BF16, name="STf"); STb = g.carve([128, 4, 128], BF16, name="STb")
        qd = g.carve([64, 2, 4, 128], BF16, name="qd")
        t1 = g.carve([128, 512], name="t1m"); sq = g.carve([128, 512], name="sq")
        st8 = g.carve([128, 6, 8], name="st8")
        ret = g.carve([128, 256], BF16, name="ret")
        mret = g.carve([128, 2, 128], BF16, name="mret")
        PTs = [g.carve([128, 4, 128], BF16, name="PTs") for _ in range(2)]
        matt = g.carve([64, 8, 128], BF16, name="matt")
        den = g.carve([64, 512], name="den")
        ss = g.carve([128, 4], name="ss2"); rstd = g.carve([128, 4], name="rstd2")
        junk = g.carve([128, D], name="junk2")
        xn2 = g.carve([128, D], name="xn2"); xn2b = g.carve([128, D], BF16, name="xn2b")
        h2T = g.carve([128, 8, 128], name="h2T")
        ex = [g.carve([16, 128], name="ex") for _ in range(2)]
        for s in range(2 if need_ctx else 1):
            g.ld(G1r[s][:, :], MROWd.t.ap()[0, s:s + 1, :].partition_broadcast(128), [MROWd], [G1r[s]])
        blk = 0
        for (s, t0, n) in tiles(need_ctx):
            nsub = n // 128
            lat = s == 0
            g.ld(RQK[:, :, 0:n], Fv[:, 0:8, t0:t0 + n], [Fd], [RQK])
            g.ld(AQ[:, :, 0:n], Fv[:, 8:16, t0:t0 + n], [Fd], [AQ])
            if lat:
                lo = max(t0 - 128, 0); hi = min(t0 + n + 128, T)
                koff = t0 - lo
                g.ld(AK[:, :, 0:hi - lo], Fv[:, 16:18, lo:hi], [Fd], [AK])
                g.ld(AV[:, 0:(hi - lo) // 128, :], TMd[lo:hi, 1024:1152].rearrange("(a p) c -> p a c", p=128), [TMd], [AV])
            g.ld(TMt[:, 0:nsub, :], TMd[t0:t0 + n, 256:1024].rearrange("(a p) c -> p a c", p=128), [TMd], [TMt])
            zc = zcol(t0)
            g.ld(Zt[:, :, 0:n + 2], Zd.t.ap()[:, :, zc - 1:zc + n + 1].rearrange("c p t -> p c t"), [Zd], [Zt])
            g.ld(CBt[:, :, 0:n], CBd.t.ap()[:, :, t0:t0 + n].rearrange("c p t -> p c t"), [CBd], [CBt])
            g.ld(xin[:, 0:nsub, :], xres[t0:t0 + n, :].rearrange("(a p) d -> p a d", p=128), [xres], [xin])
            ch0 = chunk_of(t0)
            for d_ in range(2):
                g.ld(SPt[:, d_, 0:nsub, :], SPd.t.ap()[d_, ch0:ch0 + nsub].rearrange("a p f -> p a f"), [SPd], [SPt])
            for c in range(2 if "conv" not in SKIP else 0):
                wb = l * 6
                g.ts("dve", ctmp[:, 0:n], Zt[:, c, 0:n], cw[:, wb + 0 * 2 + c:wb + 0 * 2 + c + 1], None, ALU.mult, None, [Zt, cw], [ctmp])
                g.stt(ctmp[:, 0:n], Zt[:, c, 1:n + 1], cw[:, wb + 1 * 2 + c:wb + 1 * 2 + c + 1], ctmp[:, 0:n], ALU.mult, ALU.add, [Zt, cw, ctmp], [ctmp])
                g.stt(ctmp[:, 0:n], Zt[:, c, 2:n + 2], cw[:, wb + 2 * 2 + c:wb + 2 * 2 + c + 1], ctmp[:, 0:n], ALU.mult, ALU.add, [Zt, cw, ctmp], [ctmp])
                g.tt("dve", mconv[:, c, 0:n], ctmp[:, 0:n], CBt[:, c, 0:n], ALU.mult, [ctmp, CBt], [mconv])
            for a in range(nsub if "blocks" not in SKIP else 0):
                sl = slice(a * 128, (a + 1) * 128)
                if "ret" not in SKIP:
                    stp = PB[0]
                    for h in range(4):
                        g.mm(stp[:, h * 128:(h + 1) * 128], RQK[:, 4 + h, sl], RQK[:, h, sl], True, True, [RQK], [stp])
                    g.tt("dve", STf[:, :, :], v3(stp[:, :], 4), DMf[:, :, :], ALU.mult, [stp, DMf], [STf])
                    g.tt("dve", STb[:, :, :], v3(stp[:, :], 4), DMb[:, :, :], ALU.mult, [stp, DMb], [STb])
                    for d in range(2):
                        g.tt(PE2, qd[:, d, :, :], RQK[:, 0:4, sl], QD[:, d, :, :], ALU.mult, [RQK, QD], [qd])
                    op_ = PB[1]
                    for d in range(2 if "ret_o" not in SKIP else 0):
                        STd = STf if d == 0 else STb
                        for h in range(4):
                            o_ap = op_[:, d * 256 + h * 64:d * 256 + (h + 1) * 64]
                            g.mm(o_ap, STd[:, h, :], TMt[:, a, h * 64:(h + 1) * 64], True, "ret_q" in SKIP, [STd, TMt], [op_])
                            if "ret_q" not in SKIP:
                                g.mm(o_ap, qd[:, d, h, :], SPt[:, d, a, h * 64:(h + 1) * 64], False, True, [qd, SPt], [op_])
                    o3 = v3(op_[:, :], 8)
                    g.red(st8[:, 0, :], o3, [op_], [st8])
                    g.act(sq[:, :], op_[:, :], AF.Square, [op_], [sq])
                    g.red(st8[:, 1, :], v3(sq[:, :], 8), [sq], [st8])
                    g.ts("dve", st8[:, 2, :], st8[:, 0, :], 1.0 / 64, None, ALU.mult, None, [st8], [st8])
                    g.tt("dve", st8[:, 5, :], st8[:, 2, :], st8[:, 2, :], ALU.mult, [st8], [st8])
                    g.stt(st8[:, 3, :], st8[:, 1, :], 1.0 / 64, st8[:, 5, :], ALU.mult, ALU.subtract, [st8], [st8])
                    g.ts("dve", st8[:, 3, :], st8[:, 3, :], EPS, None, ALU.add, None, [st8], [st8])
                    g.act(st8[:, 3, :], st8[:, 3, :], AF.Sqrt, [st8], [st8])
                    g.recip(st8[:, 3, :], st8[:, 3, :], [st8], [st8])
                    g.stt(st8[:, 4, :], st8[:, 2, :], -1.0, st8[:, 3, :], ALU.mult, ALU.mult, [st8], [st8])
                    t13 = v3(t1[:, :], 8)
                    g.tt("dve", t13, o3, st8[:, 3, :].unsqueeze(2).to_broadcast([128, 8, 64]), ALU.mult, [op_, st8], [t1])
                    g.tt(PE2, t13, t13, st8[:, 4, :].unsqueeze(2).to_broadcast([128, 8, 64]), ALU.add, [t1, st8], [t1])
                    g.tt(PE2, t1[:, :], t1[:, :], TMt[:, a, 256:768], ALU.mult, [t1, TMt], [t1])
                    g.tt("dve", ret[:, :], t1[:, 0:256], t1[:, 256:512], ALU.add, [t1], [ret])
                    rtp = PT[0]
                    for c in range(2):
                        g.tr(rtp[:, c * 128:(c + 1) * 128], ret[:, c * 128:(c + 1) * 128], identb[:, :], [ret, identb], [rtp])
                    g.cp("act", mret[:, :, :], v3(rtp[:, 0:256], 2), [rtp], [mret])
                if "att" not in SKIP:
                    qb = t0 // 128 + a
                    for kvh in range(2):
                        kbl = []
                        if lat:
                            if qb > 0:
                                kbl.append(("prev", AK[:, kvh, koff + (a - 1) * 128:koff + a * 128], AV[:, (koff // 128) + a - 1, kvh * 64:(kvh + 1) * 64], [AK], [AV]))
                            kbl.append(("same", AK[:, kvh, koff + a * 128:koff + (a + 1) * 128], AV[:, (koff // 128) + a, kvh * 64:(kvh + 1) * 64], [AK], [AV]))
                            if qb < T // 128 - 1:
                                kbl.append(("next", AK[:, kvh, koff + (a + 1) * 128:koff + (a + 2) * 128], AV[:, (koff // 128) + a + 1, kvh * 64:(kvh + 1) * 64], [AK], [AV]))
                        for cb_ in range(2):
                            kbl.append(("ctx", KC[:, kvh, cb_ * 128:(cb_ + 1) * 128], VC[:, cb_, kvh * 64:(kvh + 1) * 64], [KC], [VC]))
                        aps, bps = PB[4], PB[5]
                        for i, (kind, kT, vv, kR, vR) in enumerate(kbl):
                            sp_ = PB[2 + blk % 2]
                            pts = PTs[blk % 2]
                            blk += 1
                            g.mm(v3(sp_[:, :], 4), kT, AQ[:, kvh * 4:(kvh + 1) * 4, sl], True, True, kR + [AQ], [sp_])
                            g.act(pts[:, :, :], v3(sp_[:, :], 4), AF.Exp, [sp_], [pts], scale=0.125)
                            if kind == "prev":
                                g.tt(PE2, pts[:, :, :], pts[:, :, :], Mprev[:, :, :], ALU.mult, [pts, Mprev], [pts])
                            elif kind == "next":
                                g.tt(PE2, pts[:, :, :], pts[:, :, :], Mnext[:, :, :], ALU.mult, [pts, Mnext], [pts])
                            g.mm(v3(aps[0:64, :], 4), vv, pts[:, :, :], i == 0, i == len(kbl) - 1, vR + [pts], [aps])
                            g.mm(v3(bps[0:64, :], 4), ones_bf[:, :], pts[:, :, :], i == 0, i == len(kbl) - 1, [ones_bf, pts], [bps])
                        g.tt("dve", den[:, :], bps[0:64, :], SINKE[:, kvh, :], ALU.add, [bps, SINKE], [den])
                        g.recip(den[:, :], den[:, :], [den], [den])
                        g.tt("dve", matt[:, kvh * 4:(kvh + 1) * 4, :], v3(aps[0:64, :], 4), v3(den[:, :], 4), ALU.mult, [aps, den], [matt])
                if "wout" not in SKIP:
                    yps = [PB[0], PB[1]]
                    for nh in range(2):
                        ns = slice(nh * 512, (nh + 1) * 512)
                        for c in range(2):
                            g.mm(yps[nh][:, :], mconv[:, c, sl], wo_cr[:, c, ns], c == 0, False, [mconv, (WBIG, 0)], [yps[nh]])
                        for c in range(2):
                            g.mm(yps[nh][:, :], mret[:, c, :], wo_cr[:, 2 + c, ns], False, False, [mret, (WBIG, 0)], [yps[nh]])
                        for h in range(8):
                            g.mm(yps[nh][:, :], matt[:, h, :], wo_at[:, h, ns], False, h == 7, [matt, (WBIG, 1)], [yps[nh]])
                        g.tt("dve", junk[:, ns], yps[nh][:, :], G1r[s][:, ns], ALU.mult, [yps[nh], G1r[s]], [junk])
                        g.tt(PE2, xin[:, a, ns], xin[:, a, ns], junk[:, ns], ALU.add, [xin, junk], [xin])
                    g.ld(xres[t0 + a * 128:t0 + (a + 1) * 128, :], xin[:, a, :], [xin], [xres])
                if "n2" not in SKIP:
                    g.act(junk[:, :], xin[:, a, :], AF.Square, [xin], [junk, ss], accum=ss[:, 0:1])
                    g.ts("dve", rstd[:, 0:1], ss[:, 0:1], 1.0 / D, EPS, ALU.mult, ALU.add, [ss], [rstd])
                    g.act(rstd[:, 0:1], rstd[:, 0:1], AF.Sqrt, [rstd], [rstd])
                    g.recip(rstd[:, 0:1], rstd[:, 0:1], [rstd], [rstd])
                    g.ts("dve", xn2[:, :], xin[:, a, :], rstd[:, 0:1], None, ALU.mult, None, [xin, rstd], [xn2])
                    g.cp(PE2, xn2b[:, :], xn2[:, :], [xn2], [xn2b])
                    g.ld(XN2d[t0 + a * 128:t0 + (a + 1) * 128, :], xn2b[:, :], [xn2b], [XN2d])
                    for kc in range(8):
                        tps = PB[2 + (kc // 4)]
                        g.tr(tps[:, (kc % 4) * 128:(kc % 4 + 1) * 128], xn2[:, kc * 128:(kc + 1) * 128], identf[:, :], [xn2, identf], [tps])
                        g.act(h2T[:, kc, :], tps[:, (kc % 4) * 128:(kc % 4 + 1) * 128], AF.Identity, [tps, AB], [h2T],
                              bias=AB[:, 3, kc, s:s + 1], scale=AB[:, 2, kc, s:s + 1])
                    lps = PB[4]
                    for kc in range(8):
                        g.mm(lps[0:16, 0:128], Wr[:, kc, :], h2T[:, kc, :], kc == 0, kc == 7, [Wr, h2T], [lps])
                    exb = ex[a % 2]
                    g.act(exb[:, :], lps[0:16, 0:128], AF.Exp, [lps], [exb])
                    g.ld(EXd[:, t0 + a * 128:t0 + (a + 1) * 128], exb[:, :], [exb], [EXd])

    def phase_route(l, need_ctx):
        g.reset()
        EXT = g.carve([16, TT], name="EXT")
        rden = g.carve([16, 512], name="rden")
        COMB = g.carve([32, T], name="COMB")
        PBc = [g.carve([128, T], name="PBc") for _ in range(2)]
        jA = g.carve([128, T], BF16, name="jA")
        jB = g.carve([128, T], BF16, name="jB")
        sc = g.carve([32, 8], name="scr")
        lo, hi, mid, cnt, pp, dd, kvec = (sc[:, i:i + 1] for i in range(7))
        g.ld(EXT[:, :], EXd[:, :], [EXd], [EXT])
        for c0 in range(0, TT, 512):
            w = min(512, TT - c0)
            dps = PB[(c0 // 512) % 2]
            g.mm(dps[0:16, 0:w], ones16[:, :], EXT[:, c0:c0 + w], True, True, [ones16, EXT], [dps])
            g.recip(rden[:, 0:w], dps[0:16, 0:w], [dps], [rden])
            g.tt("dve", EXT[:, c0:c0 + w], EXT[:, c0:c0 + w], rden[:, 0:w], ALU.mult, [EXT, rden], [EXT])
        g.ld(EXd[:, :], EXT[:, :], [EXT], [EXd])
        g.memset("dve", COMB[:, :], 0.0, [COMB])
        g.ld(COMB[0:16, :], EXd[:, 0:T], [EXd], [COMB])
        if need_ctx:
            g.ld(COMB[16:32, 0:C], EXd[:, T:TT], [EXd], [COMB])
        g.memset("dve", sc[:, :], 0.0, [sc])
        g.memset("dve", hi, 1.0, [sc])
        g.memset("dve", kvec, float(CAPC), [sc])
        g.memset("dve", sc[0:16, 6:7], float(CAP), [sc])
        for it in range(36):
            g.tt("dve", mid, lo, hi, ALU.add, [sc], [sc])
            g.ts("dve", mid, mid, 0.5, None, ALU.mult, None, [sc], [sc])
            g.ts("dve", jA[0:32, :], COMB[:, :], mid, 0.0, ALU.is_gt, ALU.add, [COMB, sc], [jA, sc], accum=cnt)
            g.tt("dve", pp, cnt, kvec, ALU.is_gt, [sc], [sc])
            g.tt("dve", dd, mid, lo, ALU.subtract, [sc], [sc])
            g.stt(lo, dd, pp, lo, ALU.mult, ALU.add, [sc], [sc])
            g.tt("dve", dd, hi, mid, ALU.subtract, [sc], [sc])
            g.stt(hi, dd, pp, mid, ALU.mult, ALU.add, [sc], [sc])
        g.ts("dve", jA[0:32, :], COMB[:, :], hi, None, ALU.is_gt, None, [COMB, sc], [jA])
        zr = PBc[1]
        g.memset("dve", zr[0:32, :], 0.0, [zr])
        kb.op("dve", lambda e: e.tensor_tensor_scan(COMB[:, :], jA[0:32, :], zr[0:32, :], 0.0, ALU.add, ALU.add), [jA, zr], [COMB])
        g.ld(POSd[:, :], COMB[:, :], [COMB], [POSd])
        for e in range(NE):
            pb = PBc[e % 2]
            g.ld(pb[:, :], POSd[e:e + 1, :].partition_broadcast(128), [POSd], [pb])
            for jt in range(4):
                col = e * 4 + jt
                if jt < 2:
                    g.ts("dve", jA[:, :], pb[:, :], jval[:, jt:jt + 1], 0.0, ALU.is_le, ALU.add,
                         [pb, jval], [jA, LISTF], accum=LISTF[:, col:col + 1])
                else:
                    g.act(jB[:, :], pb[:, :], AF.Sign, [pb, jb], [jB, SACC], bias=jb[:, jt:jt + 1], scale=-1.0,
                          accum=SACC[:, col:col + 1])
        for e in range(NE):
            g.ts("dve", LISTF[:, e * 4 + 2:e * 4 + 4], SACC[:, e * 4 + 2:e * 4 + 4], float(T), 0.5, ALU.add, ALU.mult, [SACC], [LISTF])
        g.cp("dve", LISTI[:, :], LISTF[:, :], [LISTF], [LISTI])
        if need_ctx:
            for e in range(NE):
                pb = PBc[e % 2]
                g.ld(pb[0:32, 0:C], POSd[16 + e:17 + e, 0:C].partition_broadcast(32), [POSd], [pb])
                g.ts("dve", jA[0:32, 0:C], pb[0:32, 0:C], jval[0:32, 0:1], 0.0, ALU.is_le, ALU.add,
                     [pb, jval], [jA, LISTCF], accum=LISTCF[:, e:e + 1])
            g.ts("dve", LISTCF[:, :], LISTCF[:, :], float(T), None, ALU.add, None, [LISTCF], [LISTCF])
            g.cp("dve", LISTCI[:, :], LISTCF[:, :], [LISTCF], [LISTCI])

    def phase_ffn(l, need_ctx):
        g.reset()
        wparts = [WBIG[:, i * 8192:(i + 1) * 8192].rearrange("p (k n) -> p k n", k=8) for i in range(4)]
        G2r = [g.carve([128, D], name="G2r") for _ in range(2)]
        for s in range(2 if need_ctx else 1):
            g.ld(G2r[s][:, :], MROWd.t.ap()[1, s:s + 1, :].partition_broadcast(128), [MROWd], [G2r[s]])
        XG = [[g.carve([128, D], BF16, name="XG") for _ in range(4)] for _ in range(2)]
        XGc = [g.carve([32, D], BF16, name="XGc") for _ in range(2)]
        GT = [g.carve([128, 4], name="GT") for _ in range(2)]
        GTc = [g.carve([32, 1], name="GTc") for _ in range(2)]
        xsT = g.carve([128, 8, 544], BF16, name="xsT")
        actT = g.carve([128, 8, 544], BF16, name="actT")
        sg = [g.carve([128, 544], name="sg") for _ in range(2)]
        YS = [g.carve([128, D], name="YS") for _ in range(2)]
        NCOL = 544 if need_ctx else 512
        exd_flat = EXd.t.ap().rearrange("e (t o) -> (e t) o", o=1)
        wcount = 0

        def load_w(src):
            nonlocal wcount
            part = wcount % 4
            wcount += 1
            wv = wparts[part]
            for hh in range(2):
                g.ld(wv[:, hh * 4:(hh + 1) * 4, :], src[hh * 512:(hh + 1) * 512, :].rearrange("(k p) n -> p k n", p=128),
                     [w_gate, w_up, w_down], [(WBIG, part)], q="pool")
            return wv, (WBIG, part)

        def gathers(e):
            b = e % 2
            for jt in range(4):
                idx = LISTI[:, e * 4 + jt:e * 4 + jt + 1]
                g.gather(XG[b][jt][:, :], XN2d[:, :], idx, [XN2d, LISTI], [XG[b][jt]])
                g.gather(GT[b][:, jt:jt + 1], exd_flat, idx, [EXd, LISTI], [GT[b]], elem_off=e * TT)
            if need_ctx:
                idc = LISTCI[:, e:e + 1]
                g.gather(XGc[b][:, :], XN2d[:, :], idc, [XN2d, LISTCI], [XGc[b]])
                g.gather(GTc[b][:, :], exd_flat, idc, [EXd, LISTCI], [GTc[b]], elem_off=e * TT)

        gathers(0)
        for e in range(NE):
            b = e % 2
            wg, wgp = load_w(w_gate.t.ap()[l, e])
            wu, wup = load_w(w_up.t.ap()[l, e])
            wd, wdp = load_w(w_down.t.ap()[l, e])
            if e + 1 < NE:
                gathers(e + 1)
            for kc in range(8):
                tp = PT[kc % 2]
                for jt in range(4):
                    g.tr(tp[:, jt * 128:(jt + 1) * 128], XG[b][jt][:, kc * 128:(kc + 1) * 128], identb[:, :], [XG[b][jt], identb], [tp])
                g.act(xsT[:, kc, 0:512], tp[:, 0:512], AF.Identity, [tp, AB], [xsT], bias=AB[:, 3, kc, 0:1], scale=AB[:, 2, kc, 0:1])
                if need_ctx:
                    g.tr(tp[:, 512:544], XGc[b][:, kc * 128:(kc + 1) * 128], identb[0:32, 0:32], [XGc[b], identb], [tp])
                    g.act(xsT[:, kc, 512:544], tp[:, 512:544], AF.Identity, [tp, AB], [xsT], bias=AB[:, 3, kc, 1:2], scale=AB[:, 2, kc, 1:2])
            for fb in range(8):
                aps, ups = PB[(fb % 2) * 2], PB[(fb % 2) * 2 + 1]
                apc = PB[4]
                fs = slice(fb * 128, (fb + 1) * 128)
                for kc in range(8):
                    g.mm(aps[:, :], wg[:, kc, fs], xsT[:, kc, 0:512], kc == 0, kc == 7, [wgp, xsT], [aps])
                for kc in range(8):
                    g.mm(ups[:, :], wu[:, kc, fs], xsT[:, kc, 0:512], kc == 0, kc == 7, [wup, xsT], [ups])
                sgb = sg[fb % 2]
                g.act(sgb[:, 0:512], aps[:, :], AF.Silu, [aps], [sgb])
                g.tt("dve", actT[:, fb, 0:512], sgb[:, 0:512], ups[:, :], ALU.mult, [sgb, ups], [actT])
                if need_ctx:
                    for kc in range(8):
                        g.mm(apc[:, 0:32], wg[:, kc, fs], xsT[:, kc, 512:544], kc == 0, kc == 7, [wgp, xsT], [apc])
                    for kc in range(8):
                        g.mm(apc[:, 32:64], wu[:, kc, fs], xsT[:, kc, 512:544], kc == 0, kc == 7, [wup, xsT], [apc])
                    g.act(sgb[:, 512:544], apc[:, 0:32], AF.Silu, [apc], [sgb])
                    g.tt("dve", actT[:, fb, 512:544], sgb[:, 512:544], apc[:, 32:64], ALU.mult, [sgb, apc], [actT])
            for st in range(5 if need_ctx else 4):
                ys = YS[st % 2]
                m = 128 if st < 4 else 32
                for nh in range(2):
                    yp = PB[4 + nh]
                    ns = slice(nh * 512, (nh + 1) * 512)
                    cs = slice(st * 128, st * 128 + m)
                    for fb in range(8):
                        g.mm(yp[0:m, :], actT[:, fb, cs], wd[:, fb, ns], fb == 0, fb == 7, [actT, wdp], [yp])
                    if st < 4:
                        g.stt(ys[:, ns], yp[:, :], GT[b][:, st:st + 1], G2r[0][:, ns], ALU.mult, ALU.mult, [yp, GT[b], G2r[0]], [ys])
                    else:
                        g.stt(ys[0:32, ns], yp[0:32, :], GTc[b][:, 0:1], G2r[1][0:32, ns], ALU.mult, ALU.mult, [yp, GTc[b], G2r[1]], [ys])
                if st < 4:
                    g.scatter_add(xres[:, :], ys[:, :], LISTI[:, e * 4 + st:e * 4 + st + 1], [ys, LISTI], [xres])
                else:
                    g.scatter_add(xres[:, :], ys[0:32, :], LISTCI[:, e:e + 1], [ys, LISTCI], [xres])

    def phase_final():
        g.reset()
        fgr = g.carve([128, D], name="fgr")
        g.ld(fgr[:, :], fg_d[0:1, :].partition_broadcast(128), [fg_d], [fgr])
        xin = [g.carve([128, 4, D], name="xinf") for _ in range(2)]
        ss = g.carve([128, 4], name="ssf"); rstd = g.carve([128, 4], name="rstdf")
        junk = g.carve([128, D], name="junkf")
        for i, (s, t0, n) in enumerate(tiles(False)):
            xt = xin[i % 2]
            g.ld(xt[:, :, :], xres[t0:t0 + n, :].rearrange("(a p) d -> p a d", p=128), [xres], [xt])
            rms_stats(xt, 4, ss, rstd, junk)
            for a in range(4):
                g.stt(xt[:, a, :], xt[:, a, :], rstd[:, a:a + 1], fgr[:, :], ALU.mult, ALU.mult, [xt, rstd, fgr], [xt])
            g.ld(out_d[t0:t0 + n, :].rearrange("(a p) d -> p a d", p=128), xt[:, :, :], [xt], [out_d])

    setup()
    for l in range(n_layers):
        need_ctx = l < DEPTH - 1
        phase_mod(l)
        if stop_after == ("mod", l):
            break
        phase_proj(l)
        phase_bwd(l)
        if stop_after == ("proj", l):
            break
        phase_mix(l, need_ctx)
        if stop_after == ("mix", l):
            break
        phase_route(l, need_ctx)
        if stop_after == ("route", l):
            break
        phase_ffn(l, need_ctx)
    phase_final()
    if debug:
        g.ld(DBGd[:, 0:64], LISTF[:, :], [LISTF], [DBGd])
        g.ld(DBGd[0:32, 64:80], LISTCF[:, :], [LISTCF], [DBGd])
        g.ld(DBGd[:, 128:192], AB.t.ap().rearrange("p a b c -> p (a b c)") if False else AB[:, :, :, :].rearrange("p a b c -> p (a b c)"), [AB], [DBGd])
    kb.finish()
    kb.emit()
    es.close()
    return nc, kb


def _prep_inputs(x, c, ctx, c_ctx, w_mod, b_mod, norm1_g, norm2_g, w_in, conv_w, ret_decay_logit,
                 attn_sink, w_out, w_router, w_gate, w_up, w_down, final_g):
    f = lambda a: np.ascontiguousarray(np.asarray(a, dtype=np.float32))
    cos, sin = _rope_tables()
    perm = _win_perm()
    shared = {
        "w_mod": f(w_mod),
        "bm_t": f(np.asarray(b_mod).reshape(DEPTH, 48, 128).transpose(2, 0, 1)),
        "b_mod": f(b_mod),
        "g1t": f(np.asarray(norm1_g).reshape(DEPTH, 8, 128).transpose(2, 0, 1)),
        "g2t": f(np.asarray(norm2_g).reshape(DEPTH, 8, 128).transpose(2, 0, 1)),
        "w_in": f(np.asarray(w_in)[:, :, perm]),
        "cw_t": f(np.asarray(conv_w).reshape(DEPTH, 3, 2, 128).transpose(3, 0, 1, 2).reshape(128, DEPTH * 6)),
        "rdl": f(np.asarray(ret_decay_logit).reshape(1, 32)),
        "sink": f(np.asarray(attn_sink).reshape(1, 32)),
        "w_out": f(w_out), "w_router": f(w_router),
        "w_gate": f(w_gate), "w_up": f(w_up), "w_down": f(w_down),
        "final_g": f(np.asarray(final_g).reshape(1, D)),
        "rope_cos": cos, "rope_sin": sin,
    }
    maps = []
    for core in range(8):
        b = core % 4
        m = dict(shared)
        m["x"] = f(np.asarray(x)[b])
        m["ctx"] = f(np.asarray(ctx)[b])
        m["cvec"] = f(np.stack([np.asarray(c)[b], np.asarray(c_ctx)], 0))
        maps.append(m)
    return maps


_CACHE = {}


def kernel(**inputs):
    maps = _prep_inputs(**inputs)
    if "nc" not in _CACHE:
        _CACHE["nc"] = build()[0]
    nc = _CACHE["nc"]
    res = run_bass_kernel_spmd(nc, maps, core_ids=list(range(8)))
    out = np.stack([np.asarray(res.results[b]["out"], dtype=np.float32) for b in range(4)], 0)
    return out
```

```python
import os
import numpy as np
from contextlib import ExitStack
import concourse.bass as bass
import concourse.mybir as mybir
from concourse.alu_op_type import AluOpType as ALU
from concourse.bass_utils import run_bass_kernel_spmd

F32 = mybir.dt.float32
BF16 = mybir.dt.bfloat16
I32 = mybir.dt.int32
AF = mybir.ActivationFunctionType
AX = mybir.AxisListType

SEM_LIMIT = 4000
NQ = 12

D = 1024
T = 4096
C = 256
TT = T + C
DEPTH = 4
NE = 16
CAP = 512
CAPC = 32
NW = 3968
EPS = 1e-6
ZW = T + C + 4
PE2 = "pool"
SKIP = set(os.environ.get("KSKIP", "").split(","))


class Buf:
    def __init__(self, t, nparts=1, name="", excl=False):
        self.t = t
        self.name = name
        self.excl = excl
        self.lw = [None] * nparts
        self.rd = [[] for _ in range(nparts)]
        self.np_ = nparts

    def __getitem__(self, k):
        return self.t[k]


class BV:
    def __init__(self, parent, ap):
        self.parent = parent
        self.t = ap

    def __getitem__(self, k):
        return self.t[k]


def _acc(x):
    if isinstance(x, BV):
        x = x.parent
    if isinstance(x, Buf):
        return x, range(x.np_)
    b, p = x
    if isinstance(b, BV):
        b = b.parent
    if isinstance(p, int):
        p = [p]
    return b, p


class Op:
    __slots__ = ("id", "eng", "fn", "fence", "deps", "cost", "lat", "dma", "start")

    def __init__(self, id, eng, fn, fence, deps, cost, lat, dma):
        self.id = id; self.eng = eng; self.fn = fn; self.fence = fence
        self.deps = deps; self.cost = cost; self.lat = lat; self.dma = dma; self.start = 0.0


class KB:
    def __init__(self, nc, es, same_engine_sync=True):
        self.nc = nc
        self.es = es
        self.eng = {"pe": nc.tensor, "dve": nc.vector, "act": nc.scalar, "pool": nc.gpsimd, "sp": nc.sync}
        self.nsem = 0
        self.ses = same_engine_sync
        self.n_instr = 0
        self.segs = [[]]
        self.nops = 0
        self._fz = nc.alloc_sbuf_tensor("fencez", [128, 8], F32)
        self.fzb = Buf(self._fz, 1, "fencez")
        self.op("dve", lambda e: e.memset(self._fz[:, :], 0.0), (), [self.fzb])

    def _sem(self, name):
        self.nsem += 1
        return self.es.enter_context(self.nc.semaphore(f"{name}_{self.nsem}"))

    def _deps(self, reads, writes):
        deps = set()
        for x in reads:
            b, ps = _acc(x)
            for p in ps:
                if b.lw[p] is not None:
                    deps.add(b.lw[p])
                if b.excl:
                    deps.update(b.rd[p])
        for x in writes:
            b, ps = _acc(x)
            for p in ps:
                if b.lw[p] is not None:
                    deps.add(b.lw[p])
                deps.update(b.rd[p])
        return deps

    def _record(self, oid, reads, writes):
        for x in reads:
            b, ps = _acc(x)
            for p in ps:
                b.rd[p].append(oid)
        for x in writes:
            b, ps = _acc(x)
            for p in ps:
                b.lw[p] = oid
                b.rd[p] = []

    def _add(self, e, fn, reads, writes, fence, cost, lat, dma):
        if fence:
            reads = list(reads) + [self.fzb]
        oid = self.nops
        self.nops += 1
        o = Op(oid, e, fn, fence, self._deps(reads, writes), float(cost), float(lat), dma)
        self.segs[-1].append(o)
        self._record(oid, reads, writes)
        self.n_instr += 1
        return oid

    def op(self, e, fn, reads=(), writes=(), fence=False, cost=150.0):
        return self._add(e, fn, reads, writes, fence, cost, cost + 80.0, False)

    def dma(self, q, fn, reads=(), writes=(), nbytes=0):
        issue = 120.0 if q == "sp" else 900.0
        lat = 2200.0 + nbytes / 120.0
        return self._add(q, fn, reads, writes, False, issue, lat, True)

    def barrier(self):
        if self.segs[-1]:
            self.segs.append([])

    def finish(self):
        pass

    def sb(self, name, shape, dt=F32, nparts=1):
        return Buf(self.nc.alloc_sbuf_tensor(name, list(shape), dt), nparts, name)

    def ps(self, name, shape, dt=F32, nparts=1):
        return Buf(self.nc.alloc_psum_tensor(name, list(shape), dt), nparts, name, excl=True)

    def dram(self, name, shape, dt=F32, kind="Internal", nparts=1):
        return Buf(self.nc.dram_tensor(name, list(shape), dt, kind=kind), nparts, name)

    def _schedule(self, seg):
        ids = {o.id for o in seg}
        byid = {o.id: o for o in seg}
        succ = {o.id: [] for o in seg}
        indeg = {}
        for o in seg:
            d = [x for x in o.deps if x in ids]
            indeg[o.id] = len(d)
            for x in d:
                succ[x].append(o.id)
        free = {e: 0.0 for e in self.eng}
        rt = {o.id: 0.0 for o in seg}
        ready = {e: [] for e in self.eng}
        for o in seg:
            if indeg[o.id] == 0:
                ready[o.eng].append(o.id)
        order = []
        n = len(seg)
        while len(order) < n:
            best = None
            for e, lst in ready.items():
                if not lst:
                    continue
                f = free[e]
                c = min(lst, key=lambda i: (max(f, rt[i]), i))
                key = (max(f, rt[c]), c)
                if best is None or key < best[0]:
                    best = (key, e, c)
            (st, _), e, c = best
            o = byid[c]
            ready[e].remove(c)
            o.start = st
            free[e] = st + o.cost
            fin = st + o.lat
            order.append(o)
            for sx in succ[c]:
                if rt[sx] < fin:
                    rt[sx] = fin
                indeg[sx] -= 1
                if indeg[sx] == 0:
                    ready[byid[sx].eng].append(sx)
        return order

    def emit(self):
        esem = {}; ecnt = {}; eretired = []
        dsem = {}; dcnt = {}; dretired = []
        for e in self.eng:
            esem[e] = self._sem("e_" + e); ecnt[e] = 0
        for q in ("sp", "pool"):
            dsem[q] = [self._sem(f"d_{q}{i}") for i in range(NQ)]; dcnt[q] = 0
        waited = {e: {} for e in self.eng}
        prog = {e: [] for e in self.eng}
        event = {}

        def wait(e, evs):
            w = waited[e]
            best = {}
            for (sem, val, src) in evs:
                if src == e and (e == "pe" or not self.ses):
                    continue
                k = id(sem)
                if w.get(k, 0) >= val:
                    continue
                if k not in best or best[k][1] < val:
                    best[k] = (sem, val)
            for k, (sem, val) in best.items():
                prog[e].append(("wait", sem, val))
                w[k] = val

        def all_events():
            evs = list(eretired) + list(dretired)
            for e in self.eng:
                if ecnt[e] > 0:
                    evs.append((esem[e], ecnt[e], e))
            for q in dsem:
                n = dcnt[q]
                for j, sm in enumerate(dsem[q]):
                    cnt = (n - j + NQ - 1) // NQ if n > j else 0
                    if cnt > 0:
                        evs.append((sm, 16 * cnt, "dma"))
            return evs

        self.sched_span = []
        for si, seg in enumerate(self.segs):
            if not seg:
                continue
            if si > 0:
                evs = all_events()
                for e in self.eng:
                    wait(e, evs)
            order = self._schedule(seg)
            self.sched_span.append(max(o.start + o.lat for o in order))
            for o in order:
                e = o.eng
                evs = [event[d] for d in o.deps if d in event]
                if o.dma:
                    i = dcnt[e]
                    if 16 * (i // NQ) + 16 > SEM_LIMIT:
                        for j, sm in enumerate(dsem[e]):
                            cnt = (i - j + NQ - 1) // NQ if i > j else 0
                            if cnt > 0:
                                dretired.append((sm, 16 * cnt, "dma"))
                        dsem[e] = [self._sem(f"d_{e}{k}") for k in range(NQ)]
                        dcnt[e] = 0
                        i = 0
                    sem = dsem[e][i % NQ]
                    prev = 16 * (i // NQ)
                    if prev > 0:
                        evs.append((sem, prev, "dma"))
                    wait(e, evs)
                    prog[e].append(("op", o.fn, False, sem, 16))
                    dcnt[e] += 1
                    event[o.id] = (sem, prev + 16, "dma")
                else:
                    if ecnt[e] >= SEM_LIMIT:
                        eretired.append((esem[e], ecnt[e], e))
                        esem[e] = self._sem("e_" + e); ecnt[e] = 0
                    wait(e, evs)
                    prog[e].append(("op", o.fn, o.fence, esem[e], 1))
                    ecnt[e] += 1
                    event[o.id] = (esem[e], ecnt[e], e)
        wait("sp", all_events())
        self.prog = prog
        with self.nc.Block() as block:
            for e, dec in (("pe", block.tensor), ("dve", block.vector), ("act", block.scalar),
                           ("pool", block.gpsimd), ("sp", block.sync)):
                pr = prog[e]
                if not pr:
                    continue

                def body(eng, pr=pr, e=e):
                    for it in pr:
                        if it[0] == "wait":
                            eng.wait_ge(it[1], it[2])
                        else:
                            _, fn, fence, sem, inc = it
                            ins = fn(eng)
                            if fence:
                                ins = self._fence(e)
                            ins.then_inc(sem, inc)

                dec(body)

    def _fence(self, e):
        if e == "dve":
            return self.nc.vector.tensor_copy(self._fz[0:1, 0:1], self._fz[0:1, 1:2])
        if e == "act":
            return self.nc.scalar.copy(self._fz[0:1, 2:3], self._fz[0:1, 3:4])
        raise ValueError(e)


class G:
    def __init__(self, kb, arena_words):
        self.kb = kb
        self.nc = kb.nc
        self.arena = kb.nc.alloc_sbuf_tensor("arena", [128, arena_words], F32)
        self.aw = arena_words
        self.ap_ = 0
        self.uid = 0

    def reset(self):
        self.kb.barrier()
        self.ap_ = 0

    def carve(self, shape, dt=F32, parts=128, nparts=1, name="a"):
        n = 1
        for s in shape[1:]:
            n *= s
        words = n if dt in (F32, I32) else (n + 1) // 2
        words = (words + 1) // 2 * 2
        off = self.ap_
        self.ap_ += words
        assert self.ap_ <= self.aw, f"arena overflow {self.ap_} > {self.aw} ({name})"
        v = self.arena[0:shape[0], off:off + words]
        if dt != F32:
            v = v.bitcast(dt)
        if dt == BF16:
            v = v[:, 0:n]
        else:
            v = v[:, 0:n]
        if len(shape) == 3:
            v = v.rearrange("p (a b) -> p a b", a=shape[1])
        elif len(shape) == 4:
            v = v.rearrange("p (a b c) -> p a b c", a=shape[1], b=shape[2])
        self.uid += 1
        return Buf(v, nparts, f"{name}{self.uid}")

    @staticmethod
    def nf(ap):
        n = 1
        for d in list(ap.shape)[1:]:
            n *= d
        return n

    def _ecost(self, eng, n):
        if eng == "dve":
            return 70.0 + 1.1 * n
        if eng == "act":
            return 110.0 + 0.95 * n
        return 120.0 + 2.3 * n

    def mm(self, out, lhsT, rhs, st, sp, R, W):
        n = self.nf(rhs)
        c = 70.0 + 0.4 * n
        if lhsT.shape[0] < 128 or self.nf(lhsT) < 128:
            c = 70.0 + 0.85 * n
        self.kb.op("pe", lambda e: e.matmul(out, lhsT, rhs, start=st, stop=sp), R, W, cost=c)

    def tr(self, out, in_, ident, R, W):
        self.kb.op("pe", lambda e: e.transpose(out, in_, ident), R, W, cost=180.0)

    def act(self, out, in_, func, R, W, bias=None, scale=None, accum=None):
        kw = {}
        if bias is not None:
            kw["bias"] = bias
        if scale is not None:
            kw["scale"] = scale
        if accum is not None:
            kw["accum_out"] = accum
        c = self._ecost("act", self.nf(out)) + (150.0 if accum is not None else 0.0)
        self.kb.op("act", lambda e: e.activation(out, in_, func, **kw), R, W, fence=accum is not None, cost=c)

    def tt(self, eng, out, a, b, op, R, W):
        self.kb.op(eng, lambda e: e.tensor_tensor(out, a, b, op), R, W, cost=self._ecost(eng, self.nf(out)))

    def ts(self, eng, out, a, s1, s2, op0, op1, R, W, accum=None):
        c = self._ecost(eng, self.nf(out))
        if accum is not None:
            self.kb.op(eng, lambda e: e.tensor_scalar(out, a, s1, None, op0, op1, accum_out=accum), R, W, fence=True, cost=c + 150.0)
        elif op1 is None:
            self.kb.op(eng, lambda e: e.tensor_scalar(out, a, s1, None, op0), R, W, cost=c)
        else:
            self.kb.op(eng, lambda e: e.tensor_scalar(out, a, s1, s2, op0, op1), R, W, cost=c)

    def stt(self, out, in0, scalar, in1, op0, op1, R, W):
        self.kb.op("dve", lambda e: e.scalar_tensor_tensor(out, in0, scalar, in1, op0, op1), R, W, cost=70.0 + 2.0 * self.nf(out))

    def cp(self, eng, out, in_, R, W):
        c = self._ecost(eng, self.nf(out))
        if eng == "act":
            self.kb.op("act", lambda e: e.copy(out, in_), R, W, cost=c)
        else:
            self.kb.op(eng, lambda e: e.tensor_copy(out, in_), R, W, cost=c)

    def memset(self, eng, out, val, W):
        self.kb.op(eng, lambda e: e.memset(out, val), (), W, cost=self._ecost(eng, self.nf(out)))

    def recip(self, out, in_, R, W):
        self.kb.op("dve", lambda e: e.reciprocal(out, in_), R, W, cost=70.0 + 3.0 * self.nf(out))

    def red(self, out, in_, R, W):
        self.kb.op("dve", lambda e: e.tensor_reduce(out, in_, AX.X, ALU.add), R, W, cost=70.0 + 1.1 * self.nf(in_))

    @staticmethod
    def _nbytes(ap):
        n = 1
        for d in list(ap.shape):
            n *= d
        return n * 4

    def ld(self, out, in_, R, W, q="sp", slow=False):
        nb = self._nbytes(out)
        if slow:
            self.kb.dma(q, lambda e: e.dma_start(out=out, in_=in_, allow_slow_non_contiguous=True), R, W, nbytes=nb)
        else:
            self.kb.dma(q, lambda e: e.dma_start(out=out, in_=in_), R, W, nbytes=nb)

    def gather(self, out, src, idx, R, W, elem_off=0):
        self.kb.dma("pool", lambda e: e.indirect_dma_start(
            out=out, out_offset=None, in_=src,
            in_offset=bass.IndirectOffsetOnAxis(ap=idx, axis=0), element_offset=elem_off), R, W, nbytes=self._nbytes(out))

    def scatter_add(self, dst, src, idx, R, W):
        self.kb.dma("pool", lambda e: e.indirect_dma_start(
            out=dst, out_offset=bass.IndirectOffsetOnAxis(ap=idx, axis=0),
            in_=src, in_offset=None, compute_op=ALU.add), R, W, nbytes=2 * self._nbytes(src))


def _win_perm():
    names = ['cb', 'cc', 'cx', 'rq', 'rk', 'rv', 'rgf', 'rgb', 'aq', 'ak', 'av']
    sizes = [256] * 3 + [256] * 5 + [512, 128, 128]
    off = {}
    o = 0
    for n, s in zip(names, sizes):
        off[n] = o
        o += s

    def sw(nh):
        idx = []
        for h in range(nh):
            for i in range(64):
                b4, j = i // 16, i % 16
                idx.append(h * 64 + (b4 ^ 1) * 16 + j)
        return np.array(idx)

    cols = []
    for n in ('cb', 'cc', 'cx'):
        cols += list(off[n] + np.arange(256))
    cols += list(off['rq'] + np.arange(256)) + list(off['rq'] + sw(4))
    cols += list(off['rk'] + np.arange(256)) + list(off['rk'] + sw(4))
    cols += list(off['aq'] + np.arange(512)) + list(off['aq'] + sw(8))
    cols += list(off['ak'] + np.arange(128)) + list(off['ak'] + sw(2))
    cols += list(off['rv'] + np.arange(256)) + list(off['rgf'] + np.arange(256))
    cols += list(off['rgb'] + np.arange(256)) + list(off['av'] + np.arange(128))
    cols = np.array(cols)
    assert cols.shape[0] == NW
    return cols


def _rope_tables():
    t = np.arange(T)
    row = (t // 64).astype(np.float32)
    col = (t % 64).astype(np.float32)
    inv = (np.float32(10000.0) ** (-np.arange(0, 32, 2, dtype=np.float32) / np.float32(32))).astype(np.float32)
    ar = (row[:, None] * inv[None, :]).astype(np.float32)
    ac = (col[:, None] * inv[None, :]).astype(np.float32)
    cr, sr, cc_, sc_ = np.cos(ar).T, np.sin(ar).T, np.cos(ac).T, np.sin(ac).T
    cos64 = np.concatenate([cr, cr, cc_, cc_], 0)
    sin64 = np.concatenate([-sr, sr, -sc_, sc_], 0)
    cos = np.ones((128, TT), np.float32)
    sin = np.zeros((128, TT), np.float32)
    cos[:, :T] = np.concatenate([cos64, cos64], 0)
    sin[:, :T] = np.concatenate([sin64, sin64], 0)
    return cos, sin


def build(n_layers=DEPTH, debug=False, stop_after=None, wl=DEPTH, ne_w=NE):
    nc = bass.Bass("TRN2", target_bir_lowering=False)
    es = ExitStack()
    kb = KB(nc, es)
    skind = "ExternalOutput" if debug else "Internal"

    def din(name, shape, dt=F32):
        return kb.dram(name, shape, dt, kind="ExternalInput")

    x_in = din("x", [T, D]); ctx_in = din("ctx", [C, D]); cvec = din("cvec", [2, D])
    w_mod = din("w_mod", [wl, D, 6 * D]); bm_t = din("bm_t", [128, DEPTH, 48]); b_mod = din("b_mod", [DEPTH, 6 * D])
    g1t_d = din("g1t", [128, DEPTH, 8]); g2t_d = din("g2t", [128, DEPTH, 8])
    w_in = din("w_in", [wl, D, NW]); cw_d = din("cw_t", [128, DEPTH * 6])
    rdl_d = din("rdl", [1, 32]); sink_d = din("sink", [1, 32])
    w_out = din("w_out", [wl, D, D]); w_router = din("w_router", [wl, D, NE])
    w_gate = din("w_gate", [wl, ne_w, D, D]); w_up = din("w_up", [wl, ne_w, D, D]); w_down = din("w_down", [wl, ne_w, D, D])
    fg_d = din("final_g", [1, D]); cos_d = din("rope_cos", [128, TT]); sin_d = din("rope_sin", [128, TT])
    out_d = kb.dram("out", [T, D], F32, kind="ExternalOutput")

    xres = kb.dram("xres", [TT, D], F32, kind=skind)
    Fd = kb.dram("Fd", [18 * 64, TT], BF16, kind=skind)
    TMd = kb.dram("TMd", [TT, 1152], BF16, kind=skind)
    Zd = kb.dram("Zd", [2, 128, ZW], F32, kind=skind)
    CBd = kb.dram("CBd", [2, 128, TT], BF16, kind=skind)
    SPd = kb.dram("SPd", [2, 34, 64, 256], BF16, kind=skind)
    XN2d = kb.dram("XN2d", [TT, D], BF16, kind=skind)
    EXd = kb.dram("EXd", [NE, TT], F32, kind=skind)
    POSd = kb.dram("POSd", [32, T], F32, kind=skind)
    MROWd = kb.dram("MROWd", [2, 2, D], F32, kind=skind)
    DBGd = kb.dram("DBGd", [128, 256], F32, kind=skind)

    identb = kb.sb("identb", [128, 128], BF16); identf = kb.sb("identf", [128, 128])
    Dm = kb.sb("Dm", [128, 128]); Dpos = kb.sb("Dpos", [128, 128]); Dneg = kb.sb("Dneg", [128, 128])
    Mge8 = kb.sb("Mge8", [128, 128]); Mle8 = kb.sb("Mle8", [128, 128])
    Mprev = kb.sb("Mprev", [128, 4, 128], BF16); Mnext = kb.sb("Mnext", [128, 4, 128], BF16)
    ones_bf = kb.sb("ones_bf", [128, 64], BF16); ones16 = kb.sb("ones16", [16, 16])
    pcol = kb.sb("pcol", [128, 1]); p127 = kb.sb("p127", [128, 1]); jval = kb.sb("jval", [128, 4]); jb = kb.sb("jb", [128, 4])
    ip1 = kb.sb("ip1", [64, 128]); rev = kb.sb("rev", [64, 128])
    scv = kb.sb("scv", [128, 8, 2]); bm_f = kb.sb("bm_f", [128, DEPTH, 48])
    g1t = kb.sb("g1t_s", [128, DEPTH, 8]); g2t = kb.sb("g2t_s", [128, DEPTH, 8]); cw = kb.sb("cw", [128, DEPTH * 6])
    rdl_b = kb.sb("rdl_b", [128, 32]); sink_b = kb.sb("sink_b", [128, 32])
    AB = kb.sb("AB", [128, 4, 8, 2])
    lg = kb.sb("lg", [128, 8]); wkc = kb.sb("wkc", [128, 8]); dcol = kb.sb("dcol", [128, 8]); se = kb.sb("se", [128, 8])
    DMf = kb.sb("DMf", [128, 4, 128]); DMb = kb.sb("DMb", [128, 4, 128])
    QD = kb.sb("QD", [64, 2, 4, 128], BF16); WK = kb.sb("WK", [128, 2, 256]); DEC = kb.sb("DEC", [64, 2, 256])
    SINKE = kb.sb("SINKE", [64, 2, 512]); Wr = kb.sb("Wr", [128, 8, NE])
    KC = kb.sb("KC", [64, 2, 256], BF16); VC = kb.sb("VC", [128, 2, 128], BF16)
    Sf = kb.sb("Sf", [64, 256]); Sb = kb.sb("Sb", [64, 256])
    LISTF = kb.sb("LISTF", [128, 64]); LISTI = kb.sb("LISTI", [128, 64], I32)
    LISTCF = kb.sb("LISTCF", [32, 16]); LISTCI = kb.sb("LISTCI", [32, 16], I32)
    SACC = kb.sb("SACC", [128, 64])
    tmp128 = kb.sb("tmp128", [128, 128])
    WBIG = kb.sb("WBIG", [128, 32768], BF16, nparts=4)
    PB = [kb.ps(f"PB{i}", [128, 512]) for i in range(8)]
    PBF = [BV(PB[i], PB[i][:, :].bitcast(BF16)) for i in range(8)]
    PT = [PBF[6], PBF[7]]

    g = G(kb, 28672)

    def v3(ap, a):
        return ap.rearrange("p (a b) -> p a b", a=a)

    def setup():
        kb.op("pool", lambda e: e.iota(Dm[:, :], [[1, 128]], base=0, channel_multiplier=-1,
                                       allow_small_or_imprecise_dtypes=True), (), [Dm])
        g.ts("dve", identf[:, :], Dm[:, :], 0.0, None, ALU.is_equal, None, [Dm], [identf])
        g.cp("dve", identb[:, :], identf[:, :], [identf], [identb])
        g.ts("dve", Dpos[:, :], Dm[:, :], 0.0, None, ALU.max, None, [Dm], [Dpos])
        g.ts("dve", Dneg[:, :], Dm[:, :], -1.0, 0.0, ALU.mult, ALU.max, [Dm], [Dneg])
        g.ts("dve", Mge8[:, :], Dm[:, :], 0.0, 0.125, ALU.is_ge, ALU.mult, [Dm], [Mge8])
        g.ts("dve", Mle8[:, :], Dm[:, :], 0.0, 0.125, ALU.is_le, ALU.mult, [Dm], [Mle8])
        for gg in range(4):
            g.ts("dve", Mprev[:, gg, :], Dm[:, :], 0.0, None, ALU.is_le, None, [Dm], [Mprev])
            g.ts("dve", Mnext[:, gg, :], Dm[:, :], 0.0, None, ALU.is_ge, None, [Dm], [Mnext])
        g.memset("dve", ones_bf[:, :], 1.0, [ones_bf])
        g.memset("dve", ones16[:, :], 1.0, [ones16])
        kb.op("pool", lambda e: e.iota(pcol[:, :], [[0, 1]], base=0, channel_multiplier=1,
                                       allow_small_or_imprecise_dtypes=True), (), [pcol])
        g.ts("dve", p127[:, :], pcol[:, :], -1.0, 127.0, ALU.mult, ALU.add, [pcol], [p127])
        kb.op("pool", lambda e: e.iota(jval[:, :], [[128, 4]], base=0, channel_multiplier=1,
                                       allow_small_or_imprecise_dtypes=True), (), [jval])
        g.ts("dve", jb[:, :], jval[:, :], 0.5, None, ALU.add, None, [jval], [jb])
        kb.op("pool", lambda e: e.iota(ip1[:, :], [[1, 128]], base=1, channel_multiplier=0,
                                       allow_small_or_imprecise_dtypes=True), (), [ip1])
        kb.op("pool", lambda e: e.iota(rev[:, :], [[-1, 128]], base=128, channel_multiplier=0,
                                       allow_small_or_imprecise_dtypes=True), (), [rev])
        g.memset("dve", LISTF[:, :], 0.0, [LISTF])
        g.memset("dve", LISTCF[:, :], 0.0, [LISTCF])
        g.ld(xres[0:T, :], x_in[:, :], [x_in], [xres])
        g.ld(xres[T:TT, :], ctx_in[:, :], [ctx_in], [xres])
        g.memset("dve", tmp128[:, :], 0.0, [tmp128])
        for c0 in (0, T + 1, T + 2, T + C + 3):
            g.ld(Zd.t.ap()[:, :, c0:c0 + 1].rearrange("c p o -> p c o"), v3(tmp128[:, 0:2], 2), [tmp128], [Zd], slow=True)
        for s_ in range(2):
            g.ld(scv[:, :, s_:s_ + 1], cvec.t.ap()[s_:s_ + 1, :].rearrange("s (kc p) -> p kc s", p=128), [cvec], [scv], slow=True)
        g.act(scv[:, :, :], scv[:, :, :], AF.Silu, [scv], [scv])
        g.ld(bm_f[:, :, :], bm_t[:, :, :], [bm_t], [bm_f])
        g.ld(g1t[:, :, :], g1t_d[:, :, :], [g1t_d], [g1t])
        g.ld(g2t[:, :, :], g2t_d[:, :, :], [g2t_d], [g2t])
        g.ld(cw[:, :], cw_d[:, :], [cw_d], [cw])
        g.ld(rdl_b[:, :], rdl_d[0:1, :].partition_broadcast(128), [rdl_d], [rdl_b])
        g.ld(sink_b[:, :], sink_d[0:1, :].partition_broadcast(128), [sink_d], [sink_b])

    def phase_mod(l):
        g.reset()
        modf_ps = v3(PB[0][:, 0:64], 32)
        rows_ps = PB[1]
        rows_sb = g.carve([2, 2048], name="rows")
        bmr = g.carve([2, 2048], name="bmr")
        modf = g.carve([128, 32, 2], name="modf")
        tmpm = g.carve([128, 8, 2], name="tmpm")
        WM = [g.carve([128, 8, 256], name="WM") for _ in range(2)]
        g.ld(bmr[0:2, 0:1024], b_mod[l:l + 1, 2 * D:3 * D].partition_broadcast(2), [b_mod], [bmr])
        g.ld(bmr[0:2, 1024:2048], b_mod[l:l + 1, 5 * D:6 * D].partition_broadcast(2), [b_mod], [bmr])
        wsrc = w_mod.t.ap()[l].rearrange("(kc p) n -> p kc n", p=128)
        for j in range(24):
            grp = j // 4
            wm = WM[j % 2]
            g.ld(wm[:, :, :], wsrc[:, :, j * 256:(j + 1) * 256], [w_mod], [wm])
            if grp in (0, 1, 3, 4):
                gi = {0: 0, 1: 1, 3: 2, 4: 3}[grp]
                for nb in range(2):
                    fm = gi * 8 + (j % 4) * 2 + nb
                    for kc in range(8):
                        g.mm(modf_ps[:, fm, :], wm[:, kc, nb * 128:(nb + 1) * 128], scv[:, kc, :], kc == 0, kc == 7,
                             [wm, scv], [PB[0]])
            else:
                ri = 0 if grp == 2 else 1
                col = ri * 1024 + (j % 4) * 256
                for kc in range(8):
                    g.mm(rows_ps[0:2, 0:256], scv[:, kc, :], wm[:, kc, :], kc == 0, kc == 7, [wm, scv], [PB[1]])
                g.tt("dve", rows_sb[0:2, col:col + 256], rows_ps[0:2, 0:256], bmr[0:2, col:col + 256], ALU.add,
                     [PB[1], bmr], [rows_sb])
        for gi, Gi in enumerate((0, 1, 3, 4)):
            g.tt("dve", modf[:, gi * 8:(gi + 1) * 8, :], modf_ps[:, gi * 8:(gi + 1) * 8, :],
                 bm_f[:, l, Gi * 8:(Gi + 1) * 8].unsqueeze(2).to_broadcast([128, 8, 2]), ALU.add, [PB[0], bm_f], [modf])
        g.ts("dve", tmpm[:, :, :], modf[:, 8:16, :], 1.0, None, ALU.add, None, [modf], [tmpm])
        g.tt("dve", AB[:, 0, :, :], tmpm[:, :, :], g1t[:, l, :].unsqueeze(2).to_broadcast([128, 8, 2]), ALU.mult,
             [tmpm, g1t], [AB])
        g.cp("dve", AB[:, 1, :, :], modf[:, 0:8, :], [modf], [AB])
        g.ts("dve", tmpm[:, :, :], modf[:, 24:32, :], 1.0, None, ALU.add, None, [modf], [tmpm])
        g.tt("dve", AB[:, 2, :, :], tmpm[:, :, :], g2t[:, l, :].unsqueeze(2).to_broadcast([128, 8, 2]), ALU.mult,
             [tmpm, g2t], [AB])
        g.cp("dve", AB[:, 3, :, :], modf[:, 16:24, :], [modf], [AB])
        g.ld(MROWd.t.ap()[0], rows_sb[0:2, 0:1024], [rows_sb], [MROWd])
        g.ld(MROWd.t.ap()[1], rows_sb[0:2, 1024:2048], [rows_sb], [MROWd])
        g.act(lg[:, :], rdl_b[:, l * 8:(l + 1) * 8], AF.Exp, [rdl_b], [lg], scale=-1.0)
        g.ts("dve", lg[:, :], lg[:, :], 1.0, None, ALU.add, None, [lg], [lg])
        g.act(lg[:, :], lg[:, :], AF.Ln, [lg], [lg])
        g.ts("dve", lg[:, :], lg[:, :], -1.0, None, ALU.mult, None, [lg], [lg])
        for d in range(2):
            for h in range(4):
                dh = d * 4 + h
                g.act(tmp128[:, :], (Dpos if d == 0 else Dneg)[:, :], AF.Exp, [Dpos, Dneg, lg], [tmp128], scale=lg[:, dh:dh + 1])
                g.tt("dve", (DMf if d == 0 else DMb)[:, h, :], tmp128[:, :], (Mge8 if d == 0 else Mle8)[:, :], ALU.mult,
                     [tmp128, Mge8, Mle8], [DMf if d == 0 else DMb])
                g.act(QD[:, d, h, :], (ip1 if d == 0 else rev)[:, :], AF.Exp, [ip1, rev, lg], [QD], scale=lg[0:64, dh:dh + 1])
                g.act(wkc[:, dh:dh + 1], (p127 if d == 0 else pcol)[:, :], AF.Exp, [p127, pcol, lg], [wkc], scale=lg[:, dh:dh + 1])
        g.ts("dve", wkc[:, :], wkc[:, :], 0.125, None, ALU.mult, None, [wkc], [wkc])
        g.act(dcol[:, :], lg[:, :], AF.Exp, [lg], [dcol], scale=128.0)
        g.act(se[:, :], sink_b[:, l * 8:(l + 1) * 8], AF.Exp, [sink_b], [se])
        for d in range(2):
            g.cp("dve", v3(WK[:, d, :], 4), wkc[:, d * 4:(d + 1) * 4].unsqueeze(2).to_broadcast([128, 4, 64]), [wkc], [WK])
            g.cp("dve", v3(DEC[:, d, :], 4), dcol[0:64, d * 4:(d + 1) * 4].unsqueeze(2).to_broadcast([64, 4, 64]), [dcol], [DEC])
            g.cp("dve", v3(SINKE[:, d, :], 4), se[0:64, d * 4:(d + 1) * 4].unsqueeze(2).to_broadcast([64, 4, 128]), [se], [SINKE])
        g.ld(Wr[:, :, :], w_router.t.ap()[l].rearrange("(kc p) e -> p kc e", p=128), [w_router], [Wr])

    FM_PAIRS = [
        (6, 8, 0, False), (7, 9, 2, False),
        (10, 12, 4, True), (11, 13, 6, True),
        (14, 18, 8, False), (15, 19, 10, False), (16, 20, 12, False), (17, 21, 14, False),
        (22, 23, 16, False),
    ]

    def zcol(tt):
        return tt + 1 if tt < T else tt + 3

    def chunk_of(tt):
        return tt // 128 if tt < T else 32 + (tt - T) // 128

    def tiles(with_ctx=True):
        ts_ = []
        if with_ctx:
            ts_.append((1, T, C))
        for i in range(T // 512):
            ts_.append((0, i * 512, 512))
        return ts_

    def rms_stats(xin, nsub, ss, rstd, junk):
        for a in range(nsub):
            g.act(junk[:, :], xin[:, a, :], AF.Square, [xin], [junk, ss], accum=ss[:, a:a + 1])
        g.ts("dve", rstd[:, 0:nsub], ss[:, 0:nsub], 1.0 / D, EPS, ALU.mult, ALU.add, [ss], [rstd])
        g.act(rstd[:, 0:nsub], rstd[:, 0:nsub], AF.Sqrt, [rstd], [rstd])
        g.recip(rstd[:, 0:nsub], rstd[:, 0:nsub], [rstd], [rstd])

    def phase_proj(l):
        g.reset()
        win = WBIG[:, 0:8 * NW].rearrange("p (k n) -> p k n", k=8)
        for kc in range(8):
            for hh in range(2):
                g.ld(win[:, kc, hh * 1984:(hh + 1) * 1984],
                     w_in.t.ap()[l, kc * 128:(kc + 1) * 128, hh * 1984:(hh + 1) * 1984], [w_in], [WBIG], q="pool")
        xin = g.carve([128, 4, D], name="xin")
        xn = g.carve([128, 4, D], BF16, name="xn")
        hT = g.carve([128, 8, 512], BF16, name="hT")
        cosb = g.carve([128, 512], name="cosb"); sinb = g.carve([128, 512], name="sinb")
        ss = g.carve([128, 4], name="ss"); rstd = g.carve([128, 4], name="rstd")
        junk = g.carve([128, D], name="junk")
        tmst = g.carve([128, 4, 1152], BF16, name="tmst")
        t1 = g.carve([128, 512], name="t1"); t2 = g.carve([128, 512], name="t2")
        fst = [g.carve([128, 512], BF16, name="fst") for _ in range(2)]
        kst = [g.carve([128, 512], BF16, name="kst") for _ in range(2)]
        ccs = g.carve([128, 512], name="ccs")
        zst = g.carve([128, 2, 512], name="zst")
        cbst = g.carve([128, 2, 512], BF16, name="cbst")
        kw = g.carve([128, 256], BF16, name="kw")
        spst = [g.carve([64, 256], BF16, name="spst") for _ in range(2)]
        g.memset("dve", Sf[:, :], 0.0, [Sf])
        fcount = 0
        pbi = 0
        Ffull = Fd.t.ap()
        for (s, t0, n) in tiles(True):
            nsub = n // 128
            g.ld(xin[:, 0:nsub, :], xres[t0:t0 + n, :].rearrange("(a p) d -> p a d", p=128), [xres], [xin])
            g.ld(cosb[:, 0:n], cos_d[:, t0:t0 + n], [cos_d], [cosb])
            g.ld(sinb[:, 0:n], sin_d[:, t0:t0 + n], [sin_d], [sinb])
            rms_stats(xin, nsub, ss, rstd, junk)
            for a in range(nsub):
                g.ts("dve", xn[:, a, :], xin[:, a, :], rstd[:, a:a + 1], None, ALU.mult, None, [xin, rstd], [xn])
            for kc in range(8):
                tp = PT[kc % 2]
                for a in range(nsub):
                    g.tr(tp[:, a * 128:(a + 1) * 128], xn[:, a, kc * 128:(kc + 1) * 128], identb[:, :], [xn, identb], [tp])
                g.act(hT[:, kc, 0:n], tp[:, 0:n], AF.Identity, [tp, AB], [hT],
                      bias=AB[:, 1, kc, s:s + 1], scale=AB[:, 0, kc, s:s + 1])

            def fm_block(fb):
                nonlocal pbi
                pb = PB[pbi % 4]
                pbi += 1
                for kc in range(8):
                    g.mm(pb[:, 0:n], win[:, kc, fb * 128:(fb + 1) * 128], hT[:, kc, 0:n], kc == 0, kc == 7, [WBIG, hT], [pb])
                return pb

            for c in range(2):
                pb = fm_block(c)
                g.cp("act", cbst[:, c, 0:n], pb[:, 0:n], [pb], [cbst])
            for c in range(2):
                pcc = fm_block(2 + c)
                g.cp("act", ccs[:, 0:n], pcc[:, 0:n], [pcc], [ccs])
                pcx = fm_block(4 + c)
                g.tt("dve", zst[:, c, 0:n], pcx[:, 0:n], ccs[:, 0:n], ALU.mult, [pcx, ccs], [zst])
            g.ld(CBd.t.ap()[:, :, t0:t0 + n].rearrange("c p t -> p c t"), cbst[:, :, 0:n], [cbst], [CBd])
            g.ld(Zd.t.ap()[:, :, zcol(t0):zcol(t0) + n].rearrange("c p t -> p c t"), zst[:, :, 0:n], [zst], [Zd])
            for (fb, fbs, hidx, isk) in FM_PAIRS:
                px = fm_block(fb)
                g.tt("dve", t1[:, 0:n], px[:, 0:n], cosb[:, 0:n], ALU.mult, [px, cosb], [t1])
                psw = fm_block(fbs)
                g.tt("dve", t2[:, 0:n], psw[:, 0:n], sinb[:, 0:n], ALU.mult, [psw, sinb], [t2])
                if isk:
                    dst = kst[(hidx - 4) // 2]
                else:
                    dst = fst[fcount % 2]
                    fcount += 1
                g.tt("pool", dst[:, 0:n], t1[:, 0:n], t2[:, 0:n], ALU.add, [t1, t2], [dst])
                g.ld(Ffull[hidx * 64:hidx * 64 + 128, t0:t0 + n], dst[:, 0:n], [dst], [Fd])
            ktp = PT[0]
            for a in range(nsub):
                for hp in range(2):
                    g.tr(ktp[:, a * 256 + hp * 128:a * 256 + (hp + 1) * 128], kst[hp][:, a * 128:(a + 1) * 128], identb[:, :],
                         [kst[hp], identb], [ktp])
            g.cp("act", tmst[:, 0:nsub, 0:256], v3(ktp[:, 0:nsub * 256], nsub), [ktp], [tmst])
            for a in range(nsub):
                pa, pbb = PB[4], PB[5]
                for kc in range(8):
                    g.mm(pa[:, 0:512], hT[:, kc, a * 128:(a + 1) * 128], win[:, kc, 3072:3584], kc == 0, kc == 7, [WBIG, hT], [pa])
                for kc in range(8):
                    g.mm(pbb[:, 0:384], hT[:, kc, a * 128:(a + 1) * 128], win[:, kc, 3584:3968], kc == 0, kc == 7, [WBIG, hT], [pbb])
                g.cp("dve", tmst[:, a, 256:512], pa[:, 0:256], [pa], [tmst])
                g.act(tmst[:, a, 512:768], pa[:, 256:512], AF.Silu, [pa], [tmst])
                g.act(tmst[:, a, 768:1024], pbb[:, 0:256], AF.Silu, [pbb], [tmst])
                g.cp("dve", tmst[:, a, 1024:1152], pbb[:, 256:384], [pbb], [tmst])
            g.ld(TMd[t0:t0 + n, :].rearrange("(a p) c -> p a c", p=128), tmst[:, 0:nsub, :], [tmst], [TMd])
            for a in range(nsub):
                ch = chunk_of(t0 + a * 128)
                g.tt("dve", kw[:, :], tmst[:, a, 0:256], WK[:, 0, :], ALU.mult, [tmst, WK], [kw])
                ups = PB[4]
                for h in range(4):
                    g.mm(ups[0:64, h * 64:(h + 1) * 64], kw[:, h * 64:(h + 1) * 64], tmst[:, a, 256 + h * 64:256 + (h + 1) * 64],
                         True, True, [kw, tmst], [ups])
                sp = spst[ch % 2]
                g.cp("act", sp[:, :], Sf[:, :], [Sf], [sp])
                g.ld(SPd.t.ap()[0, ch], sp[:, :], [sp], [SPd])
                g.tt("pool", Sf[:, :], Sf[:, :], DEC[:, 0, :], ALU.mult, [Sf, DEC], [Sf])
                g.tt("dve", Sf[:, :], Sf[:, :], ups[0:64, 0:256], ALU.add, [Sf, ups], [Sf])

    def phase_bwd(l):
        g.reset()
        kvb = [g.carve([128, 512], BF16, name="kvb") for _ in range(2)]
        kw = g.carve([128, 256], BF16, name="kwb")
        spst = [g.carve([64, 256], BF16, name="spstb") for _ in range(2)]
        g.memset("dve", Sb[:, :], 0.0, [Sb])
        order = [33, 32] + list(range(31, -1, -1))
        for i, ch in enumerate(order):
            r0 = ch * 128 if ch < 32 else T + (ch - 32) * 128
            kv = kvb[i % 2]
            g.ld(kv[:, :], TMd[r0:r0 + 128, 0:512], [TMd], [kv])
            g.tt("dve", kw[:, :], kv[:, 0:256], WK[:, 1, :], ALU.mult, [kv, WK], [kw])
            ups = PB[i % 2]
            for h in range(4):
                g.mm(ups[0:64, h * 64:(h + 1) * 64], kw[:, h * 64:(h + 1) * 64], kv[:, 256 + h * 64:256 + (h + 1) * 64],
                     True, True, [kw, kv], [ups])
            sp = spst[i % 2]
            g.cp("act", sp[:, :], Sb[:, :], [Sb], [sp])
            g.ld(SPd.t.ap()[1, ch], sp[:, :], [sp], [SPd])
            g.tt("pool", Sb[:, :], Sb[:, :], DEC[:, 1, :], ALU.mult, [Sb, DEC], [Sb])
            g.tt("dve", Sb[:, :], Sb[:, :], ups[0:64, 0:256], ALU.add, [Sb, ups], [Sb])

    def mix_tiles(with_ctx):
        ts_ = []
        if with_ctx:
            ts_.append((1, T, 256))
        for i in range(T // 256):
            ts_.append((0, i * 256, 256))
        return ts_

    def phase_mix(l, need_ctx):
        g.reset()
        wo_cr = WBIG[:, 0:4096].rearrange("p (c n) -> p c n", c=4)
        wo_at = WBIG[0:64, 8192:16384].rearrange("p (h n) -> p h n", h=8)
        g.ld(wo_cr, w_out.t.ap()[l, 0:512, :].rearrange("(c p) n -> p c n", p=128), [w_out], [(WBIG, 0)], q="pool")
        g.ld(wo_at, w_out.t.ap()[l, 512:1024, :].rearrange("(h p) n -> p h n", p=64), [w_out], [(WBIG, 1)], q="pool")
        Fv = Fd.t.ap().rearrange("(h p) t -> p h t", p=64)
        g.ld(KC[:, :, :], Fv[:, 16:18, T:TT], [Fd], [KC])
        g.ld(VC[:, :, :], TMd[T:TT, 1024:1152].rearrange("(a p) c -> p a c", p=128), [TMd], [VC])
        NT = 256

        def tset():
            return dict(RQK=g.carve([64, 8, NT], BF16, name="RQK"), AQ=g.carve([64, 8, NT], BF16, name="AQ"),
                        AK=g.carve([64, 2, NT + 256], BF16, name="AK"), TMt=g.carve([128, 2, 768], BF16, name="TMt"),
                        AV=g.carve([128, 4, 128], BF16, name="AV"), Zt=g.carve([128, 2, NT + 2], name="Zt"),
                        CBt=g.carve([128, 2, NT], BF16, name="CBt"), SPt=g.carve([64, 2, 2, 256], BF16, name="SPt"),
                        xin=g.carve([128, 2, D], name="xin2"), mconv=g.carve([128, 2, NT], BF16, name="mconv"),
                        matt=g.carve([64, 8, NT], BF16, name="matt"))
        TB = [tset(), tset()]
        G1r = g.carve([128, D], name="G1r")
        ctmp = g.carve([128, NT], name="ctmp")
        STf = [g.carve([128, 4, 128], BF16, name="STf") for _ in range(2)]
        STb = [g.carve([128, 4, 128], BF16, name="STb") for _ in range(2)]
        qd = [g.carve([64, 2, 4, 128], BF16, name="qd") for _ in range(2)]
        t1 = [g.carve([128, 512], name="t1m") for _ in range(2)]
        sq = g.carve([128, 512], name="sq")
        st8 = [g.carve([128, 6, 8], name="st8") for _ in range(2)]
        ret = [g.carve([128, 256], BF16, name="ret") for _ in range(2)]
        mret = g.carve([128, 2, NT], BF16, name="mret")
        PTs = [g.carve([128, 4, 128], BF16, name="PTs") for _ in range(3)]
        den = [g.carve([64, 512], name="den") for _ in range(2)]
        tmpy = [g.carve([128, 512], name="tmpy") for _ in range(2)]
        junk = g.carve([128, D], BF16, name="junk2")
        ss = g.carve([128, 4], name="ss2"); rstd = g.carve([128, 4], name="rstd2")
        xn2 = g.carve([128, D], name="xn2"); xn2b = g.carve([128, D], BF16, name="xn2b")
        h2T = g.carve([128, 8, 128], name="h2T")
        ex = [g.carve([16, 128], name="ex") for _ in range(2)]
        tl = mix_tiles(need_ctx)
        info = {}

        def load_tile(ti):
            (s, t0, n) = tl[ti]
            B = TB[ti % 2]
            nsub = n // 128
            g.ld(B["RQK"][:, :, 0:n], Fv[:, 0:8, t0:t0 + n], [Fd], [B["RQK"]])
            g.ld(B["AQ"][:, :, 0:n], Fv[:, 8:16, t0:t0 + n], [Fd], [B["AQ"]])
            koff = 0
            if s == 0:
                lo = max(t0 - 128, 0); hi = min(t0 + n + 128, T)
                koff = t0 - lo
                g.ld(B["AK"][:, :, 0:hi - lo], Fv[:, 16:18, lo:hi], [Fd], [B["AK"]])
                g.ld(B["AV"][:, 0:(hi - lo) // 128, :], TMd[lo:hi, 1024:1152].rearrange("(a p) c -> p a c", p=128), [TMd], [B["AV"]])
            g.ld(B["TMt"][:, 0:nsub, :], TMd[t0:t0 + n, 256:1024].rearrange("(a p) c -> p a c", p=128), [TMd], [B["TMt"]])
            zc = zcol(t0)
            g.ld(B["Zt"][:, :, 0:n + 2], Zd.t.ap()[:, :, zc - 1:zc + n + 1].rearrange("c p t -> p c t"), [Zd], [B["Zt"]])
            g.ld(B["CBt"][:, :, 0:n], CBd.t.ap()[:, :, t0:t0 + n].rearrange("c p t -> p c t"), [CBd], [B["CBt"]])
            g.ld(B["xin"][:, 0:nsub, :], xres[t0:t0 + n, :].rearrange("(a p) d -> p a d", p=128), [xres], [B["xin"]])
            ch0 = chunk_of(t0)
            for d_ in range(2):
                g.ld(B["SPt"][:, d_, 0:nsub, :], SPd.t.ap()[d_, ch0:ch0 + nsub].rearrange("a p f -> p a f"), [SPd], [B["SPt"]])
            info[ti] = koff

        load_tile(0)
        cur_s = None
        cnt = 0
        for ti, (s, t0, n) in enumerate(tl):
            if ti + 1 < len(tl):
                load_tile(ti + 1)
            if s != cur_s:
                g.ld(G1r[:, :], MROWd.t.ap()[0, s:s + 1, :].partition_broadcast(128), [MROWd], [G1r])
                cur_s = s
            B = TB[ti % 2]
            RQK, AQ, AK, TMt, AV, Zt, CBt, SPt, xin, mconv, matt = (B[k] for k in ("RQK", "AQ", "AK", "TMt", "AV", "Zt", "CBt", "SPt", "xin", "mconv", "matt"))
            koff = info[ti]
            nsub = n // 128
            lat = s == 0
            for c in range(2):
                wb = l * 6
                g.ts("dve", ctmp[:, 0:n], Zt[:, c, 0:n], cw[:, wb + 0 * 2 + c:wb + 0 * 2 + c + 1], None, ALU.mult, None, [Zt, cw], [ctmp])
                g.stt(ctmp[:, 0:n], Zt[:, c, 1:n + 1], cw[:, wb + 1 * 2 + c:wb + 1 * 2 + c + 1], ctmp[:, 0:n], ALU.mult, ALU.add, [Zt, cw, ctmp], [ctmp])
                g.stt(ctmp[:, 0:n], Zt[:, c, 2:n + 2], cw[:, wb + 2 * 2 + c:wb + 2 * 2 + c + 1], ctmp[:, 0:n], ALU.mult, ALU.add, [Zt, cw, ctmp], [ctmp])
                g.tt("dve", mconv[:, c, 0:n], ctmp[:, 0:n], CBt[:, c, 0:n], ALU.mult, [ctmp, CBt], [mconv])
            for a in range(nsub):
                sl = slice(a * 128, (a + 1) * 128)
                stp = PB[a]
                for h in range(4):
                    g.mm(stp[:, h * 128:(h + 1) * 128], RQK[:, 4 + h, sl], RQK[:, h, sl], True, True, [RQK], [stp])
                g.tt("dve", STf[a][:, :, :], v3(stp[:, :], 4), DMf[:, :, :], ALU.mult, [stp, DMf], [STf[a]])
                g.tt("dve", STb[a][:, :, :], v3(stp[:, :], 4), DMb[:, :, :], ALU.mult, [stp, DMb], [STb[a]])
                for d in range(2):
                    g.tt("pool", qd[a][:, d, :, :], RQK[:, 0:4, sl], QD[:, d, :, :], ALU.mult, [RQK, QD], [qd[a]])
                op_ = PB[2 + a]
                for d in range(2):
                    STd = STf[a] if d == 0 else STb[a]
                    for h in range(4):
                        o_ap = op_[:, d * 256 + h * 64:d * 256 + (h + 1) * 64]
                        g.mm(o_ap, STd[:, h, :], TMt[:, a, h * 64:(h + 1) * 64], True, False, [STd, TMt], [op_])
                        g.mm(o_ap, qd[a][:, d, h, :], SPt[:, d, a, h * 64:(h + 1) * 64], False, True, [qd[a], SPt], [op_])
                o3 = v3(op_[:, :], 8)
                s8 = st8[a]
                g.red(s8[:, 0, :], o3, [op_], [s8])
                g.act(sq[:, :], op_[:, :], AF.Square, [op_], [sq])
                g.red(s8[:, 1, :], v3(sq[:, :], 8), [sq], [s8])
                g.ts("dve", s8[:, 2, :], s8[:, 0, :], 1.0 / 64, None, ALU.mult, None, [s8], [s8])
                g.tt("dve", s8[:, 5, :], s8[:, 2, :], s8[:, 2, :], ALU.mult, [s8], [s8])
                g.stt(s8[:, 3, :], s8[:, 1, :], 1.0 / 64, s8[:, 5, :], ALU.mult, ALU.subtract, [s8], [s8])
                g.ts("dve", s8[:, 3, :], s8[:, 3, :], EPS, None, ALU.add, None, [s8], [s8])
                g.act(s8[:, 3, :], s8[:, 3, :], AF.Sqrt, [s8], [s8])
                g.recip(s8[:, 3, :], s8[:, 3, :], [s8], [s8])
                g.stt(s8[:, 4, :], s8[:, 2, :], -1.0, s8[:, 3, :], ALU.mult, ALU.mult, [s8], [s8])
                t13 = v3(t1[a][:, :], 8)
                g.tt("dve", t13, o3, s8[:, 3, :].unsqueeze(2).to_broadcast([128, 8, 64]), ALU.mult, [op_, s8], [t1[a]])
                g.tt("pool", t13, t13, s8[:, 4, :].unsqueeze(2).to_broadcast([128, 8, 64]), ALU.add, [t1[a], s8], [t1[a]])
                g.tt("pool", t1[a][:, :], t1[a][:, :], TMt[:, a, 256:768], ALU.mult, [t1[a], TMt], [t1[a]])
                g.tt("pool", ret[a][:, :], t1[a][:, 0:256], t1[a][:, 256:512], ALU.add, [t1[a]], [ret[a]])
                rtp = PBF[4 + a]
                for c in range(2):
                    g.tr(rtp[:, c * 128:(c + 1) * 128], ret[a][:, c * 128:(c + 1) * 128], identb[:, :], [ret[a], identb], [rtp])
                g.cp("act", mret[:, :, sl], v3(rtp[:, 0:256], 2), [rtp], [mret])
            for a in range(nsub):
                sl = slice(a * 128, (a + 1) * 128)
                qb = t0 // 128 + a
                for kvh in range(2):
                    kbl = []
                    if lat:
                        ka = koff // 128 + a
                        if qb > 0:
                            kbl.append(("prev", AK[:, kvh, (ka - 1) * 128:ka * 128], AV[:, ka - 1, kvh * 64:(kvh + 1) * 64], [AK], [AV]))
                        kbl.append(("same", AK[:, kvh, ka * 128:(ka + 1) * 128], AV[:, ka, kvh * 64:(kvh + 1) * 64], [AK], [AV]))
                        if qb < T // 128 - 1:
                            kbl.append(("next", AK[:, kvh, (ka + 1) * 128:(ka + 2) * 128], AV[:, ka + 1, kvh * 64:(kvh + 1) * 64], [AK], [AV]))
                    for cb_ in range(2):
                        kbl.append(("ctx", KC[:, kvh, cb_ * 128:(cb_ + 1) * 128], VC[:, cb_, kvh * 64:(kvh + 1) * 64], [KC], [VC]))
                    jj = (a * 2 + kvh) % 2
                    aps, bps = PB[3 + 2 * jj], PB[4 + 2 * jj]
                    for i, (kind, kT, vv, kR, vR) in enumerate(kbl):
                        sp_ = PB[cnt % 3]
                        pts = PTs[cnt % 3]
                        cnt += 1
                        g.mm(v3(sp_[:, :], 4), kT, AQ[:, kvh * 4:(kvh + 1) * 4, sl], True, True, kR + [AQ], [sp_])
                        g.act(pts[:, :, :], v3(sp_[:, :], 4), AF.Exp, [sp_], [pts], scale=0.125)
                        if kind == "prev":
                            g.tt("pool", pts[:, :, :], pts[:, :, :], Mprev[:, :, :], ALU.mult, [pts, Mprev], [pts])
                        elif kind == "next":
                            g.tt("pool", pts[:, :, :], pts[:, :, :], Mnext[:, :, :], ALU.mult, [pts, Mnext], [pts])
                        g.mm(v3(aps[0:64, :], 4), vv, pts[:, :, :], i == 0, i == len(kbl) - 1, vR + [pts], [aps])
                        g.mm(v3(bps[0:64, :], 4), ones_bf[:, :], pts[:, :, :], i == 0, i == len(kbl) - 1, [ones_bf, pts], [bps])
                    dn = den[jj]
                    g.tt("dve", dn[:, :], bps[0:64, :], SINKE[:, kvh, :], ALU.add, [bps, SINKE], [dn])
                    g.recip(dn[:, :], dn[:, :], [dn], [dn])
                    g.tt("dve", matt[:, kvh * 4:(kvh + 1) * 4, sl], v3(aps[0:64, :], 4), v3(dn[:, :], 4), ALU.mult, [aps, dn], [matt])
            for a in range(nsub):
                sl = slice(a * 128, (a + 1) * 128)
                for nh in range(2):
                    yp = PB[a * 2 + nh]
                    ns = slice(nh * 512, (nh + 1) * 512)
                    for c in range(2):
                        g.mm(yp[:, :], mconv[:, c, sl], wo_cr[:, c, ns], c == 0, False, [mconv, (WBIG, 0)], [yp])
                    for c in range(2):
                        g.mm(yp[:, :], mret[:, c, sl], wo_cr[:, 2 + c, ns], False, False, [mret, (WBIG, 0)], [yp])
                    for h in range(8):
                        g.mm(yp[:, :], matt[:, h, sl], wo_at[:, h, ns], False, h == 7, [matt, (WBIG, 1)], [yp])
                    ty = tmpy[nh]
                    g.tt("dve", ty[:, :], yp[:, :], G1r[:, ns], ALU.mult, [yp, G1r], [ty])
                    g.tt("pool", xin[:, a, ns], xin[:, a, ns], ty[:, :], ALU.add, [xin, ty], [xin])
                g.ld(xres[t0 + a * 128:t0 + (a + 1) * 128, :], xin[:, a, :], [xin], [xres])
            for a in range(nsub):
                g.act(junk[:, :], xin[:, a, :], AF.Square, [xin], [junk, ss], accum=ss[:, a:a + 1])
                g.ts("dve", rstd[:, a:a + 1], ss[:, a:a + 1], 1.0 / D, EPS, ALU.mult, ALU.add, [ss], [rstd])
                g.act(rstd[:, a:a + 1], rstd[:, a:a + 1], AF.Sqrt, [rstd], [rstd])
                g.recip(rstd[:, a:a + 1], rstd[:, a:a + 1], [rstd], [rstd])
                g.ts("dve", xn2[:, :], xin[:, a, :], rstd[:, a:a + 1], None, ALU.mult, None, [xin, rstd], [xn2])
                g.cp("pool", xn2b[:, :], xn2[:, :], [xn2], [xn2b])
                g.ld(XN2d[t0 + a * 128:t0 + (a + 1) * 128, :], xn2b[:, :], [xn2b], [XN2d])
                for kc in range(8):
                    tps = PB[4 + 2 * a + (kc // 4)]
                    g.tr(tps[:, (kc % 4) * 128:(kc % 4 + 1) * 128], xn2[:, kc * 128:(kc + 1) * 128], identf[:, :], [xn2, identf], [tps])
                for kc in range(8):
                    tps = PB[4 + 2 * a + (kc // 4)]
                    g.act(h2T[:, kc, :], tps[:, (kc % 4) * 128:(kc % 4 + 1) * 128], AF.Identity, [tps, AB], [h2T],
                          bias=AB[:, 3, kc, s:s + 1], scale=AB[:, 2, kc, s:s + 1])
                lps = PB[a]
                for kc in range(8):
                    g.mm(lps[0:16, 0:128], Wr[:, kc, :], h2T[:, kc, :], kc == 0, kc == 7, [Wr, h2T], [lps])
                exb = ex[a % 2]
                g.act(exb[:, :], lps[0:16, 0:128], AF.Exp, [lps], [exb])
                g.ld(EXd[:, t0 + a * 128:t0 + (a + 1) * 128], exb[:, :], [exb], [EXd])

    def phase_route(l, need_ctx):
        g.reset()
        EXT = g.carve([16, TT], name="EXT")
        rden = g.carve([16, 512], name="rden")
        COMB = g.carve([32, T], name="COMB")
        PBc = [g.carve([128, T], name="PBc") for _ in range(2)]
        jA = g.carve([128, T], BF16, name="jA")
        jB = g.carve([128, T], BF16, name="jB")
        sc = g.carve([32, 8], name="scr")
        lo, hi, mid, cnt, pp, dd, kvec = (sc[:, i:i + 1] for i in range(7))
        g.ld(EXT[:, :], EXd[:, :], [EXd], [EXT])
        for c0 in range(0, TT, 512):
            w = min(512, TT - c0)
            dps = PB[(c0 // 512) % 2]
            g.mm(dps[0:16, 0:w], ones16[:, :], EXT[:, c0:c0 + w], True, True, [ones16, EXT], [dps])
            g.recip(rden[:, 0:w], dps[0:16, 0:w], [dps], [rden])
            g.tt("dve", EXT[:, c0:c0 + w], EXT[:, c0:c0 + w], rden[:, 0:w], ALU.mult, [EXT, rden], [EXT])
        g.ld(EXd[:, :], EXT[:, :], [EXT], [EXd])
        g.memset("dve", COMB[:, :], 0.0, [COMB])
        g.ld(COMB[0:16, :], EXd[:, 0:T], [EXd], [COMB])
        if need_ctx:
            g.ld(COMB[16:32, 0:C], EXd[:, T:TT], [EXd], [COMB])
        g.memset("dve", sc[:, :], 0.0, [sc])
        g.memset("dve", hi, 1.0, [sc])
        g.memset("dve", kvec, float(CAPC), [sc])
        g.memset("dve", sc[0:16, 6:7], float(CAP), [sc])
        for it in range(36):
            g.tt("dve", mid, lo, hi, ALU.add, [sc], [sc])
            g.ts("dve", mid, mid, 0.5, None, ALU.mult, None, [sc], [sc])
            g.ts("dve", jA[0:32, :], COMB[:, :], mid, 0.0, ALU.is_gt, ALU.add, [COMB, sc], [jA, sc], accum=cnt)
            g.tt("dve", pp, cnt, kvec, ALU.is_gt, [sc], [sc])
            g.tt("dve", dd, mid, lo, ALU.subtract, [sc], [sc])
            g.stt(lo, dd, pp, lo, ALU.mult, ALU.add, [sc], [sc])
            g.tt("dve", dd, hi, mid, ALU.subtract, [sc], [sc])
            g.stt(hi, dd, pp, mid, ALU.mult, ALU.add, [sc], [sc])
        g.ts("dve", jA[0:32, :], COMB[:, :], hi, None, ALU.is_gt, None, [COMB, sc], [jA])
        zr = PBc[1]
        g.memset("dve", zr[0:32, :], 0.0, [zr])
        kb.op("dve", lambda e: e.tensor_tensor_scan(COMB[:, :], jA[0:32, :], zr[0:32, :], 0.0, ALU.add, ALU.add), [jA, zr], [COMB], cost=9000.0)
        g.ld(POSd[:, :], COMB[:, :], [COMB], [POSd])
        for e in range(NE):
            pb = PBc[e % 2]
            g.ld(pb[:, :], POSd[e:e + 1, :].partition_broadcast(128), [POSd], [pb])
            for jt in range(4):
                col = e * 4 + jt
                if jt < 2:
                    g.ts("dve", jA[:, :], pb[:, :], jval[:, jt:jt + 1], 0.0, ALU.is_le, ALU.add,
                         [pb, jval], [jA, LISTF], accum=LISTF[:, col:col + 1])
                else:
                    g.act(jB[:, :], pb[:, :], AF.Sign, [pb, jb], [jB, SACC], bias=jb[:, jt:jt + 1], scale=-1.0,
                          accum=SACC[:, col:col + 1])
        for e in range(NE):
            g.ts("dve", LISTF[:, e * 4 + 2:e * 4 + 4], SACC[:, e * 4 + 2:e * 4 + 4], float(T), 0.5, ALU.add, ALU.mult, [SACC], [LISTF])
        g.cp("dve", LISTI[:, :], LISTF[:, :], [LISTF], [LISTI])
        if need_ctx:
            for e in range(NE):
                pb = PBc[e % 2]
                g.ld(pb[0:32, 0:C], POSd[16 + e:17 + e, 0:C].partition_broadcast(32), [POSd], [pb])
                g.ts("dve", jA[0:32, 0:C], pb[0:32, 0:C], jval[0:32, 0:1], 0.0, ALU.is_le, ALU.add,
                     [pb, jval], [jA, LISTCF], accum=LISTCF[:, e:e + 1])
            g.ts("dve", LISTCF[:, :], LISTCF[:, :], float(T), None, ALU.add, None, [LISTCF], [LISTCF])
            g.cp("dve", LISTCI[:, :], LISTCF[:, :], [LISTCF], [LISTCI])

    def phase_ffn(l, need_ctx):
        g.reset()
        wparts = [WBIG[:, i * 8192:(i + 1) * 8192].rearrange("p (k n) -> p k n", k=8) for i in range(4)]
        G2r = [g.carve([128, D], name="G2r") for _ in range(2)]
        for s in range(2 if need_ctx else 1):
            g.ld(G2r[s][:, :], MROWd.t.ap()[1, s:s + 1, :].partition_broadcast(128), [MROWd], [G2r[s]])
        XG = [[g.carve([128, D], BF16, name="XG") for _ in range(4)] for _ in range(2)]
        XGc = [g.carve([32, D], BF16, name="XGc") for _ in range(2)]
        GT = [g.carve([128, 4], name="GT") for _ in range(2)]
        GTc = [g.carve([32, 1], name="GTc") for _ in range(2)]
        xsT = g.carve([128, 8, 544], BF16, name="xsT")
        actT = g.carve([128, 8, 544], BF16, name="actT")
        sg = [g.carve([128, 544], name="sg") for _ in range(2)]
        YS = [g.carve([128, D], name="YS") for _ in range(2)]
        NCOL = 544 if need_ctx else 512
        exd_flat = EXd.t.ap().rearrange("e (t o) -> (e t) o", o=1)
        wcount = 0

        def load_w(src):
            nonlocal wcount
            part = wcount % 4
            wcount += 1
            wv = wparts[part]
            for hh in range(2):
                g.ld(wv[:, hh * 4:(hh + 1) * 4, :], src[hh * 512:(hh + 1) * 512, :].rearrange("(k p) n -> p k n", p=128),
                     [w_gate, w_up, w_down], [(WBIG, part)], q="pool")
            return wv, (WBIG, part)

        def gathers(e):
            b = e % 2
            for jt in range(4):
                idx = LISTI[:, e * 4 + jt:e * 4 + jt + 1]
                g.gather(XG[b][jt][:, :], XN2d[:, :], idx, [XN2d, LISTI], [XG[b][jt]])
                g.gather(GT[b][:, jt:jt + 1], exd_flat, idx, [EXd, LISTI], [GT[b]], elem_off=e * TT)
            if need_ctx:
                idc = LISTCI[:, e:e + 1]
                g.gather(XGc[b][:, :], XN2d[:, :], idc, [XN2d, LISTCI], [XGc[b]])
                g.gather(GTc[b][:, :], exd_flat, idc, [EXd, LISTCI], [GTc[b]], elem_off=e * TT)

        gathers(0)
        for e in range(NE):
            b = e % 2
            wg, wgp = load_w(w_gate.t.ap()[l, e])
            wu, wup = load_w(w_up.t.ap()[l, e])
            wd, wdp = load_w(w_down.t.ap()[l, e])
            if e + 1 < NE:
                gathers(e + 1)
            for kc in range(8):
                tp = PT[kc % 2]
                for jt in range(4):
                    g.tr(tp[:, jt * 128:(jt + 1) * 128], XG[b][jt][:, kc * 128:(kc + 1) * 128], identb[:, :], [XG[b][jt], identb], [tp])
                g.act(xsT[:, kc, 0:512], tp[:, 0:512], AF.Identity, [tp, AB], [xsT], bias=AB[:, 3, kc, 0:1], scale=AB[:, 2, kc, 0:1])
                if need_ctx:
                    g.tr(tp[:, 512:544], XGc[b][:, kc * 128:(kc + 1) * 128], identb[0:32, 0:32], [XGc[b], identb], [tp])
                    g.act(xsT[:, kc, 512:544], tp[:, 512:544], AF.Identity, [tp, AB], [xsT], bias=AB[:, 3, kc, 1:2], scale=AB[:, 2, kc, 1:2])
            for fb in range(8):
                aps, ups = PB[(fb % 2) * 2], PB[(fb % 2) * 2 + 1]
                apc = PB[4]
                fs = slice(fb * 128, (fb + 1) * 128)
                for kc in range(8):
                    g.mm(aps[:, :], wg[:, kc, fs], xsT[:, kc, 0:512], kc == 0, kc == 7, [wgp, xsT], [aps])
                for kc in range(8):
                    g.mm(ups[:, :], wu[:, kc, fs], xsT[:, kc, 0:512], kc == 0, kc == 7, [wup, xsT], [ups])
                sgb = sg[fb % 2]
                g.act(sgb[:, 0:512], aps[:, :], AF.Silu, [aps], [sgb])
                g.tt("dve", actT[:, fb, 0:512], sgb[:, 0:512], ups[:, :], ALU.mult, [sgb, ups], [actT])
                if need_ctx:
                    for kc in range(8):
                        g.mm(apc[:, 0:32], wg[:, kc, fs], xsT[:, kc, 512:544], kc == 0, kc == 7, [wgp, xsT], [apc])
                    for kc in range(8):
                        g.mm(apc[:, 32:64], wu[:, kc, fs], xsT[:, kc, 512:544], kc == 0, kc == 7, [wup, xsT], [apc])
                    g.act(sgb[:, 512:544], apc[:, 0:32], AF.Silu, [apc], [sgb])
                    g.tt("dve", actT[:, fb, 512:544], sgb[:, 512:544], apc[:, 32:64], ALU.mult, [sgb, apc], [actT])
            for st in range(5 if need_ctx else 4):
                ys = YS[st % 2]
                m = 128 if st < 4 else 32
                for nh in range(2):
                    yp = PB[4 + nh]
                    ns = slice(nh * 512, (nh + 1) * 512)
                    cs = slice(st * 128, st * 128 + m)
                    for fb in range(8):
                        g.mm(yp[0:m, :], actT[:, fb, cs], wd[:, fb, ns], fb == 0, fb == 7, [actT, wdp], [yp])
                    if st < 4:
                        g.stt(ys[:, ns], yp[:, :], GT[b][:, st:st + 1], G2r[0][:, ns], ALU.mult, ALU.mult, [yp, GT[b], G2r[0]], [ys])
                    else:
                        g.stt(ys[0:32, ns], yp[0:32, :], GTc[b][:, 0:1], G2r[1][0:32, ns], ALU.mult, ALU.mult, [yp, GTc[b], G2r[1]], [ys])
                if st < 4:
                    g.scatter_add(xres[:, :], ys[:, :], LISTI[:, e * 4 + st:e * 4 + st + 1], [ys, LISTI], [xres])
                else:
                    g.scatter_add(xres[:, :], ys[0:32, :], LISTCI[:, e:e + 1], [ys, LISTCI], [xres])

    def phase_final():
        g.reset()
        fgr = g.carve([128, D], name="fgr")
        g.ld(fgr[:, :], fg_d[0:1, :].partition_broadcast(128), [fg_d], [fgr])
        xin = [g.carve([128, 4, D], name="xinf") for _ in range(2)]
        ss = g.carve([128, 4], name="ssf"); rstd = g.carve([128, 4], name="rstdf")
        junk = g.carve([128, D], name="junkf")
        for i, (s, t0, n) in enumerate(tiles(False)):
            xt = xin[i % 2]
            g.ld(xt[:, :, :], xres[t0:t0 + n, :].rearrange("(a p) d -> p a d", p=128), [xres], [xt])
            rms_stats(xt, 4, ss, rstd, junk)
            for a in range(4):
                g.stt(xt[:, a, :], xt[:, a, :], rstd[:, a:a + 1], fgr[:, :], ALU.mult, ALU.mult, [xt, rstd, fgr], [xt])
            g.ld(out_d[t0:t0 + n, :].rearrange("(a p) d -> p a d", p=128), xt[:, :, :], [xt], [out_d])

    setup()
    for l in range(n_layers):
        need_ctx = l < DEPTH - 1
        phase_mod(l)
        if stop_after == ("mod", l):
            break
        phase_proj(l)
        phase_bwd(l)
        if stop_after == ("proj", l):
            break
        phase_mix(l, need_ctx)
        if stop_after == ("mix", l):
            break
        phase_route(l, need_ctx)
        if stop_after == ("route", l):
            break
        phase_ffn(l, need_ctx)
    phase_final()
    if debug:
        g.ld(DBGd[:, 0:64], LISTF[:, :], [LISTF], [DBGd])
        g.ld(DBGd[0:32, 64:80], LISTCF[:, :], [LISTCF], [DBGd])
        g.ld(DBGd[:, 128:192], AB.t.ap().rearrange("p a b c -> p (a b c)") if False else AB[:, :, :, :].rearrange("p a b c -> p (a b c)"), [AB], [DBGd])
    kb.finish()
    kb.emit()
    es.close()
    return nc, kb


def _prep_inputs(x, c, ctx, c_ctx, w_mod, b_mod, norm1_g, norm2_g, w_in, conv_w, ret_decay_logit,
                 attn_sink, w_out, w_router, w_gate, w_up, w_down, final_g):
    f = lambda a: np.ascontiguousarray(np.asarray(a, dtype=np.float32))
    cos, sin = _rope_tables()
    perm = _win_perm()
    shared = {
        "w_mod": f(w_mod),
        "bm_t": f(np.asarray(b_mod).reshape(DEPTH, 48, 128).transpose(2, 0, 1)),
        "b_mod": f(b_mod),
        "g1t": f(np.asarray(norm1_g).reshape(DEPTH, 8, 128).transpose(2, 0, 1)),
        "g2t": f(np.asarray(norm2_g).reshape(DEPTH, 8, 128).transpose(2, 0, 1)),
        "w_in": f(np.asarray(w_in)[:, :, perm]),
        "cw_t": f(np.asarray(conv_w).reshape(DEPTH, 3, 2, 128).transpose(3, 0, 1, 2).reshape(128, DEPTH * 6)),
        "rdl": f(np.asarray(ret_decay_logit).reshape(1, 32)),
        "sink": f(np.asarray(attn_sink).reshape(1, 32)),
        "w_out": f(w_out), "w_router": f(w_router),
        "w_gate": f(w_gate), "w_up": f(w_up), "w_down": f(w_down),
        "final_g": f(np.asarray(final_g).reshape(1, D)),
        "rope_cos": cos, "rope_sin": sin,
    }
    maps = []
    for core in range(8):
        b = core % 4
        m = dict(shared)
        m["x"] = f(np.asarray(x)[b])
        m["ctx"] = f(np.asarray(ctx)[b])
        m["cvec"] = f(np.stack([np.asarray(c)[b], np.asarray(c_ctx)], 0))
        maps.append(m)
    return maps


_CACHE = {}


def kernel(**inputs):
    maps = _prep_inputs(**inputs)
    if "nc" not in _CACHE:
        _CACHE["nc"] = build()[0]
    nc = _CACHE["nc"]
    res = run_bass_kernel_spmd(nc, maps, core_ids=list(range(8)))
    out = np.stack([np.asarray(res.results[b]["out"], dtype=np.float32) for b in range(4)], 0)
    return out
```

```python
import os
import numpy as np
from contextlib import ExitStack
import concourse.bass as bass
import concourse.mybir as mybir
from concourse.alu_op_type import AluOpType as ALU
from concourse.bass_utils import run_bass_kernel_spmd

F32 = mybir.dt.float32
BF16 = mybir.dt.bfloat16
I32 = mybir.dt.int32
AF = mybir.ActivationFunctionType
AX = mybir.AxisListType

SEM_LIMIT = 4000
NQ = 12

D = 1024
T = 4096
C = 256
TT = T + C
DEPTH = 4
NE = 16
CAP = 512
CAPC = 32
NW = 3968
EPS = 1e-6
ZW = T + C + 4
PE2 = "pool"
SKIP = set(os.environ.get("KSKIP", "").split(","))


class Buf:
    def __init__(self, t, nparts=1, name="", excl=False):
        self.t = t
        self.name = name
        self.excl = excl
        self.lw = [None] * nparts
        self.rd = [[] for _ in range(nparts)]
        self.np_ = nparts

    def __getitem__(self, k):
        return self.t[k]


class BV:
    def __init__(self, parent, ap):
        self.parent = parent
        self.t = ap

    def __getitem__(self, k):
        return self.t[k]


def _acc(x):
    if isinstance(x, BV):
        x = x.parent
    if isinstance(x, Buf):
        return x, range(x.np_)
    b, p = x
    if isinstance(b, BV):
        b = b.parent
    if isinstance(p, int):
        p = [p]
    return b, p


class Op:
    __slots__ = ("id", "eng", "fn", "fence", "deps", "cost", "lat", "dma", "start")

    def __init__(self, id, eng, fn, fence, deps, cost, lat, dma):
        self.id = id; self.eng = eng; self.fn = fn; self.fence = fence
        self.deps = deps; self.cost = cost; self.lat = lat; self.dma = dma; self.start = 0.0


class KB:
    def __init__(self, nc, es, same_engine_sync=True):
        self.nc = nc
        self.es = es
        self.eng = {"pe": nc.tensor, "dve": nc.vector, "act": nc.scalar, "pool": nc.gpsimd, "sp": nc.sync}
        self.nsem = 0
        self.ses = same_engine_sync
        self.n_instr = 0
        self.segs = [[]]
        self.nops = 0
        self._fz = nc.alloc_sbuf_tensor("fencez", [128, 8], F32)
        self.fzb = Buf(self._fz, 1, "fencez")
        self.op("dve", lambda e: e.memset(self._fz[:, :], 0.0), (), [self.fzb])

    def _sem(self, name):
        self.nsem += 1
        return self.es.enter_context(self.nc.semaphore(f"{name}_{self.nsem}"))

    def _deps(self, reads, writes):
        deps = set()
        for x in reads:
            b, ps = _acc(x)
            for p in ps:
                if b.lw[p] is not None:
                    deps.add(b.lw[p])
                if b.excl:
                    deps.update(b.rd[p])
        for x in writes:
            b, ps = _acc(x)
            for p in ps:
                if b.lw[p] is not None:
                    deps.add(b.lw[p])
                deps.update(b.rd[p])
        return deps

    def _record(self, oid, reads, writes):
        for x in reads:
            b, ps = _acc(x)
            for p in ps:
                b.rd[p].append(oid)
        for x in writes:
            b, ps = _acc(x)
            for p in ps:
                b.lw[p] = oid
                b.rd[p] = []

    def _add(self, e, fn, reads, writes, fence, cost, lat, dma):
        if fence:
            reads = list(reads) + [self.fzb]
        oid = self.nops
        self.nops += 1
        o = Op(oid, e, fn, fence, self._deps(reads, writes), float(cost), float(lat), dma)
        self.segs[-1].append(o)
        self._record(oid, reads, writes)
        self.n_instr += 1
        return oid

    def op(self, e, fn, reads=(), writes=(), fence=False, cost=150.0):
        return self._add(e, fn, reads, writes, fence, cost, cost + 80.0, False)

    def dma(self, q, fn, reads=(), writes=(), nbytes=0):
        issue = 120.0 if q == "sp" else 900.0
        lat = 2200.0 + nbytes / 120.0
        return self._add(q, fn, reads, writes, False, issue, lat, True)

    def barrier(self):
        if self.segs[-1]:
            self.segs.append([])

    def finish(self):
        pass

    def sb(self, name, shape, dt=F32, nparts=1):
        return Buf(self.nc.alloc_sbuf_tensor(name, list(shape), dt), nparts, name)

    def ps(self, name, shape, dt=F32, nparts=1):
        return Buf(self.nc.alloc_psum_tensor(name, list(shape), dt), nparts, name, excl=True)

    def dram(self, name, shape, dt=F32, kind="Internal", nparts=1):
        return Buf(self.nc.dram_tensor(name, list(shape), dt, kind=kind), nparts, name)

    def _schedule(self, seg):
        ids = {o.id for o in seg}
        byid = {o.id: o for o in seg}
        succ = {o.id: [] for o in seg}
        indeg = {}
        for o in seg:
            d = [x for x in o.deps if x in ids]
            indeg[o.id] = len(d)
            for x in d:
                succ[x].append(o.id)
        free = {e: 0.0 for e in self.eng}
        rt = {o.id: 0.0 for o in seg}
        ready = {e: [] for e in self.eng}
        for o in seg:
            if indeg[o.id] == 0:
                ready[o.eng].append(o.id)
        order = []
        n = len(seg)
        while len(order) < n:
            best = None
            for e, lst in ready.items():
                if not lst:
                    continue
                f = free[e]
                c = min(lst, key=lambda i: (max(f, rt[i]), i))
                key = (max(f, rt[c]), c)
                if best is None or key < best[0]:
                    best = (key, e, c)
            (st, _), e, c = best
            o = byid[c]
            ready[e].remove(c)
            o.start = st
            free[e] = st + o.cost
            fin = st + o.lat
            order.append(o)
            for sx in succ[c]:
                if rt[sx] < fin:
                    rt[sx] = fin
                indeg[sx] -= 1
                if indeg[sx] == 0:
                    ready[byid[sx].eng].append(sx)
        return order

    def emit(self):
        esem = {}; ecnt = {}; eretired = []
        dsem = {}; dcnt = {}; dretired = []
        for e in self.eng:
            esem[e] = self._sem("e_" + e); ecnt[e] = 0
        for q in ("sp", "pool"):
            dsem[q] = [self._sem(f"d_{q}{i}") for i in range(NQ)]; dcnt[q] = 0
        waited = {e: {} for e in self.eng}
        prog = {e: [] for e in self.eng}
        event = {}

        def wait(e, evs):
            w = waited[e]
            best = {}
            for (sem, val, src) in evs:
                if src == e and (e == "pe" or not self.ses):
                    continue
                k = id(sem)
                if w.get(k, 0) >= val:
                    continue
                if k not in best or best[k][1] < val:
                    best[k] = (sem, val)
            for k, (sem, val) in best.items():
                prog[e].append(("wait", sem, val))
                w[k] = val

        def all_events():
            evs = list(eretired) + list(dretired)
            for e in self.eng:
                if ecnt[e] > 0:
                    evs.append((esem[e], ecnt[e], e))
            for q in dsem:
                n = dcnt[q]
                for j, sm in enumerate(dsem[q]):
                    cnt = (n - j + NQ - 1) // NQ if n > j else 0
                    if cnt > 0:
                        evs.append((sm, 16 * cnt, "dma"))
            return evs

        self.sched_span = []
        for si, seg in enumerate(self.segs):
            if not seg:
                continue
            if si > 0:
                evs = all_events()
                for e in self.eng:
                    wait(e, evs)
            order = self._schedule(seg)
            self.sched_span.append(max(o.start + o.lat for o in order))
            for o in order:
                e = o.eng
                evs = [event[d] for d in o.deps if d in event]
                if o.dma:
                    i = dcnt[e]
                    if 16 * (i // NQ) + 16 > SEM_LIMIT:
                        for j, sm in enumerate(dsem[e]):
                            cnt = (i - j + NQ - 1) // NQ if i > j else 0
                            if cnt > 0:
                                dretired.append((sm, 16 * cnt, "dma"))
                        dsem[e] = [self._sem(f"d_{e}{k}") for k in range(NQ)]
                        dcnt[e] = 0
                        i = 0
                    sem = dsem[e][i % NQ]
                    prev = 16 * (i // NQ)
                    if prev > 0:
                        evs.append((sem, prev, "dma"))
                    wait(e, evs)
                    prog[e].append(("op", o.fn, False, sem, 16))
                    dcnt[e] += 1
                    event[o.id] = (sem, prev + 16, "dma")
                else:
                    if ecnt[e] >= SEM_LIMIT:
                        eretired.append((esem[e], ecnt[e], e))
                        esem[e] = self._sem("e_" + e); ecnt[e] = 0
                    wait(e, evs)
                    prog[e].append(("op", o.fn, o.fence, esem[e], 1))
                    ecnt[e] += 1
                    event[o.id] = (esem[e], ecnt[e], e)
        wait("sp", all_events())
        self.prog = prog
        with self.nc.Block() as block:
            for e, dec in (("pe", block.tensor), ("dve", block.vector), ("act", block.scalar),
                           ("pool", block.gpsimd), ("sp", block.sync)):
                pr = prog[e]
                if not pr:
                    continue

                def body(eng, pr=pr, e=e):
                    for it in pr:
                        if it[0] == "wait":
                            eng.wait_ge(it[1], it[2])
                        else:
                            _, fn, fence, sem, inc = it
                            ins = fn(eng)
                            if fence:
                                ins = self._fence(e)
                            ins.then_inc(sem, inc)

                dec(body)

    def _fence(self, e):
        if e == "dve":
            return self.nc.vector.tensor_copy(self._fz[0:1, 0:1], self._fz[0:1, 1:2])
        if e == "act":
            return self.nc.scalar.copy(self._fz[0:1, 2:3], self._fz[0:1, 3:4])
        raise ValueError(e)


class G:
    def __init__(self, kb, arena_words):
        self.kb = kb
        self.nc = kb.nc
        self.arena = kb.nc.alloc_sbuf_tensor("arena", [128, arena_words], F32)
        self.aw = arena_words
        self.ap_ = 0
        self.uid = 0
        self.live = []
        self.old = []
        self.gen = 0
        self.use_barrier = False

    def reset(self):
        if self.use_barrier:
            self.kb.barrier()
        self.old = self.old + self.live
        self.live = []
        self.gen += 1
        self.ap_ = 0

    def carve(self, shape, dt=F32, parts=128, nparts=1, name="a"):
        n = 1
        for s in shape[1:]:
            n *= s
        words = n if dt in (F32, I32) else (n + 1) // 2
        words = (words + 1) // 2 * 2
        off = self.ap_
        self.ap_ += words
        assert self.ap_ <= self.aw, f"arena overflow {self.ap_} > {self.aw} ({name})"
        v = self.arena[0:shape[0], off:off + words]
        if dt != F32:
            v = v.bitcast(dt)
        if dt == BF16:
            v = v[:, 0:n]
        else:
            v = v[:, 0:n]
        if len(shape) == 3:
            v = v.rearrange("p (a b) -> p a b", a=shape[1])
        elif len(shape) == 4:
            v = v.rearrange("p (a b c) -> p a b c", a=shape[1], b=shape[2])
        self.uid += 1
        nb = Buf(v, nparts, f"{name}{self.uid}")
        nb.gen = self.gen
        inherit = set()
        for (s_, e_, b_) in self.old:
            if s_ < off + words and off < e_:
                for p in range(b_.np_):
                    if b_.lw[p] is not None:
                        inherit.add(b_.lw[p])
                    inherit.update(b_.rd[p])
        if inherit:
            for p in range(nparts):
                nb.rd[p] = list(inherit)
        self.old = [(s_, e_, b_) for (s_, e_, b_) in self.old if not (off <= s_ and e_ <= off + words)]
        self.live.append((off, off + words, nb))
        return nb

    @staticmethod
    def nf(ap):
        n = 1
        for d in list(ap.shape)[1:]:
            n *= d
        return n

    def _ecost(self, eng, n):
        if eng == "dve":
            return 70.0 + 1.1 * n
        if eng == "act":
            return 110.0 + 0.95 * n
        return 120.0 + 2.3 * n

    def mm(self, out, lhsT, rhs, st, sp, R, W):
        n = self.nf(rhs)
        c = 70.0 + 0.4 * n
        if lhsT.shape[0] < 128 or self.nf(lhsT) < 128:
            c = 70.0 + 0.85 * n
        self.kb.op("pe", lambda e: e.matmul(out, lhsT, rhs, start=st, stop=sp), R, W, cost=c)

    def tr(self, out, in_, ident, R, W):
        self.kb.op("pe", lambda e: e.transpose(out, in_, ident), R, W, cost=180.0)

    def act(self, out, in_, func, R, W, bias=None, scale=None, accum=None):
        kw = {}
        if bias is not None:
            kw["bias"] = bias
        if scale is not None:
            kw["scale"] = scale
        if accum is not None:
            kw["accum_out"] = accum
        c = self._ecost("act", self.nf(out)) + (150.0 if accum is not None else 0.0)
        self.kb.op("act", lambda e: e.activation(out, in_, func, **kw), R, W, fence=accum is not None, cost=c)

    def tt(self, eng, out, a, b, op, R, W):
        self.kb.op(eng, lambda e: e.tensor_tensor(out, a, b, op), R, W, cost=self._ecost(eng, self.nf(out)))

    def ts(self, eng, out, a, s1, s2, op0, op1, R, W, accum=None):
        c = self._ecost(eng, self.nf(out))
        if accum is not None:
            self.kb.op(eng, lambda e: e.tensor_scalar(out, a, s1, None, op0, op1, accum_out=accum), R, W, fence=True, cost=c + 150.0)
        elif op1 is None:
            self.kb.op(eng, lambda e: e.tensor_scalar(out, a, s1, None, op0), R, W, cost=c)
        else:
            self.kb.op(eng, lambda e: e.tensor_scalar(out, a, s1, s2, op0, op1), R, W, cost=c)

    def stt(self, out, in0, scalar, in1, op0, op1, R, W):
        self.kb.op("dve", lambda e: e.scalar_tensor_tensor(out, in0, scalar, in1, op0, op1), R, W, cost=70.0 + 2.0 * self.nf(out))

    def cp(self, eng, out, in_, R, W):
        c = self._ecost(eng, self.nf(out))
        if eng == "act":
            self.kb.op("act", lambda e: e.copy(out, in_), R, W, cost=c)
        else:
            self.kb.op(eng, lambda e: e.tensor_copy(out, in_), R, W, cost=c)

    def memset(self, eng, out, val, W):
        self.kb.op(eng, lambda e: e.memset(out, val), (), W, cost=self._ecost(eng, self.nf(out)))

    def recip(self, out, in_, R, W):
        self.kb.op("dve", lambda e: e.reciprocal(out, in_), R, W, cost=70.0 + 3.0 * self.nf(out))

    def red(self, out, in_, R, W):
        self.kb.op("dve", lambda e: e.tensor_reduce(out, in_, AX.X, ALU.add), R, W, cost=70.0 + 1.1 * self.nf(in_))

    @staticmethod
    def _nbytes(ap):
        n = 1
        for d in list(ap.shape):
            n *= d
        return n * 4

    def ld(self, out, in_, R, W, q="sp", slow=False):
        nb = self._nbytes(out)
        if slow:
            self.kb.dma(q, lambda e: e.dma_start(out=out, in_=in_, allow_slow_non_contiguous=True), R, W, nbytes=nb)
        else:
            self.kb.dma(q, lambda e: e.dma_start(out=out, in_=in_), R, W, nbytes=nb)

    def gather(self, out, src, idx, R, W, elem_off=0):
        self.kb.dma("pool", lambda e: e.indirect_dma_start(
            out=out, out_offset=None, in_=src,
            in_offset=bass.IndirectOffsetOnAxis(ap=idx, axis=0), element_offset=elem_off), R, W, nbytes=self._nbytes(out))

    def scatter_add(self, dst, src, idx, R, W):
        self.kb.dma("pool", lambda e: e.indirect_dma_start(
            out=dst, out_offset=bass.IndirectOffsetOnAxis(ap=idx, axis=0),
            in_=src, in_offset=None, compute_op=ALU.add), R, W, nbytes=2 * self._nbytes(src))


def _win_perm():
    names = ['cb', 'cc', 'cx', 'rq', 'rk', 'rv', 'rgf', 'rgb', 'aq', 'ak', 'av']
    sizes = [256] * 3 + [256] * 5 + [512, 128, 128]
    off = {}
    o = 0
    for n, s in zip(names, sizes):
        off[n] = o
        o += s

    def sw(nh):
        idx = []
        for h in range(nh):
            for i in range(64):
                b4, j = i // 16, i % 16
                idx.append(h * 64 + (b4 ^ 1) * 16 + j)
        return np.array(idx)

    cols = []
    for n in ('cb', 'cc', 'cx'):
        cols += list(off[n] + np.arange(256))
    cols += list(off['rq'] + np.arange(256)) + list(off['rq'] + sw(4))
    cols += list(off['rk'] + np.arange(256)) + list(off['rk'] + sw(4))
    cols += list(off['aq'] + np.arange(512)) + list(off['aq'] + sw(8))
    cols += list(off['ak'] + np.arange(128)) + list(off['ak'] + sw(2))
    cols += list(off['rv'] + np.arange(256)) + list(off['rgf'] + np.arange(256))
    cols += list(off['rgb'] + np.arange(256)) + list(off['av'] + np.arange(128))
    cols = np.array(cols)
    assert cols.shape[0] == NW
    return cols


def _rope_tables():
    t = np.arange(T)
    row = (t // 64).astype(np.float32)
    col = (t % 64).astype(np.float32)
    inv = (np.float32(10000.0) ** (-np.arange(0, 32, 2, dtype=np.float32) / np.float32(32))).astype(np.float32)
    ar = (row[:, None] * inv[None, :]).astype(np.float32)
    ac = (col[:, None] * inv[None, :]).astype(np.float32)
    cr, sr, cc_, sc_ = np.cos(ar).T, np.sin(ar).T, np.cos(ac).T, np.sin(ac).T
    cos64 = np.concatenate([cr, cr, cc_, cc_], 0)
    sin64 = np.concatenate([-sr, sr, -sc_, sc_], 0)
    cos = np.ones((128, TT), np.float32)
    sin = np.zeros((128, TT), np.float32)
    cos[:, :T] = np.concatenate([cos64, cos64], 0)
    sin[:, :T] = np.concatenate([sin64, sin64], 0)
    return cos, sin


def build(n_layers=DEPTH, debug=False, stop_after=None, wl=DEPTH, ne_w=NE):
    nc = bass.Bass("TRN2", target_bir_lowering=False)
    es = ExitStack()
    kb = KB(nc, es)
    skind = "ExternalOutput" if debug else "Internal"

    def din(name, shape, dt=F32):
        return kb.dram(name, shape, dt, kind="ExternalInput")

    x_in = din("x", [T, D]); ctx_in = din("ctx", [C, D]); cvec = din("cvec", [2, D])
    w_mod = din("w_mod", [wl, D, 6 * D]); bm_t = din("bm_t", [128, DEPTH, 48]); b_mod = din("b_mod", [DEPTH, 6 * D])
    g1t_d = din("g1t", [128, DEPTH, 8]); g2t_d = din("g2t", [128, DEPTH, 8])
    w_in = din("w_in", [wl, D, NW]); cw_d = din("cw_t", [128, DEPTH * 6])
    rdl_d = din("rdl", [1, 32]); sink_d = din("sink", [1, 32])
    w_out = din("w_out", [wl, D, D]); w_router = din("w_router", [wl, D, NE])
    w_gate = din("w_gate", [wl, ne_w, D, D]); w_up = din("w_up", [wl, ne_w, D, D]); w_down = din("w_down", [wl, ne_w, D, D])
    fg_d = din("final_g", [1, D]); cos_d = din("rope_cos", [128, TT]); sin_d = din("rope_sin", [128, TT])
    out_d = kb.dram("out", [T, D], F32, kind="ExternalOutput")

    xres = kb.dram("xres", [TT, D], F32, kind=skind)
    Fd = kb.dram("Fd", [18 * 64, TT], BF16, kind=skind)
    TMd = kb.dram("TMd", [TT, 1152], BF16, kind=skind)
    Zd = kb.dram("Zd", [2, 128, ZW], F32, kind=skind)
    CBd = kb.dram("CBd", [2, 128, TT], BF16, kind=skind)
    SPd = kb.dram("SPd", [2, 34, 64, 256], BF16, kind=skind)
    XN2d = kb.dram("XN2d", [TT, D], BF16, kind=skind)
    EXd = kb.dram("EXd", [NE, TT], F32, kind=skind)
    POSd = kb.dram("POSd", [32, T], F32, kind=skind)
    MROWd = kb.dram("MROWd", [2, 2, D], F32, kind=skind)
    DBGd = kb.dram("DBGd", [128, 256], F32, kind=skind)

    identb = kb.sb("identb", [128, 128], BF16); identf = kb.sb("identf", [128, 128])
    Dm = kb.sb("Dm", [128, 128]); Dpos = kb.sb("Dpos", [128, 128]); Dneg = kb.sb("Dneg", [128, 128])
    Mge8 = kb.sb("Mge8", [128, 128]); Mle8 = kb.sb("Mle8", [128, 128])
    Mprev = kb.sb("Mprev", [128, 4, 128], BF16); Mnext = kb.sb("Mnext", [128, 4, 128], BF16)
    ones_bf = kb.sb("ones_bf", [128, 64], BF16); ones16 = kb.sb("ones16", [16, 16])
    pcol = kb.sb("pcol", [128, 1]); p127 = kb.sb("p127", [128, 1]); jval = kb.sb("jval", [128, 4]); jb = kb.sb("jb", [128, 4])
    ip1 = kb.sb("ip1", [64, 128]); rev = kb.sb("rev", [64, 128])
    scv = kb.sb("scv", [128, 8, 2]); bm_f = kb.sb("bm_f", [128, DEPTH, 48])
    g1t = kb.sb("g1t_s", [128, DEPTH, 8]); g2t = kb.sb("g2t_s", [128, DEPTH, 8]); cw = kb.sb("cw", [128, DEPTH * 6])
    rdl_b = kb.sb("rdl_b", [128, 32]); sink_b = kb.sb("sink_b", [128, 32])
    AB = kb.sb("AB", [128, 4, 8, 2])
    lg = kb.sb("lg", [128, 8]); wkc = kb.sb("wkc", [128, 8]); dcol = kb.sb("dcol", [128, 8]); se = kb.sb("se", [128, 8])
    DMf = kb.sb("DMf", [128, 4, 128]); DMb = kb.sb("DMb", [128, 4, 128])
    QD = kb.sb("QD", [64, 2, 4, 128], BF16); WK = kb.sb("WK", [128, 2, 256]); DEC = kb.sb("DEC", [64, 2, 256])
    SINKE = kb.sb("SINKE", [64, 2, 512]); Wr = kb.sb("Wr", [128, 8, NE])
    KC = kb.sb("KC", [64, 2, 256], BF16); VC = kb.sb("VC", [128, 2, 128], BF16)
    Sf = kb.sb("Sf", [64, 256]); Sb = kb.sb("Sb", [64, 256])
    LISTF = kb.sb("LISTF", [128, 64]); LISTI = kb.sb("LISTI", [128, 64], I32)
    LISTCF = kb.sb("LISTCF", [32, 16]); LISTCI = kb.sb("LISTCI", [32, 16], I32)
    SACC = kb.sb("SACC", [128, 64])
    tmp128 = kb.sb("tmp128", [128, 128])
    WBIG = kb.sb("WBIG", [128, 32768], BF16, nparts=4)
    PB = [kb.ps(f"PB{i}", [128, 512]) for i in range(8)]
    PBF = [BV(PB[i], PB[i][:, :].bitcast(BF16)) for i in range(8)]
    PT = [PBF[6], PBF[7]]

    g = G(kb, 28672)

    def v3(ap, a):
        return ap.rearrange("p (a b) -> p a b", a=a)

    def setup():
        kb.op("pool", lambda e: e.iota(Dm[:, :], [[1, 128]], base=0, channel_multiplier=-1,
                                       allow_small_or_imprecise_dtypes=True), (), [Dm])
        g.ts("dve", identf[:, :], Dm[:, :], 0.0, None, ALU.is_equal, None, [Dm], [identf])
        g.cp("dve", identb[:, :], identf[:, :], [identf], [identb])
        g.ts("dve", Dpos[:, :], Dm[:, :], 0.0, None, ALU.max, None, [Dm], [Dpos])
        g.ts("dve", Dneg[:, :], Dm[:, :], -1.0, 0.0, ALU.mult, ALU.max, [Dm], [Dneg])
        g.ts("dve", Mge8[:, :], Dm[:, :], 0.0, 0.125, ALU.is_ge, ALU.mult, [Dm], [Mge8])
        g.ts("dve", Mle8[:, :], Dm[:, :], 0.0, 0.125, ALU.is_le, ALU.mult, [Dm], [Mle8])
        for gg in range(4):
            g.ts("dve", Mprev[:, gg, :], Dm[:, :], 0.0, None, ALU.is_le, None, [Dm], [Mprev])
            g.ts("dve", Mnext[:, gg, :], Dm[:, :], 0.0, None, ALU.is_ge, None, [Dm], [Mnext])
        g.memset("dve", ones_bf[:, :], 1.0, [ones_bf])
        g.memset("dve", ones16[:, :], 1.0, [ones16])
        kb.op("pool", lambda e: e.iota(pcol[:, :], [[0, 1]], base=0, channel_multiplier=1,
                                       allow_small_or_imprecise_dtypes=True), (), [pcol])
        g.ts("dve", p127[:, :], pcol[:, :], -1.0, 127.0, ALU.mult, ALU.add, [pcol], [p127])
        kb.op("pool", lambda e: e.iota(jval[:, :], [[128, 4]], base=0, channel_multiplier=1,
                                       allow_small_or_imprecise_dtypes=True), (), [jval])
        g.ts("dve", jb[:, :], jval[:, :], 0.5, None, ALU.add, None, [jval], [jb])
        kb.op("pool", lambda e: e.iota(ip1[:, :], [[1, 128]], base=1, channel_multiplier=0,
                                       allow_small_or_imprecise_dtypes=True), (), [ip1])
        kb.op("pool", lambda e: e.iota(rev[:, :], [[-1, 128]], base=128, channel_multiplier=0,
                                       allow_small_or_imprecise_dtypes=True), (), [rev])
        g.memset("dve", LISTF[:, :], 0.0, [LISTF])
        g.memset("dve", LISTCF[:, :], 0.0, [LISTCF])
        g.ld(xres[0:T, :], x_in[:, :], [x_in], [xres])
        g.ld(xres[T:TT, :], ctx_in[:, :], [ctx_in], [xres])
        g.memset("dve", tmp128[:, :], 0.0, [tmp128])
        for c0 in (0, T + 1, T + 2, T + C + 3):
            g.ld(Zd.t.ap()[:, :, c0:c0 + 1].rearrange("c p o -> p c o"), v3(tmp128[:, 0:2], 2), [tmp128], [Zd], slow=True)
        for s_ in range(2):
            g.ld(scv[:, :, s_:s_ + 1], cvec.t.ap()[s_:s_ + 1, :].rearrange("s (kc p) -> p kc s", p=128), [cvec], [scv], slow=True)
        g.act(scv[:, :, :], scv[:, :, :], AF.Silu, [scv], [scv])
        g.ld(bm_f[:, :, :], bm_t[:, :, :], [bm_t], [bm_f])
        g.ld(g1t[:, :, :], g1t_d[:, :, :], [g1t_d], [g1t])
        g.ld(g2t[:, :, :], g2t_d[:, :, :], [g2t_d], [g2t])
        g.ld(cw[:, :], cw_d[:, :], [cw_d], [cw])
        g.ld(rdl_b[:, :], rdl_d[0:1, :].partition_broadcast(128), [rdl_d], [rdl_b])
        g.ld(sink_b[:, :], sink_d[0:1, :].partition_broadcast(128), [sink_d], [sink_b])

    def phase_mod(l):
        g.reset()
        modf_ps = v3(PB[0][:, 0:64], 32)
        rows_ps = PB[1]
        rows_sb = g.carve([2, 2048], name="rows")
        bmr = g.carve([2, 2048], name="bmr")
        modf = g.carve([128, 32, 2], name="modf")
        tmpm = g.carve([128, 8, 2], name="tmpm")
        WM = [g.carve([128, 8, 256], name="WM") for _ in range(2)]
        g.ld(bmr[0:2, 0:1024], b_mod[l:l + 1, 2 * D:3 * D].partition_broadcast(2), [b_mod], [bmr])
        g.ld(bmr[0:2, 1024:2048], b_mod[l:l + 1, 5 * D:6 * D].partition_broadcast(2), [b_mod], [bmr])
        wsrc = w_mod.t.ap()[l].rearrange("(kc p) n -> p kc n", p=128)
        for j in range(24):
            grp = j // 4
            wm = WM[j % 2]
            g.ld(wm[:, :, :], wsrc[:, :, j * 256:(j + 1) * 256], [w_mod], [wm])
            if grp in (0, 1, 3, 4):
                gi = {0: 0, 1: 1, 3: 2, 4: 3}[grp]
                for nb in range(2):
                    fm = gi * 8 + (j % 4) * 2 + nb
                    for kc in range(8):
                        g.mm(modf_ps[:, fm, :], wm[:, kc, nb * 128:(nb + 1) * 128], scv[:, kc, :], kc == 0, kc == 7,
                             [wm, scv], [PB[0]])
            else:
                ri = 0 if grp == 2 else 1
                col = ri * 1024 + (j % 4) * 256
                for kc in range(8):
                    g.mm(rows_ps[0:2, 0:256], scv[:, kc, :], wm[:, kc, :], kc == 0, kc == 7, [wm, scv], [PB[1]])
                g.tt("dve", rows_sb[0:2, col:col + 256], rows_ps[0:2, 0:256], bmr[0:2, col:col + 256], ALU.add,
                     [PB[1], bmr], [rows_sb])
        for gi, Gi in enumerate((0, 1, 3, 4)):
            g.tt("dve", modf[:, gi * 8:(gi + 1) * 8, :], modf_ps[:, gi * 8:(gi + 1) * 8, :],
                 bm_f[:, l, Gi * 8:(Gi + 1) * 8].unsqueeze(2).to_broadcast([128, 8, 2]), ALU.add, [PB[0], bm_f], [modf])
        g.ts("dve", tmpm[:, :, :], modf[:, 8:16, :], 1.0, None, ALU.add, None, [modf], [tmpm])
        g.tt("dve", AB[:, 0, :, :], tmpm[:, :, :], g1t[:, l, :].unsqueeze(2).to_broadcast([128, 8, 2]), ALU.mult,
             [tmpm, g1t], [AB])
        g.cp("dve", AB[:, 1, :, :], modf[:, 0:8, :], [modf], [AB])
        g.ts("dve", tmpm[:, :, :], modf[:, 24:32, :], 1.0, None, ALU.add, None, [modf], [tmpm])
        g.tt("dve", AB[:, 2, :, :], tmpm[:, :, :], g2t[:, l, :].unsqueeze(2).to_broadcast([128, 8, 2]), ALU.mult,
             [tmpm, g2t], [AB])
        g.cp("dve", AB[:, 3, :, :], modf[:, 16:24, :], [modf], [AB])
        g.ld(MROWd.t.ap()[0], rows_sb[0:2, 0:1024], [rows_sb], [MROWd])
        g.ld(MROWd.t.ap()[1], rows_sb[0:2, 1024:2048], [rows_sb], [MROWd])
        g.act(lg[:, :], rdl_b[:, l * 8:(l + 1) * 8], AF.Exp, [rdl_b], [lg], scale=-1.0)
        g.ts("dve", lg[:, :], lg[:, :], 1.0, None, ALU.add, None, [lg], [lg])
        g.act(lg[:, :], lg[:, :], AF.Ln, [lg], [lg])
        g.ts("dve", lg[:, :], lg[:, :], -1.0, None, ALU.mult, None, [lg], [lg])
        for d in range(2):
            for h in range(4):
                dh = d * 4 + h
                g.act(tmp128[:, :], (Dpos if d == 0 else Dneg)[:, :], AF.Exp, [Dpos, Dneg, lg], [tmp128], scale=lg[:, dh:dh + 1])
                g.tt("dve", (DMf if d == 0 else DMb)[:, h, :], tmp128[:, :], (Mge8 if d == 0 else Mle8)[:, :], ALU.mult,
                     [tmp128, Mge8, Mle8], [DMf if d == 0 else DMb])
                g.act(QD[:, d, h, :], (ip1 if d == 0 else rev)[:, :], AF.Exp, [ip1, rev, lg], [QD], scale=lg[0:64, dh:dh + 1])
                g.act(wkc[:, dh:dh + 1], (p127 if d == 0 else pcol)[:, :], AF.Exp, [p127, pcol, lg], [wkc], scale=lg[:, dh:dh + 1])
        g.ts("dve", wkc[:, :], wkc[:, :], 0.125, None, ALU.mult, None, [wkc], [wkc])
        g.act(dcol[:, :], lg[:, :], AF.Exp, [lg], [dcol], scale=128.0)
        g.act(se[:, :], sink_b[:, l * 8:(l + 1) * 8], AF.Exp, [sink_b], [se])
        for d in range(2):
            g.cp("dve", v3(WK[:, d, :], 4), wkc[:, d * 4:(d + 1) * 4].unsqueeze(2).to_broadcast([128, 4, 64]), [wkc], [WK])
            g.cp("dve", v3(DEC[:, d, :], 4), dcol[0:64, d * 4:(d + 1) * 4].unsqueeze(2).to_broadcast([64, 4, 64]), [dcol], [DEC])
            g.cp("dve", v3(SINKE[:, d, :], 4), se[0:64, d * 4:(d + 1) * 4].unsqueeze(2).to_broadcast([64, 4, 128]), [se], [SINKE])
        g.ld(Wr[:, :, :], w_router.t.ap()[l].rearrange("(kc p) e -> p kc e", p=128), [w_router], [Wr])

    FM_PAIRS = [
        (6, 8, 0, False), (7, 9, 2, False),
        (10, 12, 4, True), (11, 13, 6, True),
        (14, 18, 8, False), (15, 19, 10, False), (16, 20, 12, False), (17, 21, 14, False),
        (22, 23, 16, False),
    ]

    def zcol(tt):
        return tt + 1 if tt < T else tt + 3

    def chunk_of(tt):
        return tt // 128 if tt < T else 32 + (tt - T) // 128

    def tiles(with_ctx=True):
        ts_ = []
        if with_ctx:
            ts_.append((1, T, C))
        for i in range(T // 512):
            ts_.append((0, i * 512, 512))
        return ts_

    def rms_stats(xin, nsub, ss, rstd, junk):
        for a in range(nsub):
            g.act(junk[:, :], xin[:, a, :], AF.Square, [xin], [junk, ss], accum=ss[:, a:a + 1])
        g.ts("dve", rstd[:, 0:nsub], ss[:, 0:nsub], 1.0 / D, EPS, ALU.mult, ALU.add, [ss], [rstd])
        g.act(rstd[:, 0:nsub], rstd[:, 0:nsub], AF.Sqrt, [rstd], [rstd])
        g.recip(rstd[:, 0:nsub], rstd[:, 0:nsub], [rstd], [rstd])

    def phase_proj(l):
        g.reset()
        win = WBIG[:, 0:8 * NW].rearrange("p (k n) -> p k n", k=8)
        for kc in range(8):
            for hh in range(2):
                g.ld(win[:, kc, hh * 1984:(hh + 1) * 1984],
                     w_in.t.ap()[l, kc * 128:(kc + 1) * 128, hh * 1984:(hh + 1) * 1984], [w_in], [WBIG], q="pool")
        xin = g.carve([128, 4, D], name="xin")
        xn = g.carve([128, 4, D], BF16, name="xn")
        hT = g.carve([128, 8, 512], BF16, name="hT")
        cosb = g.carve([128, 512], name="cosb"); sinb = g.carve([128, 512], name="sinb")
        ss = g.carve([128, 4], name="ss"); rstd = g.carve([128, 4], name="rstd")
        junk = g.carve([128, D], name="junk")
        tmst = g.carve([128, 4, 1152], BF16, name="tmst")
        t1 = g.carve([128, 512], name="t1"); t2 = g.carve([128, 512], name="t2")
        fst = [g.carve([128, 512], BF16, name="fst") for _ in range(2)]
        kst = [g.carve([128, 512], BF16, name="kst") for _ in range(2)]
        ccs = g.carve([128, 512], name="ccs")
        zst = g.carve([128, 2, 512], name="zst")
        cbst = g.carve([128, 2, 512], BF16, name="cbst")
        kw = g.carve([128, 256], BF16, name="kw")
        spst = [g.carve([64, 256], BF16, name="spst") for _ in range(2)]
        g.memset("dve", Sf[:, :], 0.0, [Sf])
        fcount = 0
        pbi = 0
        Ffull = Fd.t.ap()
        for (s, t0, n) in tiles(True):
            nsub = n // 128
            g.ld(xin[:, 0:nsub, :], xres[t0:t0 + n, :].rearrange("(a p) d -> p a d", p=128), [xres], [xin])
            g.ld(cosb[:, 0:n], cos_d[:, t0:t0 + n], [cos_d], [cosb])
            g.ld(sinb[:, 0:n], sin_d[:, t0:t0 + n], [sin_d], [sinb])
            rms_stats(xin, nsub, ss, rstd, junk)
            for a in range(nsub):
                g.ts("dve", xn[:, a, :], xin[:, a, :], rstd[:, a:a + 1], None, ALU.mult, None, [xin, rstd], [xn])
            for kc in range(8):
                tp = PT[kc % 2]
                for a in range(nsub):
                    g.tr(tp[:, a * 128:(a + 1) * 128], xn[:, a, kc * 128:(kc + 1) * 128], identb[:, :], [xn, identb], [tp])
                g.act(hT[:, kc, 0:n], tp[:, 0:n], AF.Identity, [tp, AB], [hT],
                      bias=AB[:, 1, kc, s:s + 1], scale=AB[:, 0, kc, s:s + 1])

            def fm_block(fb):
                nonlocal pbi
                pb = PB[pbi % 4]
                pbi += 1
                for kc in range(8):
                    g.mm(pb[:, 0:n], win[:, kc, fb * 128:(fb + 1) * 128], hT[:, kc, 0:n], kc == 0, kc == 7, [WBIG, hT], [pb])
                return pb

            for c in range(2):
                pb = fm_block(c)
                g.cp("act", cbst[:, c, 0:n], pb[:, 0:n], [pb], [cbst])
            for c in range(2):
                pcc = fm_block(2 + c)
                g.cp("act", ccs[:, 0:n], pcc[:, 0:n], [pcc], [ccs])
                pcx = fm_block(4 + c)
                g.tt("dve", zst[:, c, 0:n], pcx[:, 0:n], ccs[:, 0:n], ALU.mult, [pcx, ccs], [zst])
            g.ld(CBd.t.ap()[:, :, t0:t0 + n].rearrange("c p t -> p c t"), cbst[:, :, 0:n], [cbst], [CBd])
            g.ld(Zd.t.ap()[:, :, zcol(t0):zcol(t0) + n].rearrange("c p t -> p c t"), zst[:, :, 0:n], [zst], [Zd])
            for (fb, fbs, hidx, isk) in FM_PAIRS:
                px = fm_block(fb)
                g.tt("dve", t1[:, 0:n], px[:, 0:n], cosb[:, 0:n], ALU.mult, [px, cosb], [t1])
                psw = fm_block(fbs)
                g.tt("dve", t2[:, 0:n], psw[:, 0:n], sinb[:, 0:n], ALU.mult, [psw, sinb], [t2])
                if isk:
                    dst = kst[(hidx - 4) // 2]
                else:
                    dst = fst[fcount % 2]
                    fcount += 1
                g.tt("pool", dst[:, 0:n], t1[:, 0:n], t2[:, 0:n], ALU.add, [t1, t2], [dst])
                g.ld(Ffull[hidx * 64:hidx * 64 + 128, t0:t0 + n], dst[:, 0:n], [dst], [Fd])
            ktp = PT[0]
            for a in range(nsub):
                for hp in range(2):
                    g.tr(ktp[:, a * 256 + hp * 128:a * 256 + (hp + 1) * 128], kst[hp][:, a * 128:(a + 1) * 128], identb[:, :],
                         [kst[hp], identb], [ktp])
            g.cp("act", tmst[:, 0:nsub, 0:256], v3(ktp[:, 0:nsub * 256], nsub), [ktp], [tmst])
            for a in range(nsub):
                pa, pbb = PB[4], PB[5]
                for kc in range(8):
                    g.mm(pa[:, 0:512], hT[:, kc, a * 128:(a + 1) * 128], win[:, kc, 3072:3584], kc == 0, kc == 7, [WBIG, hT], [pa])
                for kc in range(8):
                    g.mm(pbb[:, 0:384], hT[:, kc, a * 128:(a + 1) * 128], win[:, kc, 3584:3968], kc == 0, kc == 7, [WBIG, hT], [pbb])
                g.cp("dve", tmst[:, a, 256:512], pa[:, 0:256], [pa], [tmst])
                g.act(tmst[:, a, 512:768], pa[:, 256:512], AF.Silu, [pa], [tmst])
                g.act(tmst[:, a, 768:1024], pbb[:, 0:256], AF.Silu, [pbb], [tmst])
                g.cp("dve", tmst[:, a, 1024:1152], pbb[:, 256:384], [pbb], [tmst])
            g.ld(TMd[t0:t0 + n, :].rearrange("(a p) c -> p a c", p=128), tmst[:, 0:nsub, :], [tmst], [TMd])
            for a in range(nsub):
                ch = chunk_of(t0 + a * 128)
                g.tt("dve", kw[:, :], tmst[:, a, 0:256], WK[:, 0, :], ALU.mult, [tmst, WK], [kw])
                ups = PB[4]
                for h in range(4):
                    g.mm(ups[0:64, h * 64:(h + 1) * 64], kw[:, h * 64:(h + 1) * 64], tmst[:, a, 256 + h * 64:256 + (h + 1) * 64],
                         True, True, [kw, tmst], [ups])
                sp = spst[ch % 2]
                g.cp("act", sp[:, :], Sf[:, :], [Sf], [sp])
                g.ld(SPd.t.ap()[0, ch], sp[:, :], [sp], [SPd])
                g.tt("pool", Sf[:, :], Sf[:, :], DEC[:, 0, :], ALU.mult, [Sf, DEC], [Sf])
                g.tt("dve", Sf[:, :], Sf[:, :], ups[0:64, 0:256], ALU.add, [Sf, ups], [Sf])

    def phase_bwd(l):
        g.reset()
        kvb = [g.carve([128, 512], BF16, name="kvb") for _ in range(2)]
        kw = g.carve([128, 256], BF16, name="kwb")
        spst = [g.carve([64, 256], BF16, name="spstb") for _ in range(2)]
        g.memset("dve", Sb[:, :], 0.0, [Sb])
        order = [33, 32] + list(range(31, -1, -1))
        for i, ch in enumerate(order):
            r0 = ch * 128 if ch < 32 else T + (ch - 32) * 128
            kv = kvb[i % 2]
            g.ld(kv[:, :], TMd[r0:r0 + 128, 0:512], [TMd], [kv])
            g.tt("dve", kw[:, :], kv[:, 0:256], WK[:, 1, :], ALU.mult, [kv, WK], [kw])
            ups = PB[i % 2]
            for h in range(4):
                g.mm(ups[0:64, h * 64:(h + 1) * 64], kw[:, h * 64:(h + 1) * 64], kv[:, 256 + h * 64:256 + (h + 1) * 64],
                     True, True, [kw, kv], [ups])
            sp = spst[i % 2]
            g.cp("act", sp[:, :], Sb[:, :], [Sb], [sp])
            g.ld(SPd.t.ap()[1, ch], sp[:, :], [sp], [SPd])
            g.tt("pool", Sb[:, :], Sb[:, :], DEC[:, 1, :], ALU.mult, [Sb, DEC], [Sb])
            g.tt("dve", Sb[:, :], Sb[:, :], ups[0:64, 0:256], ALU.add, [Sb, ups], [Sb])

    def mix_tiles(with_ctx):
        ts_ = []
        if with_ctx:
            ts_.append((1, T, 256))
        for i in range(T // 256):
            ts_.append((0, i * 256, 256))
        return ts_

    def phase_mix(l, need_ctx):
        g.reset()
        wo_cr = WBIG[:, 0:4096].rearrange("p (c n) -> p c n", c=4)
        wo_at = WBIG[0:64, 8192:16384].rearrange("p (h n) -> p h n", h=8)
        g.ld(wo_cr, w_out.t.ap()[l, 0:512, :].rearrange("(c p) n -> p c n", p=128), [w_out], [(WBIG, 0)], q="pool")
        g.ld(wo_at, w_out.t.ap()[l, 512:1024, :].rearrange("(h p) n -> p h n", p=64), [w_out], [(WBIG, 1)], q="pool")
        Fv = Fd.t.ap().rearrange("(h p) t -> p h t", p=64)
        g.ld(KC[:, :, :], Fv[:, 16:18, T:TT], [Fd], [KC])
        g.ld(VC[:, :, :], TMd[T:TT, 1024:1152].rearrange("(a p) c -> p a c", p=128), [TMd], [VC])
        NT = 256

        def tset():
            return dict(RQK=g.carve([64, 8, NT], BF16, name="RQK"), AQ=g.carve([64, 8, NT], BF16, name="AQ"),
                        AK=g.carve([64, 2, NT + 256], BF16, name="AK"), TMt=g.carve([128, 2, 768], BF16, name="TMt"),
                        AV=g.carve([128, 4, 128], BF16, name="AV"), Zt=g.carve([128, 2, NT + 2], name="Zt"),
                        CBt=g.carve([128, 2, NT], BF16, name="CBt"), SPt=g.carve([64, 2, 2, 256], BF16, name="SPt"),
                        xin=g.carve([128, 2, D], name="xin2"), mconv=g.carve([128, 2, NT], BF16, name="mconv"),
                        matt=g.carve([64, 8, NT], BF16, name="matt"), mret=g.carve([128, 2, NT], BF16, name="mret"))
        TB = [tset(), tset()]
        G1r = g.carve([128, D], name="G1r")
        ctmp = g.carve([128, NT], name="ctmp")
        STf = [g.carve([128, 4, 128], BF16, name="STf") for _ in range(2)]
        STb = [g.carve([128, 4, 128], BF16, name="STb") for _ in range(2)]
        qd = [g.carve([64, 2, 4, 128], BF16, name="qd") for _ in range(2)]
        t1 = [g.carve([128, 512], name="t1m") for _ in range(2)]
        sq = g.carve([128, 512], name="sq")
        st8 = [g.carve([128, 6, 8], name="st8") for _ in range(2)]
        ret = [g.carve([128, 256], BF16, name="ret") for _ in range(2)]
        PTs = [g.carve([128, 4, 128], BF16, name="PTs") for _ in range(3)]
        den = [g.carve([64, 512], name="den") for _ in range(2)]
        tmpy = [g.carve([128, 512], name="tmpy") for _ in range(2)]
        junk = g.carve([128, D], BF16, name="junk2")
        ss = g.carve([128, 4], name="ss2"); rstd = g.carve([128, 4], name="rstd2")
        xn2 = g.carve([128, D], name="xn2"); xn2b = g.carve([128, D], BF16, name="xn2b")
        h2T = g.carve([128, 8, 128], name="h2T")
        ex = [g.carve([16, 128], name="ex") for _ in range(2)]
        tl = mix_tiles(need_ctx)
        info = {}

        def load_tile(ti):
            (s, t0, n) = tl[ti]
            B = TB[ti % 2]
            nsub = n // 128
            g.ld(B["RQK"][:, :, 0:n], Fv[:, 0:8, t0:t0 + n], [Fd], [B["RQK"]])
            g.ld(B["AQ"][:, :, 0:n], Fv[:, 8:16, t0:t0 + n], [Fd], [B["AQ"]])
            koff = 0
            if s == 0:
                lo = max(t0 - 128, 0); hi = min(t0 + n + 128, T)
                koff = t0 - lo
                g.ld(B["AK"][:, :, 0:hi - lo], Fv[:, 16:18, lo:hi], [Fd], [B["AK"]])
                g.ld(B["AV"][:, 0:(hi - lo) // 128, :], TMd[lo:hi, 1024:1152].rearrange("(a p) c -> p a c", p=128), [TMd], [B["AV"]])
            g.ld(B["TMt"][:, 0:nsub, :], TMd[t0:t0 + n, 256:1024].rearrange("(a p) c -> p a c", p=128), [TMd], [B["TMt"]])
            zc = zcol(t0)
            g.ld(B["Zt"][:, :, 0:n + 2], Zd.t.ap()[:, :, zc - 1:zc + n + 1].rearrange("c p t -> p c t"), [Zd], [B["Zt"]])
            g.ld(B["CBt"][:, :, 0:n], CBd.t.ap()[:, :, t0:t0 + n].rearrange("c p t -> p c t"), [CBd], [B["CBt"]])
            g.ld(B["xin"][:, 0:nsub, :], xres[t0:t0 + n, :].rearrange("(a p) d -> p a d", p=128), [xres], [B["xin"]])
            ch0 = chunk_of(t0)
            for d_ in range(2):
                g.ld(B["SPt"][:, d_, 0:nsub, :], SPd.t.ap()[d_, ch0:ch0 + nsub].rearrange("a p f -> p a f"), [SPd], [B["SPt"]])
            info[ti] = koff

        load_tile(0)
        cur_s = None
        cnt = 0
        for ti, (s, t0, n) in enumerate(tl):
            if ti + 1 < len(tl):
                load_tile(ti + 1)
            if s != cur_s:
                g.ld(G1r[:, :], MROWd.t.ap()[0, s:s + 1, :].partition_broadcast(128), [MROWd], [G1r])
                cur_s = s
            B = TB[ti % 2]
            RQK, AQ, AK, TMt, AV, Zt, CBt, SPt, xin, mconv, matt, mret = (B[k] for k in ("RQK", "AQ", "AK", "TMt", "AV", "Zt", "CBt", "SPt", "xin", "mconv", "matt", "mret"))
            koff = info[ti]
            nsub = n // 128
            lat = s == 0
            for c in range(2):
                wb = l * 6
                g.ts("dve", ctmp[:, 0:n], Zt[:, c, 0:n], cw[:, wb + 0 * 2 + c:wb + 0 * 2 + c + 1], None, ALU.mult, None, [Zt, cw], [ctmp])
                g.stt(ctmp[:, 0:n], Zt[:, c, 1:n + 1], cw[:, wb + 1 * 2 + c:wb + 1 * 2 + c + 1], ctmp[:, 0:n], ALU.mult, ALU.add, [Zt, cw, ctmp], [ctmp])
                g.stt(ctmp[:, 0:n], Zt[:, c, 2:n + 2], cw[:, wb + 2 * 2 + c:wb + 2 * 2 + c + 1], ctmp[:, 0:n], ALU.mult, ALU.add, [Zt, cw, ctmp], [ctmp])
                g.tt("dve", mconv[:, c, 0:n], ctmp[:, 0:n], CBt[:, c, 0:n], ALU.mult, [ctmp, CBt], [mconv])
            for a in range(nsub):
                sl = slice(a * 128, (a + 1) * 128)
                stp = PB[0]
                for h in range(4):
                    g.mm(stp[:, h * 128:(h + 1) * 128], RQK[:, 4 + h, sl], RQK[:, h, sl], True, True, [RQK], [stp])
                g.tt("dve", STf[a][:, :, :], v3(stp[:, :], 4), DMf[:, :, :], ALU.mult, [stp, DMf], [STf[a]])
                g.tt("dve", STb[a][:, :, :], v3(stp[:, :], 4), DMb[:, :, :], ALU.mult, [stp, DMb], [STb[a]])
                for d in range(2):
                    g.tt("pool", qd[a][:, d, :, :], RQK[:, 0:4, sl], QD[:, d, :, :], ALU.mult, [RQK, QD], [qd[a]])
                op_ = PB[1]
                for d in range(2):
                    STd = STf[a] if d == 0 else STb[a]
                    for h in range(4):
                        o_ap = op_[:, d * 256 + h * 64:d * 256 + (h + 1) * 64]
                        g.mm(o_ap, STd[:, h, :], TMt[:, a, h * 64:(h + 1) * 64], True, False, [STd, TMt], [op_])
                        g.mm(o_ap, qd[a][:, d, h, :], SPt[:, d, a, h * 64:(h + 1) * 64], False, True, [qd[a], SPt], [op_])
                o3 = v3(op_[:, :], 8)
                s8 = st8[a]
                g.red(s8[:, 0, :], o3, [op_], [s8])
                g.act(sq[:, :], op_[:, :], AF.Square, [op_], [sq])
                g.red(s8[:, 1, :], v3(sq[:, :], 8), [sq], [s8])
                g.ts("dve", s8[:, 2, :], s8[:, 0, :], 1.0 / 64, None, ALU.mult, None, [s8], [s8])
                g.tt("dve", s8[:, 5, :], s8[:, 2, :], s8[:, 2, :], ALU.mult, [s8], [s8])
                g.stt(s8[:, 3, :], s8[:, 1, :], 1.0 / 64, s8[:, 5, :], ALU.mult, ALU.subtract, [s8], [s8])
                g.ts("dve", s8[:, 3, :], s8[:, 3, :], EPS, None, ALU.add, None, [s8], [s8])
                g.act(s8[:, 3, :], s8[:, 3, :], AF.Sqrt, [s8], [s8])
                g.recip(s8[:, 3, :], s8[:, 3, :], [s8], [s8])
                g.stt(s8[:, 4, :], s8[:, 2, :], -1.0, s8[:, 3, :], ALU.mult, ALU.mult, [s8], [s8])
                t13 = v3(t1[a][:, :], 8)
                g.tt("dve", t13, o3, s8[:, 3, :].unsqueeze(2).to_broadcast([128, 8, 64]), ALU.mult, [op_, s8], [t1[a]])
                g.tt("pool", t13, t13, s8[:, 4, :].unsqueeze(2).to_broadcast([128, 8, 64]), ALU.add, [t1[a], s8], [t1[a]])
                g.tt("pool", t1[a][:, :], t1[a][:, :], TMt[:, a, 256:768], ALU.mult, [t1[a], TMt], [t1[a]])
                g.tt("pool", ret[a][:, :], t1[a][:, 0:256], t1[a][:, 256:512], ALU.add, [t1[a]], [ret[a]])
                rtp = PBF[0]
                for c in range(2):
                    g.tr(rtp[:, c * 128:(c + 1) * 128], ret[a][:, c * 128:(c + 1) * 128], identb[:, :], [ret[a], identb], [rtp])
                g.cp("act", mret[:, :, sl], v3(rtp[:, 0:256], 2), [rtp], [mret])
            for a in range(nsub):
                sl = slice(a * 128, (a + 1) * 128)
                qb = t0 // 128 + a
                for kvh in range(2):
                    kbl = []
                    if lat:
                        ka = koff // 128 + a
                        if qb > 0:
                            kbl.append(("prev", AK[:, kvh, (ka - 1) * 128:ka * 128], AV[:, ka - 1, kvh * 64:(kvh + 1) * 64], [AK], [AV]))
                        kbl.append(("same", AK[:, kvh, ka * 128:(ka + 1) * 128], AV[:, ka, kvh * 64:(kvh + 1) * 64], [AK], [AV]))
                        if qb < T // 128 - 1:
                            kbl.append(("next", AK[:, kvh, (ka + 1) * 128:(ka + 2) * 128], AV[:, ka + 1, kvh * 64:(kvh + 1) * 64], [AK], [AV]))
                    for cb_ in range(2):
                        kbl.append(("ctx", KC[:, kvh, cb_ * 128:(cb_ + 1) * 128], VC[:, cb_, kvh * 64:(kvh + 1) * 64], [KC], [VC]))
                    jj = (a * 2 + kvh) % 2
                    aps, bps = PB[4], PB[5]
                    for i, (kind, kT, vv, kR, vR) in enumerate(kbl):
                        sp_ = PB[2 + cnt % 2]
                        pts = PTs[cnt % 3]
                        cnt += 1
                        g.mm(v3(sp_[:, :], 4), kT, AQ[:, kvh * 4:(kvh + 1) * 4, sl], True, True, kR + [AQ], [sp_])
                        g.act(pts[:, :, :], v3(sp_[:, :], 4), AF.Exp, [sp_], [pts], scale=0.125)
                        if kind == "prev":
                            g.tt("pool", pts[:, :, :], pts[:, :, :], Mprev[:, :, :], ALU.mult, [pts, Mprev], [pts])
                        elif kind == "next":
                            g.tt("pool", pts[:, :, :], pts[:, :, :], Mnext[:, :, :], ALU.mult, [pts, Mnext], [pts])
                        g.mm(v3(aps[0:64, :], 4), vv, pts[:, :, :], i == 0, i == len(kbl) - 1, vR + [pts], [aps])
                        g.mm(v3(bps[0:64, :], 4), ones_bf[:, :], pts[:, :, :], i == 0, i == len(kbl) - 1, [ones_bf, pts], [bps])
                    dn = den[jj]
                    g.tt("dve", dn[:, :], bps[0:64, :], SINKE[:, kvh, :], ALU.add, [bps, SINKE], [dn])
                    g.recip(dn[:, :], dn[:, :], [dn], [dn])
                    g.tt("dve", matt[:, kvh * 4:(kvh + 1) * 4, sl], v3(aps[0:64, :], 4), v3(dn[:, :], 4), ALU.mult, [aps, dn], [matt])
            for a in range(nsub):
                sl = slice(a * 128, (a + 1) * 128)
                for nh in range(2):
                    yp = PB[6]
                    ns = slice(nh * 512, (nh + 1) * 512)
                    for c in range(2):
                        g.mm(yp[:, :], mconv[:, c, sl], wo_cr[:, c, ns], c == 0, False, [mconv, (WBIG, 0)], [yp])
                    for c in range(2):
                        g.mm(yp[:, :], mret[:, c, sl], wo_cr[:, 2 + c, ns], False, False, [mret, (WBIG, 0)], [yp])
                    for h in range(8):
                        g.mm(yp[:, :], matt[:, h, sl], wo_at[:, h, ns], False, h == 7, [matt, (WBIG, 1)], [yp])
                    ty = tmpy[nh]
                    g.tt("dve", ty[:, :], yp[:, :], G1r[:, ns], ALU.mult, [yp, G1r], [ty])
                    g.tt("pool", xin[:, a, ns], xin[:, a, ns], ty[:, :], ALU.add, [xin, ty], [xin])
                g.ld(xres[t0 + a * 128:t0 + (a + 1) * 128, :], xin[:, a, :], [xin], [xres])
            for a in range(nsub):
                g.act(junk[:, :], xin[:, a, :], AF.Square, [xin], [junk, ss], accum=ss[:, a:a + 1])
                g.ts("dve", rstd[:, a:a + 1], ss[:, a:a + 1], 1.0 / D, EPS, ALU.mult, ALU.add, [ss], [rstd])
                g.act(rstd[:, a:a + 1], rstd[:, a:a + 1], AF.Sqrt, [rstd], [rstd])
                g.recip(rstd[:, a:a + 1], rstd[:, a:a + 1], [rstd], [rstd])
                g.ts("dve", xn2[:, :], xin[:, a, :], rstd[:, a:a + 1], None, ALU.mult, None, [xin, rstd], [xn2])
                g.cp("pool", xn2b[:, :], xn2[:, :], [xn2], [xn2b])
                g.ld(XN2d[t0 + a * 128:t0 + (a + 1) * 128, :], xn2b[:, :], [xn2b], [XN2d])
                for rnd in range(2):
                    tps = PB[7]
                    for k4 in range(4):
                        kc = rnd * 4 + k4
                        g.tr(tps[:, k4 * 128:(k4 + 1) * 128], xn2[:, kc * 128:(kc + 1) * 128], identf[:, :], [xn2, identf], [tps])
                    for k4 in range(4):
                        kc = rnd * 4 + k4
                        g.act(h2T[:, kc, :], tps[:, k4 * 128:(k4 + 1) * 128], AF.Identity, [tps, AB], [h2T],
                              bias=AB[:, 3, kc, s:s + 1], scale=AB[:, 2, kc, s:s + 1])
                lps = PB[7]
                for kc in range(8):
                    g.mm(lps[0:16, 0:128], Wr[:, kc, :], h2T[:, kc, :], kc == 0, kc == 7, [Wr, h2T], [lps])
                exb = ex[a % 2]
                g.act(exb[:, :], lps[0:16, 0:128], AF.Exp, [lps], [exb])
                g.ld(EXd[:, t0 + a * 128:t0 + (a + 1) * 128], exb[:, :], [exb], [EXd])

    def phase_route(l, need_ctx):
        g.reset()
        EXT = g.carve([16, TT], name="EXT")
        rden = g.carve([16, 512], name="rden")
        COMB = g.carve([32, T], name="COMB")
        PBc = [g.carve([128, T], name="PBc") for _ in range(2)]
        jA = g.carve([128, T], BF16, name="jA")
        jB = g.carve([128, T], BF16, name="jB")
        sc = g.carve([32, 8], name="scr")
        lo, hi, mid, cnt, pp, dd, kvec = (sc[:, i:i + 1] for i in range(7))
        g.ld(EXT[:, :], EXd[:, :], [EXd], [EXT])
        for c0 in range(0, TT, 512):
            w = min(512, TT - c0)
            dps = PB[(c0 // 512) % 2]
            g.mm(dps[0:16, 0:w], ones16[:, :], EXT[:, c0:c0 + w], True, True, [ones16, EXT], [dps])
            g.recip(rden[:, 0:w], dps[0:16, 0:w], [dps], [rden])
            g.tt("dve", EXT[:, c0:c0 + w], EXT[:, c0:c0 + w], rden[:, 0:w], ALU.mult, [EXT, rden], [EXT])
        g.ld(EXd[:, :], EXT[:, :], [EXT], [EXd])
        g.memset("dve", COMB[:, :], 0.0, [COMB])
        g.ld(COMB[0:16, :], EXd[:, 0:T], [EXd], [COMB])
        if need_ctx:
            g.ld(COMB[16:32, 0:C], EXd[:, T:TT], [EXd], [COMB])
        g.memset("dve", sc[:, :], 0.0, [sc])
        g.memset("dve", hi, 1.0, [sc])
        g.memset("dve", kvec, float(CAPC), [sc])
        g.memset("dve", sc[0:16, 6:7], float(CAP), [sc])
        for it in range(36):
            g.tt("dve", mid, lo, hi, ALU.add, [sc], [sc])
            g.ts("dve", mid, mid, 0.5, None, ALU.mult, None, [sc], [sc])
            g.ts("dve", jA[0:32, :], COMB[:, :], mid, 0.0, ALU.is_gt, ALU.add, [COMB, sc], [jA, sc], accum=cnt)
            g.tt("dve", pp, cnt, kvec, ALU.is_gt, [sc], [sc])
            g.tt("dve", dd, mid, lo, ALU.subtract, [sc], [sc])
            g.stt(lo, dd, pp, lo, ALU.mult, ALU.add, [sc], [sc])
            g.tt("dve", dd, hi, mid, ALU.subtract, [sc], [sc])
            g.stt(hi, dd, pp, mid, ALU.mult, ALU.add, [sc], [sc])
        g.ts("dve", jA[0:32, :], COMB[:, :], hi, None, ALU.is_gt, None, [COMB, sc], [jA])
        zr = PBc[1]
        g.memset("dve", zr[0:32, :], 0.0, [zr])
        kb.op("dve", lambda e: e.tensor_tensor_scan(COMB[:, :], jA[0:32, :], zr[0:32, :], 0.0, ALU.add, ALU.add), [jA, zr], [COMB], cost=9000.0)
        g.ld(POSd[:, :], COMB[:, :], [COMB], [POSd])
        for e in range(NE):
            pb = PBc[e % 2]
            g.ld(pb[:, :], POSd[e:e + 1, :].partition_broadcast(128), [POSd], [pb])
            for jt in range(4):
                col = e * 4 + jt
                if jt < 2:
                    g.ts("dve", jA[:, :], pb[:, :], jval[:, jt:jt + 1], 0.0, ALU.is_le, ALU.add,
                         [pb, jval], [jA, LISTF], accum=LISTF[:, col:col + 1])
                else:
                    g.act(jB[:, :], pb[:, :], AF.Sign, [pb, jb], [jB, SACC], bias=jb[:, jt:jt + 1], scale=-1.0,
                          accum=SACC[:, col:col + 1])
        for e in range(NE):
            g.ts("dve", LISTF[:, e * 4 + 2:e * 4 + 4], SACC[:, e * 4 + 2:e * 4 + 4], float(T), 0.5, ALU.add, ALU.mult, [SACC], [LISTF])
        g.cp("dve", LISTI[:, :], LISTF[:, :], [LISTF], [LISTI])
        if need_ctx:
            for e in range(NE):
                pb = PBc[e % 2]
                g.ld(pb[0:32, 0:C], POSd[16 + e:17 + e, 0:C].partition_broadcast(32), [POSd], [pb])
                g.ts("dve", jA[0:32, 0:C], pb[0:32, 0:C], jval[0:32, 0:1], 0.0, ALU.is_le, ALU.add,
                     [pb, jval], [jA, LISTCF], accum=LISTCF[:, e:e + 1])
            g.ts("dve", LISTCF[:, :], LISTCF[:, :], float(T), None, ALU.add, None, [LISTCF], [LISTCF])
            g.cp("dve", LISTCI[:, :], LISTCF[:, :], [LISTCF], [LISTCI])

    def phase_ffn(l, need_ctx):
        g.reset()
        wparts = [WBIG[:, i * 8192:(i + 1) * 8192].rearrange("p (k n) -> p k n", k=8) for i in range(4)]
        G2r = [g.carve([128, D], name="G2r") for _ in range(2)]
        for s in range(2 if need_ctx else 1):
            g.ld(G2r[s][:, :], MROWd.t.ap()[1, s:s + 1, :].partition_broadcast(128), [MROWd], [G2r[s]])
        XG = [[g.carve([128, D], BF16, name="XG") for _ in range(4)] for _ in range(2)]
        XGc = [g.carve([32, D], BF16, name="XGc") for _ in range(2)]
        GT = [g.carve([128, 4], name="GT") for _ in range(2)]
        GTc = [g.carve([32, 1], name="GTc") for _ in range(2)]
        xsT = g.carve([128, 8, 544], BF16, name="xsT")
        actT = g.carve([128, 8, 544], BF16, name="actT")
        sg = [g.carve([128, 544], name="sg") for _ in range(2)]
        YS = [g.carve([128, D], name="YS") for _ in range(2)]
        NCOL = 544 if need_ctx else 512
        exd_flat = EXd.t.ap().rearrange("e (t o) -> (e t) o", o=1)
        wcount = 0

        def load_w(src):
            nonlocal wcount
            part = wcount % 4
            wcount += 1
            wv = wparts[part]
            for hh in range(2):
                g.ld(wv[:, hh * 4:(hh + 1) * 4, :], src[hh * 512:(hh + 1) * 512, :].rearrange("(k p) n -> p k n", p=128),
                     [w_gate, w_up, w_down], [(WBIG, part)], q="pool")
            return wv, (WBIG, part)

        def gathers(e):
            b = e % 2
            for jt in range(4):
                idx = LISTI[:, e * 4 + jt:e * 4 + jt + 1]
                g.gather(XG[b][jt][:, :], XN2d[:, :], idx, [XN2d, LISTI], [XG[b][jt]])
                g.gather(GT[b][:, jt:jt + 1], exd_flat, idx, [EXd, LISTI], [GT[b]], elem_off=e * TT)
            if need_ctx:
                idc = LISTCI[:, e:e + 1]
                g.gather(XGc[b][:, :], XN2d[:, :], idc, [XN2d, LISTCI], [XGc[b]])
                g.gather(GTc[b][:, :], exd_flat, idc, [EXd, LISTCI], [GTc[b]], elem_off=e * TT)

        gathers(0)
        for e in range(NE):
            b = e % 2
            wg, wgp = load_w(w_gate.t.ap()[l, e])
            wu, wup = load_w(w_up.t.ap()[l, e])
            wd, wdp = load_w(w_down.t.ap()[l, e])
            if e + 1 < NE:
                gathers(e + 1)
            for kc in range(8):
                tp = PT[kc % 2]
                for jt in range(4):
                    g.tr(tp[:, jt * 128:(jt + 1) * 128], XG[b][jt][:, kc * 128:(kc + 1) * 128], identb[:, :], [XG[b][jt], identb], [tp])
                g.act(xsT[:, kc, 0:512], tp[:, 0:512], AF.Identity, [tp, AB], [xsT], bias=AB[:, 3, kc, 0:1], scale=AB[:, 2, kc, 0:1])
                if need_ctx:
                    g.tr(tp[:, 512:544], XGc[b][:, kc * 128:(kc + 1) * 128], identb[0:32, 0:32], [XGc[b], identb], [tp])
                    g.act(xsT[:, kc, 512:544], tp[:, 512:544], AF.Identity, [tp, AB], [xsT], bias=AB[:, 3, kc, 1:2], scale=AB[:, 2, kc, 1:2])
            for fb in range(8):
                aps, ups = PB[(fb % 2) * 2], PB[(fb % 2) * 2 + 1]
                apc = PB[4]
                fs = slice(fb * 128, (fb + 1) * 128)
                for kc in range(8):
                    g.mm(aps[:, :], wg[:, kc, fs], xsT[:, kc, 0:512], kc == 0, kc == 7, [wgp, xsT], [aps])
                for kc in range(8):
                    g.mm(ups[:, :], wu[:, kc, fs], xsT[:, kc, 0:512], kc == 0, kc == 7, [wup, xsT], [ups])
                sgb = sg[fb % 2]
                g.act(sgb[:, 0:512], aps[:, :], AF.Silu, [aps], [sgb])
                g.tt("dve", actT[:, fb, 0:512], sgb[:, 0:512], ups[:, :], ALU.mult, [sgb, ups], [actT])
                if need_ctx:
                    for kc in range(8):
                        g.mm(apc[:, 0:32], wg[:, kc, fs], xsT[:, kc, 512:544], kc == 0, kc == 7, [wgp, xsT], [apc])
                    for kc in range(8):
                        g.mm(apc[:, 32:64], wu[:, kc, fs], xsT[:, kc, 512:544], kc == 0, kc == 7, [wup, xsT], [apc])
                    g.act(sgb[:, 512:544], apc[:, 0:32], AF.Silu, [apc], [sgb])
                    g.tt("dve", actT[:, fb, 512:544], sgb[:, 512:544], apc[:, 32:64], ALU.mult, [sgb, apc], [actT])
            for st in range(5 if need_ctx else 4):
                ys = YS[st % 2]
                m = 128 if st < 4 else 32
                for nh in range(2):
                    yp = PB[4 + nh]
                    ns = slice(nh * 512, (nh + 1) * 512)
                    cs = slice(st * 128, st * 128 + m)
                    for fb in range(8):
                        g.mm(yp[0:m, :], actT[:, fb, cs], wd[:, fb, ns], fb == 0, fb == 7, [actT, wdp], [yp])
                    if st < 4:
                        g.stt(ys[:, ns], yp[:, :], GT[b][:, st:st + 1], G2r[0][:, ns], ALU.mult, ALU.mult, [yp, GT[b], G2r[0]], [ys])
                    else:
                        g.stt(ys[0:32, ns], yp[0:32, :], GTc[b][:, 0:1], G2r[1][0:32, ns], ALU.mult, ALU.mult, [yp, GTc[b], G2r[1]], [ys])
                if st < 4:
                    g.scatter_add(xres[:, :], ys[:, :], LISTI[:, e * 4 + st:e * 4 + st + 1], [ys, LISTI], [xres])
                else:
                    g.scatter_add(xres[:, :], ys[0:32, :], LISTCI[:, e:e + 1], [ys, LISTCI], [xres])

    def phase_final():
        g.reset()
        fgr = g.carve([128, D], name="fgr")
        g.ld(fgr[:, :], fg_d[0:1, :].partition_broadcast(128), [fg_d], [fgr])
        xin = [g.carve([128, 4, D], name="xinf") for _ in range(2)]
        ss = g.carve([128, 4], name="ssf"); rstd = g.carve([128, 4], name="rstdf")
        junk = g.carve([128, D], name="junkf")
        for i, (s, t0, n) in enumerate(tiles(False)):
            xt = xin[i % 2]
            g.ld(xt[:, :, :], xres[t0:t0 + n, :].rearrange("(a p) d -> p a d", p=128), [xres], [xt])
            rms_stats(xt, 4, ss, rstd, junk)
            for a in range(4):
                g.stt(xt[:, a, :], xt[:, a, :], rstd[:, a:a + 1], fgr[:, :], ALU.mult, ALU.mult, [xt, rstd, fgr], [xt])
            g.ld(out_d[t0:t0 + n, :].rearrange("(a p) d -> p a d", p=128), xt[:, :, :], [xt], [out_d])

    setup()
    for l in range(n_layers):
        need_ctx = l < DEPTH - 1
        phase_mod(l)
        if stop_after == ("mod", l):
            break
        phase_proj(l)
        phase_bwd(l)
        if stop_after == ("proj", l):
            break
        phase_mix(l, need_ctx)
        if stop_after == ("mix", l):
            break
        phase_route(l, need_ctx)
        if stop_after == ("route", l):
            break
        phase_ffn(l, need_ctx)
    phase_final()
    if debug:
        g.ld(DBGd[:, 0:64], LISTF[:, :], [LISTF], [DBGd])
        g.ld(DBGd[0:32, 64:80], LISTCF[:, :], [LISTCF], [DBGd])
        g.ld(DBGd[:, 128:192], AB.t.ap().rearrange("p a b c -> p (a b c)") if False else AB[:, :, :, :].rearrange("p a b c -> p (a b c)"), [AB], [DBGd])
    kb.finish()
    kb.emit()
    es.close()
    return nc, kb


def _prep_inputs(x, c, ctx, c_ctx, w_mod, b_mod, norm1_g, norm2_g, w_in, conv_w, ret_decay_logit,
                 attn_sink, w_out, w_router, w_gate, w_up, w_down, final_g):
    f = lambda a: np.ascontiguousarray(np.asarray(a, dtype=np.float32))
    cos, sin = _rope_tables()
    perm = _win_perm()
    shared = {
        "w_mod": f(w_mod),
        "bm_t": f(np.asarray(b_mod).reshape(DEPTH, 48, 128).transpose(2, 0, 1)),
        "b_mod": f(b_mod),
        "g1t": f(np.asarray(norm1_g).reshape(DEPTH, 8, 128).transpose(2, 0, 1)),
        "g2t": f(np.asarray(norm2_g).reshape(DEPTH, 8, 128).transpose(2, 0, 1)),
        "w_in": f(np.asarray(w_in)[:, :, perm]),
        "cw_t": f(np.asarray(conv_w).reshape(DEPTH, 3, 2, 128).transpose(3, 0, 1, 2).reshape(128, DEPTH * 6)),
        "rdl": f(np.asarray(ret_decay_logit).reshape(1, 32)),
        "sink": f(np.asarray(attn_sink).reshape(1, 32)),
        "w_out": f(w_out), "w_router": f(w_router),
        "w_gate": f(w_gate), "w_up": f(w_up), "w_down": f(w_down),
        "final_g": f(np.asarray(final_g).reshape(1, D)),
        "rope_cos": cos, "rope_sin": sin,
    }
    maps = []
    for core in range(8):
        b = core % 4
        m = dict(shared)
        m["x"] = f(np.asarray(x)[b])
        m["ctx"] = f(np.asarray(ctx)[b])
        m["cvec"] = f(np.stack([np.asarray(c)[b], np.asarray(c_ctx)], 0))
        maps.append(m)
    return maps


_CACHE = {}


def kernel(**inputs):
    maps = _prep_inputs(**inputs)
    if "nc" not in _CACHE:
        _CACHE["nc"] = build()[0]
    nc = _CACHE["nc"]
    res = run_bass_kernel_spmd(nc, maps, core_ids=list(range(8)))
    out = np.stack([np.asarray(res.results[b]["out"], dtype=np.float32) for b in range(4)], 0)
    return out
```

```python
import os
import numpy as np
from contextlib import ExitStack
import concourse.bass as bass
import concourse.mybir as mybir
from concourse.alu_op_type import AluOpType as ALU
from concourse.bass_utils import run_bass_kernel_spmd

F32 = mybir.dt.float32
BF16 = mybir.dt.bfloat16
I32 = mybir.dt.int32
AF = mybir.ActivationFunctionType
AX = mybir.AxisListType

SEM_LIMIT = 4000
NQ = 16

D = 1024
T = 4096
C = 256
TT = T + C
DEPTH = 4
NE = 16
CAP = 512
CAPC = 32
NW = 2816
NFM = 1920
EPS = 1e-6
ZW = T + C + 4
PE2 = "pool"
SKIP = set(os.environ.get("KSKIP", "").split(","))


class Buf:
    def __init__(self, t, nparts=1, name="", excl=False):
        self.t = t
        self.name = name
        self.excl = excl
        self.lw = [None] * nparts
        self.rd = [[] for _ in range(nparts)]
        self.np_ = nparts

    def __getitem__(self, k):
        return self.t[k]


class BV:
    def __init__(self, parent, ap):
        self.parent = parent
        self.t = ap

    def __getitem__(self, k):
        return self.t[k]


def _acc(x):
    if isinstance(x, BV):
        x = x.parent
    if isinstance(x, Buf):
        return x, range(x.np_)
    b, p = x
    if isinstance(b, BV):
        b = b.parent
    if isinstance(p, int):
        p = [p]
    return b, p


class Op:
    __slots__ = ("id", "eng", "fn", "fence", "deps", "cost", "lat", "dma", "start")

    def __init__(self, id, eng, fn, fence, deps, cost, lat, dma):
        self.id = id; self.eng = eng; self.fn = fn; self.fence = fence
        self.deps = deps; self.cost = cost; self.lat = lat; self.dma = dma; self.start = 0.0


class KB:
    def __init__(self, nc, es, same_engine_sync=True):
        self.nc = nc
        self.es = es
        self.eng = {"pe": nc.tensor, "dve": nc.vector, "act": nc.scalar, "pool": nc.gpsimd, "sp": nc.sync}
        self.nsem = 0
        self.ses = same_engine_sync
        self.n_instr = 0
        self.segs = [[]]
        self.nops = 0
        self._fz = nc.alloc_sbuf_tensor("fencez", [128, 8], F32)
        self.fzb = Buf(self._fz, 2, "fencez")
        self.op("dve", lambda e: e.memset(self._fz[:, :], 0.0), (), [self.fzb])

    def _sem(self, name):
        self.nsem += 1
        return self.es.enter_context(self.nc.semaphore(f"{name}_{self.nsem}"))

    def _deps(self, reads, writes):
        deps = set()
        for x in reads:
            b, ps = _acc(x)
            for p in ps:
                if b.lw[p] is not None:
                    deps.add(b.lw[p])
                if b.excl:
                    deps.update(b.rd[p])
        for x in writes:
            b, ps = _acc(x)
            for p in ps:
                if b.lw[p] is not None:
                    deps.add(b.lw[p])
                deps.update(b.rd[p])
        return deps

    def _record(self, oid, reads, writes):
        for x in reads:
            b, ps = _acc(x)
            for p in ps:
                b.rd[p].append(oid)
        for x in writes:
            b, ps = _acc(x)
            for p in ps:
                b.lw[p] = oid
                b.rd[p] = []

    def _add(self, e, fn, reads, writes, fence, cost, lat, dma):
        if fence:
            writes = list(writes) + [(self.fzb, 0 if e == "dve" else 1)]
        oid = self.nops
        self.nops += 1
        o = Op(oid, e, fn, fence, self._deps(reads, writes), float(cost), float(lat), dma)
        self.segs[-1].append(o)
        self._record(oid, reads, writes)
        self.n_instr += 1
        return oid

    def op(self, e, fn, reads=(), writes=(), fence=False, cost=150.0):
        return self._add(e, fn, reads, writes, fence, cost, cost + 80.0, False)

    def dma(self, q, fn, reads=(), writes=(), nbytes=0):
        issue = 120.0 if q == "sp" else 900.0
        lat = 2200.0 + nbytes / 120.0
        return self._add(q, fn, reads, writes, False, issue, lat, True)

    def barrier(self):
        if self.segs[-1]:
            self.segs.append([])

    def finish(self):
        pass

    def sb(self, name, shape, dt=F32, nparts=1):
        return Buf(self.nc.alloc_sbuf_tensor(name, list(shape), dt), nparts, name)

    def ps(self, name, shape, dt=F32, nparts=1):
        return Buf(self.nc.alloc_psum_tensor(name, list(shape), dt), nparts, name, excl=True)

    def dram(self, name, shape, dt=F32, kind="Internal", nparts=1):
        return Buf(self.nc.dram_tensor(name, list(shape), dt, kind=kind), nparts, name)

    def _schedule(self, seg):
        ids = {o.id for o in seg}
        byid = {o.id: o for o in seg}
        succ = {o.id: [] for o in seg}
        indeg = {}
        for o in seg:
            d = [x for x in o.deps if x in ids]
            indeg[o.id] = len(d)
            for x in d:
                succ[x].append(o.id)
        free = {e: 0.0 for e in self.eng}
        rt = {o.id: 0.0 for o in seg}
        ready = {e: [] for e in self.eng}
        for o in seg:
            if indeg[o.id] == 0:
                ready[o.eng].append(o.id)
        order = []
        n = len(seg)
        while len(order) < n:
            best = None
            for e, lst in ready.items():
                if not lst:
                    continue
                f = free[e]
                c = min(lst, key=lambda i: (max(f, rt[i]), i))
                key = (max(f, rt[c]), c)
                if best is None or key < best[0]:
                    best = (key, e, c)
            (st, _), e, c = best
            o = byid[c]
            ready[e].remove(c)
            o.start = st
            free[e] = st + o.cost
            fin = st + o.lat
            order.append(o)
            for sx in succ[c]:
                if rt[sx] < fin:
                    rt[sx] = fin
                indeg[sx] -= 1
                if indeg[sx] == 0:
                    ready[byid[sx].eng].append(sx)
        return order

    def emit(self):
        esem = {}; ecnt = {}; eretired = []
        dsem = {}; dcnt = {}; dretired = []
        for e in self.eng:
            esem[e] = self._sem("e_" + e); ecnt[e] = 0
        for q in ("sp", "pool"):
            dsem[q] = [self._sem(f"d_{q}{i}") for i in range(NQ)]; dcnt[q] = 0
        waited = {e: {} for e in self.eng}
        prog = {e: [] for e in self.eng}
        event = {}

        def wait(e, evs):
            w = waited[e]
            best = {}
            for (sem, val, src) in evs:
                if src == e and (e == "pe" or not self.ses):
                    continue
                k = id(sem)
                if w.get(k, 0) >= val:
                    continue
                if k not in best or best[k][1] < val:
                    best[k] = (sem, val)
            for k, (sem, val) in best.items():
                prog[e].append(("wait", sem, val))
                w[k] = val

        def all_events():
            evs = list(eretired) + list(dretired)
            for e in self.eng:
                if ecnt[e] > 0:
                    evs.append((esem[e], ecnt[e], e))
            for q in dsem:
                n = dcnt[q]
                for j, sm in enumerate(dsem[q]):
                    cnt = (n - j + NQ - 1) // NQ if n > j else 0
                    if cnt > 0:
                        evs.append((sm, 16 * cnt, "dma"))
            return evs

        self.sched_span = []
        for si, seg in enumerate(self.segs):
            if not seg:
                continue
            if si > 0:
                evs = all_events()
                for e in self.eng:
                    wait(e, evs)
            order = self._schedule(seg)
            self.sched_span.append(max(o.start + o.lat for o in order))
            for o in order:
                e = o.eng
                evs = [event[d] for d in o.deps if d in event]
                if o.dma:
                    i = dcnt[e]
                    if 16 * (i // NQ) + 16 > SEM_LIMIT:
                        for j, sm in enumerate(dsem[e]):
                            cnt = (i - j + NQ - 1) // NQ if i > j else 0
                            if cnt > 0:
                                dretired.append((sm, 16 * cnt, "dma"))
                        dsem[e] = [self._sem(f"d_{e}{k}") for k in range(NQ)]
                        dcnt[e] = 0
                        i = 0
                    sem = dsem[e][i % NQ]
                    prev = 16 * (i // NQ)
                    if prev > 0:
                        evs.append((sem, prev, "dma"))
                    wait(e, evs)
                    prog[e].append(("op", o.fn, False, sem, 16))
                    dcnt[e] += 1
                    event[o.id] = (sem, prev + 16, "dma")
                else:
                    if ecnt[e] >= SEM_LIMIT:
                        eretired.append((esem[e], ecnt[e], e))
                        esem[e] = self._sem("e_" + e); ecnt[e] = 0
                    wait(e, evs)
                    prog[e].append(("op", o.fn, o.fence, esem[e], 1))
                    ecnt[e] += 1
                    event[o.id] = (esem[e], ecnt[e], e)
        wait("sp", all_events())
        self.prog = prog
        with self.nc.Block() as block:
            for e, dec in (("pe", block.tensor), ("dve", block.vector), ("act", block.scalar),
                           ("pool", block.gpsimd), ("sp", block.sync)):
                pr = prog[e]
                if not pr:
                    continue

                def body(eng, pr=pr, e=e):
                    for it in pr:
                        if it[0] == "wait":
                            eng.wait_ge(it[1], it[2])
                        else:
                            _, fn, fence, sem, inc = it
                            ins = fn(eng)
                            if fence:
                                ins = self._fence(e)
                            ins.then_inc(sem, inc)

                dec(body)

    def _fence(self, e):
        if e == "dve":
            return self.nc.vector.tensor_copy(self._fz[0:1, 0:1], self._fz[0:1, 1:2])
        if e == "act":
            return self.nc.scalar.copy(self._fz[0:1, 2:3], self._fz[0:1, 3:4])
        raise ValueError(e)


class G:
    def __init__(self, kb, arena_words):
        self.kb = kb
        self.nc = kb.nc
        self.arena = kb.nc.alloc_sbuf_tensor("arena", [128, arena_words], F32)
        self.aw = arena_words
        self.ap_ = 0
        self.uid = 0
        self.live = []
        self.old = []
        self.gen = 0
        self.use_barrier = False

    def reset(self):
        if self.use_barrier:
            self.kb.barrier()
        self.old = self.old + self.live
        self.live = []
        self.gen += 1
        self.ap_ = 0

    def carve(self, shape, dt=F32, parts=128, nparts=1, name="a"):
        n = 1
        for s in shape[1:]:
            n *= s
        words = n if dt in (F32, I32) else (n + 1) // 2
        words = (words + 1) // 2 * 2
        off = self.ap_
        self.ap_ += words
        assert self.ap_ <= self.aw, f"arena overflow {self.ap_} > {self.aw} ({name})"
        v = self.arena[0:shape[0], off:off + words]
        if dt != F32:
            v = v.bitcast(dt)
        if dt == BF16:
            v = v[:, 0:n]
        else:
            v = v[:, 0:n]
        if len(shape) == 3:
            v = v.rearrange("p (a b) -> p a b", a=shape[1])
        elif len(shape) == 4:
            v = v.rearrange("p (a b c) -> p a b c", a=shape[1], b=shape[2])
        self.uid += 1
        nb = Buf(v, nparts, f"{name}{self.uid}")
        nb.gen = self.gen
        inherit = set()
        for (s_, e_, b_) in self.old:
            if s_ < off + words and off < e_:
                for p in range(b_.np_):
                    if b_.lw[p] is not None:
                        inherit.add(b_.lw[p])
                    inherit.update(b_.rd[p])
        if inherit:
            for p in range(nparts):
                nb.rd[p] = list(inherit)
        self.old = [(s_, e_, b_) for (s_, e_, b_) in self.old if not (off <= s_ and e_ <= off + words)]
        self.live.append((off, off + words, nb))
        return nb

    @staticmethod
    def nf(ap):
        n = 1
        for d in list(ap.shape)[1:]:
            n *= d
        return n

    def _ecost(self, eng, n):
        if eng == "dve":
            return 70.0 + 1.1 * n
        if eng == "act":
            return 110.0 + 0.95 * n
        return 120.0 + 2.3 * n

    def mm(self, out, lhsT, rhs, st, sp, R, W):
        n = self.nf(rhs)
        c = 70.0 + 0.4 * n
        if lhsT.shape[0] < 128 or self.nf(lhsT) < 128:
            c = 70.0 + 0.85 * n
        self.kb.op("pe", lambda e: e.matmul(out, lhsT, rhs, start=st, stop=sp), R, W, cost=c)

    def tr(self, out, in_, ident, R, W):
        self.kb.op("pe", lambda e: e.transpose(out, in_, ident), R, W, cost=180.0)

    def act(self, out, in_, func, R, W, bias=None, scale=None, accum=None):
        kw = {}
        if bias is not None:
            kw["bias"] = bias
        if scale is not None:
            kw["scale"] = scale
        if accum is not None:
            kw["accum_out"] = accum
        c = self._ecost("act", self.nf(out)) + (150.0 if accum is not None else 0.0)
        self.kb.op("act", lambda e: e.activation(out, in_, func, **kw), R, W, fence=accum is not None, cost=c)

    def tt(self, eng, out, a, b, op, R, W):
        self.kb.op(eng, lambda e: e.tensor_tensor(out, a, b, op), R, W, cost=self._ecost(eng, self.nf(out)))

    def ts(self, eng, out, a, s1, s2, op0, op1, R, W, accum=None):
        c = self._ecost(eng, self.nf(out))
        if accum is not None:
            self.kb.op(eng, lambda e: e.tensor_scalar(out, a, s1, None, op0, op1, accum_out=accum), R, W, fence=True, cost=c + 150.0)
        elif op1 is None:
            self.kb.op(eng, lambda e: e.tensor_scalar(out, a, s1, None, op0), R, W, cost=c)
        else:
            self.kb.op(eng, lambda e: e.tensor_scalar(out, a, s1, s2, op0, op1), R, W, cost=c)

    def stt(self, out, in0, scalar, in1, op0, op1, R, W):
        self.kb.op("dve", lambda e: e.scalar_tensor_tensor(out, in0, scalar, in1, op0, op1), R, W, cost=70.0 + 2.0 * self.nf(out))

    def cp(self, eng, out, in_, R, W):
        c = self._ecost(eng, self.nf(out))
        if eng == "act":
            self.kb.op("act", lambda e: e.copy(out, in_), R, W, cost=c)
        else:
            self.kb.op(eng, lambda e: e.tensor_copy(out, in_), R, W, cost=c)

    def memset(self, eng, out, val, W):
        self.kb.op(eng, lambda e: e.memset(out, val), (), W, cost=self._ecost(eng, self.nf(out)))

    def recip(self, out, in_, R, W):
        self.kb.op("dve", lambda e: e.reciprocal(out, in_), R, W, cost=70.0 + 3.0 * self.nf(out))

    def red(self, out, in_, R, W):
        self.kb.op("dve", lambda e: e.tensor_reduce(out, in_, AX.X, ALU.add), R, W, cost=70.0 + 1.1 * self.nf(in_))

    @staticmethod
    def _nbytes(ap):
        n = 1
        for d in list(ap.shape):
            n *= d
        return n * 4

    def ld(self, out, in_, R, W, q="sp", slow=False):
        nb = self._nbytes(out)
        if slow:
            self.kb.dma(q, lambda e: e.dma_start(out=out, in_=in_, allow_slow_non_contiguous=True), R, W, nbytes=nb)
        else:
            self.kb.dma(q, lambda e: e.dma_start(out=out, in_=in_), R, W, nbytes=nb)

    def gather(self, out, src, idx, R, W, elem_off=0):
        self.kb.dma("pool", lambda e: e.indirect_dma_start(
            out=out, out_offset=None, in_=src,
            in_offset=bass.IndirectOffsetOnAxis(ap=idx, axis=0), element_offset=elem_off), R, W, nbytes=self._nbytes(out))

    def scatter_add(self, dst, src, idx, R, W):
        self.kb.dma("pool", lambda e: e.indirect_dma_start(
            out=dst, out_offset=bass.IndirectOffsetOnAxis(ap=idx, axis=0),
            in_=src, in_offset=None, compute_op=ALU.add), R, W, nbytes=2 * self._nbytes(src))


def _win_perm():
    names = ['cb', 'cc', 'cx', 'rq', 'rk', 'rv', 'rgf', 'rgb', 'aq', 'ak', 'av']
    sizes = [256] * 3 + [256] * 5 + [512, 128, 128]
    off = {}
    o = 0
    for n, s in zip(names, sizes):
        off[n] = o
        o += s

    def sw(nh):
        idx = []
        for h in range(nh):
            for i in range(64):
                b4, j = i // 16, i % 16
                idx.append(h * 64 + (b4 ^ 1) * 16 + j)
        return np.array(idx)

    cols = []
    for n in ('cb', 'cc', 'cx', 'rq', 'rk'):
        cols += list(off[n] + np.arange(256))
    cols += list(off['aq'] + np.arange(512))
    cols += list(off['ak'] + np.arange(128))
    cols += list(off['rv'] + np.arange(256)) + list(off['rgf'] + np.arange(256))
    cols += list(off['rgb'] + np.arange(256)) + list(off['av'] + np.arange(128))
    cols = np.array(cols)
    assert cols.shape[0] == NW
    return cols


def _rope_perm():
    p = np.zeros((128, 128), np.float32)
    for m in range(128):
        p[m ^ 16, m] = 1.0
    return p


def _rope_tables():
    t = np.arange(T)
    row = (t // 64).astype(np.float32)
    col = (t % 64).astype(np.float32)
    inv = (np.float32(10000.0) ** (-np.arange(0, 32, 2, dtype=np.float32) / np.float32(32))).astype(np.float32)
    ar = (row[:, None] * inv[None, :]).astype(np.float32)
    ac = (col[:, None] * inv[None, :]).astype(np.float32)
    cr, sr, cc_, sc_ = np.cos(ar).T, np.sin(ar).T, np.cos(ac).T, np.sin(ac).T
    cos64 = np.concatenate([cr, cr, cc_, cc_], 0)
    sin64 = np.concatenate([-sr, sr, -sc_, sc_], 0)
    cos = np.ones((128, TT), np.float32)
    sin = np.zeros((128, TT), np.float32)
    cos[:, :T] = np.concatenate([cos64, cos64], 0)
    sin[:, :T] = np.concatenate([sin64, sin64], 0)
    return cos, sin


def build(n_layers=DEPTH, debug=False, stop_after=None, wl=DEPTH, ne_w=NE):
    nc = bass.Bass("TRN2", target_bir_lowering=False)
    es = ExitStack()
    kb = KB(nc, es)
    skind = "ExternalOutput" if debug else "Internal"

    def din(name, shape, dt=F32):
        return kb.dram(name, shape, dt, kind="ExternalInput")

    x_in = din("x", [T, D]); ctx_in = din("ctx", [C, D]); cvec = din("cvec", [2, D])
    w_mod = din("w_mod", [wl, D, 6 * D]); bm_t = din("bm_t", [128, DEPTH, 48]); b_mod = din("b_mod", [DEPTH, 6 * D])
    g1t_d = din("g1t", [128, DEPTH, 8]); g2t_d = din("g2t", [128, DEPTH, 8])
    w_in = din("w_in", [wl, D, NW]); cw_d = din("cw_t", [128, DEPTH * 6])
    rdl_d = din("rdl", [1, 32]); sink_d = din("sink", [1, 32])
    w_out = din("w_out", [wl, D, D]); w_router = din("w_router", [wl, D, NE])
    w_gate = din("w_gate", [wl, ne_w, D, D]); w_up = din("w_up", [wl, ne_w, D, D]); w_down = din("w_down", [wl, ne_w, D, D])
    fg_d = din("final_g", [1, D]); cos_d = din("rope_cos", [128, TT]); sin_d = din("rope_sin", [128, TT])
    perm_d = din("rope_perm", [128, 128]); kvec_d = din("kvec4", [128, 1])
    out_d = kb.dram("out", [T, D], F32, kind="ExternalOutput")

    xres = kb.dram("xres", [TT, D], F32, kind=skind)
    Fd = kb.dram("Fd", [18 * 64, TT], BF16, kind=skind)
    TMd = kb.dram("TMd", [TT, 1152], BF16, kind=skind)
    Zd = kb.dram("Zd", [2, 128, ZW], F32, kind=skind)
    CBd = kb.dram("CBd", [2, 128, TT], BF16, kind=skind)
    SPd = kb.dram("SPd", [2, 34, 64, 256], BF16, kind=skind)
    XN2d = kb.dram("XN2d", [TT, D], BF16, kind=skind)
    EXd = kb.dram("EXd", [NE, TT], F32, kind=skind)
    POSd = kb.dram("POSd", [32, T], F32, kind=skind)
    MROWd = kb.dram("MROWd", [2, 2, D], F32, kind=skind)
    DBGd = kb.dram("DBGd", [128, 256], F32, kind=skind)

    identb = kb.sb("identb", [128, 128], BF16); identf = kb.sb("identf", [128, 128])
    Dm = kb.sb("Dm", [128, 128]); Dpos = kb.sb("Dpos", [128, 128]); Dneg = kb.sb("Dneg", [128, 128])
    Mge8 = kb.sb("Mge8", [128, 128]); Mle8 = kb.sb("Mle8", [128, 128])
    Mprev = kb.sb("Mprev", [128, 4, 128], BF16); Mnext = kb.sb("Mnext", [128, 4, 128], BF16)
    ones_bf = kb.sb("ones_bf", [128, 64], BF16); ones16 = kb.sb("ones16", [16, 16])
    permb = kb.sb("permb", [128, 128], BF16)
    SS = kb.sb("SS", [128, 128]); kvec4 = kb.sb("kvec4s", [128, 1])
    pcol = kb.sb("pcol", [128, 1]); p127 = kb.sb("p127", [128, 1]); jval = kb.sb("jval", [128, 4]); jb = kb.sb("jb", [128, 4])
    ip1 = kb.sb("ip1", [64, 128]); rev = kb.sb("rev", [64, 128])
    scv = kb.sb("scv", [128, 8, 2]); bm_f = kb.sb("bm_f", [128, DEPTH, 48])
    g1t = kb.sb("g1t_s", [128, DEPTH, 8]); g2t = kb.sb("g2t_s", [128, DEPTH, 8]); cw = kb.sb("cw", [128, DEPTH * 6])
    rdl_b = kb.sb("rdl_b", [128, 32]); sink_b = kb.sb("sink_b", [128, 32])
    AB = kb.sb("AB", [128, 4, 8, 2])
    lg = kb.sb("lg", [128, 8]); wkc = kb.sb("wkc", [128, 8]); dcol = kb.sb("dcol", [128, 8]); se = kb.sb("se", [128, 8])
    DMf = kb.sb("DMf", [128, 4, 128]); DMb = kb.sb("DMb", [128, 4, 128])
    QD = kb.sb("QD", [64, 2, 4, 128], BF16); WK = kb.sb("WK", [128, 2, 256]); DEC = kb.sb("DEC", [64, 2, 256])
    SINKE = kb.sb("SINKE", [64, 2, 512]); Wr = kb.sb("Wr", [128, 8, NE])
    KC = kb.sb("KC", [64, 2, 256], BF16); VC = kb.sb("VC", [128, 2, 128], BF16)
    Sf = kb.sb("Sf", [64, 256]); Sb = kb.sb("Sb", [64, 256])
    LISTF = kb.sb("LISTF", [128, 64], nparts=NE); LISTI = kb.sb("LISTI", [128, 64], I32, nparts=NE)
    LISTCF = kb.sb("LISTCF", [32, 16], nparts=NE); LISTCI = kb.sb("LISTCI", [32, 16], I32, nparts=NE)
    SACC = kb.sb("SACC", [128, 64], nparts=NE)
    tmp128 = kb.sb("tmp128", [128, 128])
    WBIG = kb.sb("WBIG", [128, 32768], BF16, nparts=4)
    PB = [kb.ps(f"PB{i}", [128, 512]) for i in range(8)]
    PBF = [BV(PB[i], PB[i][:, :].bitcast(BF16)) for i in range(8)]
    PT = [PBF[6], PBF[7]]

    g = G(kb, 28672)

    def v3(ap, a):
        return ap.rearrange("p (a b) -> p a b", a=a)

    def setup():
        kb.op("pool", lambda e: e.iota(Dm[:, :], [[1, 128]], base=0, channel_multiplier=-1,
                                       allow_small_or_imprecise_dtypes=True), (), [Dm])
        g.ts("dve", identf[:, :], Dm[:, :], 0.0, None, ALU.is_equal, None, [Dm], [identf])
        g.cp("dve", identb[:, :], identf[:, :], [identf], [identb])
        g.ts("dve", Dpos[:, :], Dm[:, :], 0.0, None, ALU.max, None, [Dm], [Dpos])
        g.ts("dve", Dneg[:, :], Dm[:, :], -1.0, 0.0, ALU.mult, ALU.max, [Dm], [Dneg])
        g.ts("dve", Mge8[:, :], Dm[:, :], 0.0, 0.125, ALU.is_ge, ALU.mult, [Dm], [Mge8])
        g.ts("dve", Mle8[:, :], Dm[:, :], 0.0, 0.125, ALU.is_le, ALU.mult, [Dm], [Mle8])
        for gg in range(4):
            g.ts("dve", Mprev[:, gg, :], Dm[:, :], 0.0, None, ALU.is_le, None, [Dm], [Mprev])
            g.ts("dve", Mnext[:, gg, :], Dm[:, :], 0.0, None, ALU.is_ge, None, [Dm], [Mnext])
        g.memset("dve", ones_bf[:, :], 1.0, [ones_bf])
        g.ld(permb[:, :], perm_d[:, :], [perm_d], [permb], q="pool")
        g.ld(kvec4[:, :], kvec_d[:, :], [kvec_d], [kvec4])
        g.ts("dve", SS[:, :], Dm[:, :], 0.0, None, ALU.is_equal, None, [Dm], [SS])
        for kk in (-96.0, -64.0, -32.0, 32.0, 64.0, 96.0):
            g.ts("dve", tmp128[:, :], Dm[:, :], kk, None, ALU.is_equal, None, [Dm], [tmp128])
            g.tt("dve", SS[:, :], SS[:, :], tmp128[:, :], ALU.add, [SS, tmp128], [SS])
        g.memset("dve", ones16[:, :], 1.0, [ones16])
        kb.op("pool", lambda e: e.iota(pcol[:, :], [[0, 1]], base=0, channel_multiplier=1,
                                       allow_small_or_imprecise_dtypes=True), (), [pcol])
        g.ts("dve", p127[:, :], pcol[:, :], -1.0, 127.0, ALU.mult, ALU.add, [pcol], [p127])
        kb.op("pool", lambda e: e.iota(jval[:, :], [[128, 4]], base=0, channel_multiplier=1,
                                       allow_small_or_imprecise_dtypes=True), (), [jval])
        g.ts("dve", jb[:, :], jval[:, :], 0.5, None, ALU.add, None, [jval], [jb])
        kb.op("pool", lambda e: e.iota(ip1[:, :], [[1, 128]], base=1, channel_multiplier=0,
                                       allow_small_or_imprecise_dtypes=True), (), [ip1])
        kb.op("pool", lambda e: e.iota(rev[:, :], [[-1, 128]], base=128, channel_multiplier=0,
                                       allow_small_or_imprecise_dtypes=True), (), [rev])
        g.memset("dve", LISTF[:, :], 0.0, [LISTF])
        g.memset("dve", LISTCF[:, :], 0.0, [LISTCF])
        g.ld(xres[0:T, :], x_in[:, :], [x_in], [xres])
        g.ld(xres[T:TT, :], ctx_in[:, :], [ctx_in], [xres])
        g.memset("dve", tmp128[:, :], 0.0, [tmp128])
        for c0 in (0, T + 1, T + 2, T + C + 3):
            g.ld(Zd.t.ap()[:, :, c0:c0 + 1].rearrange("c p o -> p c o"), v3(tmp128[:, 0:2], 2), [tmp128], [Zd], slow=True)
        for s_ in range(2):
            g.ld(scv[:, :, s_:s_ + 1], cvec.t.ap()[s_:s_ + 1, :].rearrange("s (kc p) -> p kc s", p=128), [cvec], [scv], slow=True)
        g.act(scv[:, :, :], scv[:, :, :], AF.Silu, [scv], [scv])
        g.ld(bm_f[:, :, :], bm_t[:, :, :], [bm_t], [bm_f])
        g.ld(g1t[:, :, :], g1t_d[:, :, :], [g1t_d], [g1t])
        g.ld(g2t[:, :, :], g2t_d[:, :, :], [g2t_d], [g2t])
        g.ld(cw[:, :], cw_d[:, :], [cw_d], [cw])
        g.ld(rdl_b[:, :], rdl_d[0:1, :].partition_broadcast(128), [rdl_d], [rdl_b])
        g.ld(sink_b[:, :], sink_d[0:1, :].partition_broadcast(128), [sink_d], [sink_b])

    def phase_mod(l):
        g.reset()
        modf_ps = v3(PB[0][:, 0:64], 32)
        rows_ps = PB[1]
        rows_sb = g.carve([2, 2048], name="rows")
        bmr = g.carve([2, 2048], name="bmr")
        modf = g.carve([128, 32, 2], name="modf")
        tmpm = g.carve([128, 8, 2], name="tmpm")
        WM = [g.carve([128, 8, 256], name="WM") for _ in range(2)]
        g.ld(bmr[0:2, 0:1024], b_mod[l:l + 1, 2 * D:3 * D].partition_broadcast(2), [b_mod], [bmr])
        g.ld(bmr[0:2, 1024:2048], b_mod[l:l + 1, 5 * D:6 * D].partition_broadcast(2), [b_mod], [bmr])
        wsrc = w_mod.t.ap()[l].rearrange("(kc p) n -> p kc n", p=128)
        for j in range(24):
            grp = j // 4
            wm = WM[j % 2]
            g.ld(wm[:, :, :], wsrc[:, :, j * 256:(j + 1) * 256], [w_mod], [wm])
            if grp in (0, 1, 3, 4):
                gi = {0: 0, 1: 1, 3: 2, 4: 3}[grp]
                for nb in range(2):
                    fm = gi * 8 + (j % 4) * 2 + nb
                    for kc in range(8):
                        g.mm(modf_ps[:, fm, :], wm[:, kc, nb * 128:(nb + 1) * 128], scv[:, kc, :], kc == 0, kc == 7,
                             [wm, scv], [PB[0]])
            else:
                ri = 0 if grp == 2 else 1
                col = ri * 1024 + (j % 4) * 256
                for kc in range(8):
                    g.mm(rows_ps[0:2, 0:256], scv[:, kc, :], wm[:, kc, :], kc == 0, kc == 7, [wm, scv], [PB[1]])
                g.tt("dve", rows_sb[0:2, col:col + 256], rows_ps[0:2, 0:256], bmr[0:2, col:col + 256], ALU.add,
                     [PB[1], bmr], [rows_sb])
        for gi, Gi in enumerate((0, 1, 3, 4)):
            g.tt("dve", modf[:, gi * 8:(gi + 1) * 8, :], modf_ps[:, gi * 8:(gi + 1) * 8, :],
                 bm_f[:, l, Gi * 8:(Gi + 1) * 8].unsqueeze(2).to_broadcast([128, 8, 2]), ALU.add, [PB[0], bm_f], [modf])
        g.ts("dve", tmpm[:, :, :], modf[:, 8:16, :], 1.0, None, ALU.add, None, [modf], [tmpm])
        g.tt("dve", AB[:, 0, :, :], tmpm[:, :, :], g1t[:, l, :].unsqueeze(2).to_broadcast([128, 8, 2]), ALU.mult,
             [tmpm, g1t], [AB])
        g.cp("dve", AB[:, 1, :, :], modf[:, 0:8, :], [modf], [AB])
        g.ts("dve", tmpm[:, :, :], modf[:, 24:32, :], 1.0, None, ALU.add, None, [modf], [tmpm])
        g.tt("dve", AB[:, 2, :, :], tmpm[:, :, :], g2t[:, l, :].unsqueeze(2).to_broadcast([128, 8, 2]), ALU.mult,
             [tmpm, g2t], [AB])
        g.cp("dve", AB[:, 3, :, :], modf[:, 16:24, :], [modf], [AB])
        g.ld(MROWd.t.ap()[0], rows_sb[0:2, 0:1024], [rows_sb], [MROWd])
        g.ld(MROWd.t.ap()[1], rows_sb[0:2, 1024:2048], [rows_sb], [MROWd])
        g.act(lg[:, :], rdl_b[:, l * 8:(l + 1) * 8], AF.Exp, [rdl_b], [lg], scale=-1.0)
        g.ts("dve", lg[:, :], lg[:, :], 1.0, None, ALU.add, None, [lg], [lg])
        g.act(lg[:, :], lg[:, :], AF.Ln, [lg], [lg])
        g.ts("dve", lg[:, :], lg[:, :], -1.0, None, ALU.mult, None, [lg], [lg])
        for d in range(2):
            for h in range(4):
                dh = d * 4 + h
                g.act(tmp128[:, :], (Dpos if d == 0 else Dneg)[:, :], AF.Exp, [Dpos, Dneg, lg], [tmp128], scale=lg[:, dh:dh + 1])
                g.tt("dve", (DMf if d == 0 else DMb)[:, h, :], tmp128[:, :], (Mge8 if d == 0 else Mle8)[:, :], ALU.mult,
                     [tmp128, Mge8, Mle8], [DMf if d == 0 else DMb])
                g.act(QD[:, d, h, :], (ip1 if d == 0 else rev)[:, :], AF.Exp, [ip1, rev, lg], [QD], scale=lg[0:64, dh:dh + 1])
                g.act(wkc[:, dh:dh + 1], (p127 if d == 0 else pcol)[:, :], AF.Exp, [p127, pcol, lg], [wkc], scale=lg[:, dh:dh + 1])
        g.ts("dve", wkc[:, :], wkc[:, :], 0.125, None, ALU.mult, None, [wkc], [wkc])
        g.act(dcol[:, :], lg[:, :], AF.Exp, [lg], [dcol], scale=128.0)
        g.act(se[:, :], sink_b[:, l * 8:(l + 1) * 8], AF.Exp, [sink_b], [se])
        for d in range(2):
            g.cp("dve", v3(WK[:, d, :], 4), wkc[:, d * 4:(d + 1) * 4].unsqueeze(2).to_broadcast([128, 4, 64]), [wkc], [WK])
            g.cp("dve", v3(DEC[:, d, :], 4), dcol[0:64, d * 4:(d + 1) * 4].unsqueeze(2).to_broadcast([64, 4, 64]), [dcol], [DEC])
            g.cp("dve", v3(SINKE[:, d, :], 4), se[0:64, d * 4:(d + 1) * 4].unsqueeze(2).to_broadcast([64, 4, 128]), [se], [SINKE])
        g.ld(Wr[:, :, :], w_router.t.ap()[l].rearrange("(kc p) e -> p kc e", p=128), [w_router], [Wr])

    FM_PAIRS = [
        (6, 0, False), (7, 2, False), (8, 4, True), (9, 6, True),
        (10, 8, False), (11, 10, False), (12, 12, False), (13, 14, False), (14, 16, False),
    ]

    def zcol(tt):
        return tt + 1 if tt < T else tt + 3

    def chunk_of(tt):
        return tt // 128 if tt < T else 32 + (tt - T) // 128

    def tiles(with_ctx=True):
        ts_ = []
        if with_ctx:
            ts_.append((1, T, C))
        for i in range(T // 512):
            ts_.append((0, i * 512, 512))
        return ts_

    def rms_stats(xin, nsub, ss, rstd, junk):
        for a in range(nsub):
            g.act(junk[:, :], xin[:, a, :], AF.Square, [xin], [junk, ss], accum=ss[:, a:a + 1])
        g.ts("dve", rstd[:, 0:nsub], ss[:, 0:nsub], 1.0 / D, EPS, ALU.mult, ALU.add, [ss], [rstd])
        g.act(rstd[:, 0:nsub], rstd[:, 0:nsub], AF.Sqrt, [rstd], [rstd])
        g.recip(rstd[:, 0:nsub], rstd[:, 0:nsub], [rstd], [rstd])

    def phase_proj(l):
        g.reset()
        win = WBIG[:, 0:8 * NW].rearrange("p (k n) -> p k n", k=8)
        for kc in range(8):
            for hh in range(2):
                g.ld(win[:, kc, hh * 1408:(hh + 1) * 1408],
                     w_in.t.ap()[l, kc * 128:(kc + 1) * 128, hh * 1408:(hh + 1) * 1408], [w_in], [WBIG], q="pool")
        xin = g.carve([128, 4, D], name="xin")
        xn = g.carve([128, 4, D], BF16, name="xn")
        hT = g.carve([128, 8, 512], BF16, name="hT")
        cosb = g.carve([128, 512], name="cosb"); sinb = g.carve([128, 512], name="sinb")
        ss = g.carve([128, 4], name="ss"); rstd = g.carve([128, 4], name="rstd")
        junk = g.carve([128, D], name="junk")
        tmst = g.carve([128, 4, 1152], BF16, name="tmst")
        t1 = g.carve([128, 512], name="t1"); t2 = g.carve([128, 512], name="t2")
        fst = [g.carve([128, 512], BF16, name="fst") for _ in range(2)]
        kst = [g.carve([128, 512], BF16, name="kst") for _ in range(2)]
        xbs = [g.carve([128, 512], BF16, name="xbs") for _ in range(2)]
        ccs = g.carve([128, 512], name="ccs")
        zst = g.carve([128, 2, 512], name="zst")
        cbst = g.carve([128, 2, 512], BF16, name="cbst")
        kw = g.carve([128, 256], BF16, name="kw")
        spst = [g.carve([64, 256], BF16, name="spst") for _ in range(2)]
        g.memset("dve", Sf[:, :], 0.0, [Sf])
        fcount = 0
        pbi = 0
        Ffull = Fd.t.ap()
        for (s, t0, n) in tiles(True):
            nsub = n // 128
            g.ld(xin[:, 0:nsub, :], xres[t0:t0 + n, :].rearrange("(a p) d -> p a d", p=128), [xres], [xin])
            g.ld(cosb[:, 0:n], cos_d[:, t0:t0 + n], [cos_d], [cosb])
            g.ld(sinb[:, 0:n], sin_d[:, t0:t0 + n], [sin_d], [sinb])
            rms_stats(xin, nsub, ss, rstd, junk)
            for a in range(nsub):
                g.ts("dve", xn[:, a, :], xin[:, a, :], rstd[:, a:a + 1], None, ALU.mult, None, [xin, rstd], [xn])
            for kc in range(8):
                tp = PT[kc % 2]
                for a in range(nsub):
                    g.tr(tp[:, a * 128:(a + 1) * 128], xn[:, a, kc * 128:(kc + 1) * 128], identb[:, :], [xn, identb], [tp])
                g.act(hT[:, kc, 0:n], tp[:, 0:n], AF.Identity, [tp, AB], [hT],
                      bias=AB[:, 1, kc, s:s + 1], scale=AB[:, 0, kc, s:s + 1])

            def fm_block(fb):
                nonlocal pbi
                pb = PB[pbi % 4]
                pbi += 1
                for kc in range(8):
                    g.mm(pb[:, 0:n], win[:, kc, fb * 128:(fb + 1) * 128], hT[:, kc, 0:n], kc == 0, kc == 7, [WBIG, hT], [pb])
                return pb

            for c in range(2):
                pb = fm_block(c)
                g.cp("act", cbst[:, c, 0:n], pb[:, 0:n], [pb], [cbst])
            for c in range(2):
                pcc = fm_block(2 + c)
                g.cp("act", ccs[:, 0:n], pcc[:, 0:n], [pcc], [ccs])
                pcx = fm_block(4 + c)
                g.tt("dve", zst[:, c, 0:n], pcx[:, 0:n], ccs[:, 0:n], ALU.mult, [pcx, ccs], [zst])
            g.ld(CBd.t.ap()[:, :, t0:t0 + n].rearrange("c p t -> p c t"), cbst[:, :, 0:n], [cbst], [CBd])
            g.ld(Zd.t.ap()[:, :, zcol(t0):zcol(t0) + n].rearrange("c p t -> p c t"), zst[:, :, 0:n], [zst], [Zd])
            for (fb, hidx, isk) in FM_PAIRS:
                px = fm_block(fb)
                xb = xbs[fcount % 2]
                g.cp("act", xb[:, 0:n], px[:, 0:n], [px], [xb])
                g.tt("dve", t1[:, 0:n], px[:, 0:n], cosb[:, 0:n], ALU.mult, [px, cosb], [t1])
                psw = PB[pbi % 4]
                pbi += 1
                g.mm(psw[:, 0:n], permb[:, :], xb[:, 0:n], True, True, [permb, xb], [psw])
                g.tt("dve", t2[:, 0:n], psw[:, 0:n], sinb[:, 0:n], ALU.mult, [psw, sinb], [t2])
                if isk:
                    dst = kst[(hidx - 4) // 2]
                else:
                    dst = fst[fcount % 2]
                fcount += 1
                g.tt("pool", dst[:, 0:n], t1[:, 0:n], t2[:, 0:n], ALU.add, [t1, t2], [dst])
                g.ld(Ffull[hidx * 64:hidx * 64 + 128, t0:t0 + n], dst[:, 0:n], [dst], [Fd])
            ktp = PT[0]
            for a in range(nsub):
                for hp in range(2):
                    g.tr(ktp[:, a * 256 + hp * 128:a * 256 + (hp + 1) * 128], kst[hp][:, a * 128:(a + 1) * 128], identb[:, :],
                         [kst[hp], identb], [ktp])
            g.cp("act", tmst[:, 0:nsub, 0:256], v3(ktp[:, 0:nsub * 256], nsub), [ktp], [tmst])
            for a in range(nsub):
                pa, pbb = PB[4], PB[5]
                for kc in range(8):
                    g.mm(pa[:, 0:512], hT[:, kc, a * 128:(a + 1) * 128], win[:, kc, NFM:NFM + 512], kc == 0, kc == 7, [WBIG, hT], [pa])
                for kc in range(8):
                    g.mm(pbb[:, 0:384], hT[:, kc, a * 128:(a + 1) * 128], win[:, kc, NFM + 512:NFM + 896], kc == 0, kc == 7, [WBIG, hT], [pbb])
                g.cp("dve", tmst[:, a, 256:512], pa[:, 0:256], [pa], [tmst])
                g.act(tmst[:, a, 512:768], pa[:, 256:512], AF.Silu, [pa], [tmst])
                g.act(tmst[:, a, 768:1024], pbb[:, 0:256], AF.Silu, [pbb], [tmst])
                g.cp("dve", tmst[:, a, 1024:1152], pbb[:, 256:384], [pbb], [tmst])
            g.ld(TMd[t0:t0 + n, :].rearrange("(a p) c -> p a c", p=128), tmst[:, 0:nsub, :], [tmst], [TMd])
            for a in range(nsub):
                ch = chunk_of(t0 + a * 128)
                g.tt("dve", kw[:, :], tmst[:, a, 0:256], WK[:, 0, :], ALU.mult, [tmst, WK], [kw])
                ups = PB[4]
                for h in range(4):
                    g.mm(ups[0:64, h * 64:(h + 1) * 64], kw[:, h * 64:(h + 1) * 64], tmst[:, a, 256 + h * 64:256 + (h + 1) * 64],
                         True, True, [kw, tmst], [ups])
                sp = spst[ch % 2]
                g.cp("act", sp[:, :], Sf[:, :], [Sf], [sp])
                g.ld(SPd.t.ap()[0, ch], sp[:, :], [sp], [SPd])
                g.tt("pool", Sf[:, :], Sf[:, :], DEC[:, 0, :], ALU.mult, [Sf, DEC], [Sf])
                g.tt("dve", Sf[:, :], Sf[:, :], ups[0:64, 0:256], ALU.add, [Sf, ups], [Sf])

    def phase_bwd(l):
        g.reset()
        kvb = [g.carve([128, 512], BF16, name="kvb") for _ in range(2)]
        kw = g.carve([128, 256], BF16, name="kwb")
        spst = [g.carve([64, 256], BF16, name="spstb") for _ in range(2)]
        g.memset("dve", Sb[:, :], 0.0, [Sb])
        order = [33, 32] + list(range(31, -1, -1))
        for i, ch in enumerate(order):
            r0 = ch * 128 if ch < 32 else T + (ch - 32) * 128
            kv = kvb[i % 2]
            g.ld(kv[:, :], TMd[r0:r0 + 128, 0:512], [TMd], [kv])
            g.tt("dve", kw[:, :], kv[:, 0:256], WK[:, 1, :], ALU.mult, [kv, WK], [kw])
            ups = PB[i % 2]
            for h in range(4):
                g.mm(ups[0:64, h * 64:(h + 1) * 64], kw[:, h * 64:(h + 1) * 64], kv[:, 256 + h * 64:256 + (h + 1) * 64],
                     True, True, [kw, kv], [ups])
            sp = spst[i % 2]
            g.cp("act", sp[:, :], Sb[:, :], [Sb], [sp])
            g.ld(SPd.t.ap()[1, ch], sp[:, :], [sp], [SPd])
            g.tt("pool", Sb[:, :], Sb[:, :], DEC[:, 1, :], ALU.mult, [Sb, DEC], [Sb])
            g.tt("dve", Sb[:, :], Sb[:, :], ups[0:64, 0:256], ALU.add, [Sb, ups], [Sb])

    def mix_tiles(with_ctx):
        ts_ = []
        if with_ctx:
            ts_.append((1, T, 256))
        for i in range(T // 256):
            ts_.append((0, i * 256, 256))
        return ts_

    def phase_mix(l, need_ctx):
        g.reset()
        wo_cr = WBIG[:, 0:4096].rearrange("p (c n) -> p c n", c=4)
        wo_at = WBIG[0:64, 8192:16384].rearrange("p (h n) -> p h n", h=8)
        g.ld(wo_cr, w_out.t.ap()[l, 0:512, :].rearrange("(c p) n -> p c n", p=128), [w_out], [(WBIG, 0)], q="pool")
        g.ld(wo_at, w_out.t.ap()[l, 512:1024, :].rearrange("(h p) n -> p h n", p=64), [w_out], [(WBIG, 1)], q="pool")
        Fv = Fd.t.ap().rearrange("(h p) t -> p h t", p=64)
        g.ld(KC[:, :, :], Fv[:, 16:18, T:TT], [Fd], [KC])
        g.ld(VC[:, :, :], TMd[T:TT, 1024:1152].rearrange("(a p) c -> p a c", p=128), [TMd], [VC])
        NT = 256

        def tset():
            return dict(RQK=g.carve([64, 8, NT], BF16, name="RQK"), AQ=g.carve([64, 8, NT], BF16, name="AQ"),
                        AK=g.carve([64, 2, NT + 256], BF16, name="AK"), TMt=g.carve([128, 2, 768], BF16, name="TMt"),
                        AV=g.carve([128, 4, 128], BF16, name="AV"), Zt=g.carve([128, 2, NT + 2], name="Zt"),
                        CBt=g.carve([128, 2, NT], BF16, name="CBt"), SPt=g.carve([64, 2, 2, 256], BF16, name="SPt"),
                        xin=g.carve([128, 2, D], name="xin2"), mconv=g.carve([128, 2, NT], BF16, name="mconv"),
                        matt=g.carve([64, 8, NT], BF16, name="matt"), mret=g.carve([128, 2, NT], BF16, name="mret"))
        TB = [tset(), tset()]
        G1r = g.carve([128, D], name="G1r")
        ctmp = g.carve([128, NT], name="ctmp")
        STf = [g.carve([128, 4, 128], BF16, name="STf") for _ in range(2)]
        STb = [g.carve([128, 4, 128], BF16, name="STb") for _ in range(2)]
        qd = [g.carve([64, 2, 4, 128], BF16, name="qd") for _ in range(2)]
        t1 = [g.carve([128, 512], name="t1m") for _ in range(2)]
        sq = g.carve([128, 512], name="sq")
        st8 = [g.carve([128, 6, 8], name="st8") for _ in range(2)]
        ret = [g.carve([128, 256], BF16, name="ret") for _ in range(2)]
        PTs = [g.carve([128, 4, 128], BF16, name="PTs") for _ in range(3)]
        den = [g.carve([64, 512], name="den") for _ in range(2)]
        tmpy = [g.carve([128, 512], name="tmpy") for _ in range(2)]
        junk = g.carve([128, D], BF16, name="junk2")
        ss = g.carve([128, 4], name="ss2"); rstd = g.carve([128, 4], name="rstd2")
        xn2 = g.carve([128, D], name="xn2"); xn2b = g.carve([128, D], BF16, name="xn2b")
        h2T = g.carve([128, 8, 128], name="h2T")
        ex = [g.carve([16, 128], name="ex") for _ in range(2)]
        tl = mix_tiles(need_ctx)
        info = {}

        def load_tile(ti):
            (s, t0, n) = tl[ti]
            B = TB[ti % 2]
            nsub = n // 128
            g.ld(B["RQK"][:, :, 0:n], Fv[:, 0:8, t0:t0 + n], [Fd], [B["RQK"]])
            g.ld(B["AQ"][:, :, 0:n], Fv[:, 8:16, t0:t0 + n], [Fd], [B["AQ"]])
            koff = 0
            if s == 0:
                lo = max(t0 - 128, 0); hi = min(t0 + n + 128, T)
                koff = t0 - lo
                g.ld(B["AK"][:, :, 0:hi - lo], Fv[:, 16:18, lo:hi], [Fd], [B["AK"]])
                g.ld(B["AV"][:, 0:(hi - lo) // 128, :], TMd[lo:hi, 1024:1152].rearrange("(a p) c -> p a c", p=128), [TMd], [B["AV"]])
            g.ld(B["TMt"][:, 0:nsub, :], TMd[t0:t0 + n, 256:1024].rearrange("(a p) c -> p a c", p=128), [TMd], [B["TMt"]])
            zc = zcol(t0)
            g.ld(B["Zt"][:, :, 0:n + 2], Zd.t.ap()[:, :, zc - 1:zc + n + 1].rearrange("c p t -> p c t"), [Zd], [B["Zt"]])
            g.ld(B["CBt"][:, :, 0:n], CBd.t.ap()[:, :, t0:t0 + n].rearrange("c p t -> p c t"), [CBd], [B["CBt"]])
            g.ld(B["xin"][:, 0:nsub, :], xres[t0:t0 + n, :].rearrange("(a p) d -> p a d", p=128), [xres], [B["xin"]])
            ch0 = chunk_of(t0)
            for d_ in range(2):
                g.ld(B["SPt"][:, d_, 0:nsub, :], SPd.t.ap()[d_, ch0:ch0 + nsub].rearrange("a p f -> p a f"), [SPd], [B["SPt"]])
            info[ti] = koff

        load_tile(0)
        cur_s = None
        cnt = 0
        for ti, (s, t0, n) in enumerate(tl):
            if ti + 1 < len(tl):
                load_tile(ti + 1)
            if s != cur_s:
                g.ld(G1r[:, :], MROWd.t.ap()[0, s:s + 1, :].partition_broadcast(128), [MROWd], [G1r])
                cur_s = s
            B = TB[ti % 2]
            RQK, AQ, AK, TMt, AV, Zt, CBt, SPt, xin, mconv, matt, mret = (B[k] for k in ("RQK", "AQ", "AK", "TMt", "AV", "Zt", "CBt", "SPt", "xin", "mconv", "matt", "mret"))
            koff = info[ti]
            nsub = n // 128
            lat = s == 0
            for c in range(2):
                wb = l * 6
                g.ts("dve", ctmp[:, 0:n], Zt[:, c, 0:n], cw[:, wb + 0 * 2 + c:wb + 0 * 2 + c + 1], None, ALU.mult, None, [Zt, cw], [ctmp])
                g.stt(ctmp[:, 0:n], Zt[:, c, 1:n + 1], cw[:, wb + 1 * 2 + c:wb + 1 * 2 + c + 1], ctmp[:, 0:n], ALU.mult, ALU.add, [Zt, cw, ctmp], [ctmp])
                g.stt(ctmp[:, 0:n], Zt[:, c, 2:n + 2], cw[:, wb + 2 * 2 + c:wb + 2 * 2 + c + 1], ctmp[:, 0:n], ALU.mult, ALU.add, [Zt, cw, ctmp], [ctmp])
                g.tt("dve", mconv[:, c, 0:n], ctmp[:, 0:n], CBt[:, c, 0:n], ALU.mult, [ctmp, CBt], [mconv])
            for a in range(nsub):
                sl = slice(a * 128, (a + 1) * 128)
                stp = PB[0]
                for h in range(4):
                    g.mm(stp[:, h * 128:(h + 1) * 128], RQK[:, 4 + h, sl], RQK[:, h, sl], True, True, [RQK], [stp])
                g.tt("dve", STf[a][:, :, :], v3(stp[:, :], 4), DMf[:, :, :], ALU.mult, [stp, DMf], [STf[a]])
                g.tt("dve", STb[a][:, :, :], v3(stp[:, :], 4), DMb[:, :, :], ALU.mult, [stp, DMb], [STb[a]])
                for d in range(2):
                    g.tt("pool", qd[a][:, d, :, :], RQK[:, 0:4, sl], QD[:, d, :, :], ALU.mult, [RQK, QD], [qd[a]])
                op_ = PB[1]
                for d in range(2):
                    STd = STf[a] if d == 0 else STb[a]
                    for h in range(4):
                        o_ap = op_[:, d * 256 + h * 64:d * 256 + (h + 1) * 64]
                        g.mm(o_ap, STd[:, h, :], TMt[:, a, h * 64:(h + 1) * 64], True, False, [STd, TMt], [op_])
                        g.mm(o_ap, qd[a][:, d, h, :], SPt[:, d, a, h * 64:(h + 1) * 64], False, True, [qd[a], SPt], [op_])
                o3 = v3(op_[:, :], 8)
                s8 = st8[a]
                g.red(s8[:, 0, :], o3, [op_], [s8])
                g.act(sq[:, :], op_[:, :], AF.Square, [op_], [sq])
                g.red(s8[:, 1, :], v3(sq[:, :], 8), [sq], [s8])
                g.ts("dve", s8[:, 2, :], s8[:, 0, :], 1.0 / 64, None, ALU.mult, None, [s8], [s8])
                g.tt("dve", s8[:, 5, :], s8[:, 2, :], s8[:, 2, :], ALU.mult, [s8], [s8])
                g.stt(s8[:, 3, :], s8[:, 1, :], 1.0 / 64, s8[:, 5, :], ALU.mult, ALU.subtract, [s8], [s8])
                g.ts("dve", s8[:, 3, :], s8[:, 3, :], EPS, None, ALU.add, None, [s8], [s8])
                g.act(s8[:, 3, :], s8[:, 3, :], AF.Sqrt, [s8], [s8])
                g.recip(s8[:, 3, :], s8[:, 3, :], [s8], [s8])
                g.stt(s8[:, 4, :], s8[:, 2, :], -1.0, s8[:, 3, :], ALU.mult, ALU.mult, [s8], [s8])
                t13 = v3(t1[a][:, :], 8)
                g.tt("dve", t13, o3, s8[:, 3, :].unsqueeze(2).to_broadcast([128, 8, 64]), ALU.mult, [op_, s8], [t1[a]])
                g.tt("pool", t13, t13, s8[:, 4, :].unsqueeze(2).to_broadcast([128, 8, 64]), ALU.add, [t1[a], s8], [t1[a]])
                g.tt("pool", t1[a][:, :], t1[a][:, :], TMt[:, a, 256:768], ALU.mult, [t1[a], TMt], [t1[a]])
                g.tt("pool", ret[a][:, :], t1[a][:, 0:256], t1[a][:, 256:512], ALU.add, [t1[a]], [ret[a]])
                rtp = PBF[0]
                for c in range(2):
                    g.tr(rtp[:, c * 128:(c + 1) * 128], ret[a][:, c * 128:(c + 1) * 128], identb[:, :], [ret[a], identb], [rtp])
                g.cp("act", mret[:, :, sl], v3(rtp[:, 0:256], 2), [rtp], [mret])
            for a in range(nsub):
                sl = slice(a * 128, (a + 1) * 128)
                qb = t0 // 128 + a
                for kvh in range(2):
                    kbl = []
                    if lat:
                        ka = koff // 128 + a
                        if qb > 0:
                            kbl.append(("prev", AK[:, kvh, (ka - 1) * 128:ka * 128], AV[:, ka - 1, kvh * 64:(kvh + 1) * 64], [AK], [AV]))
                        kbl.append(("same", AK[:, kvh, ka * 128:(ka + 1) * 128], AV[:, ka, kvh * 64:(kvh + 1) * 64], [AK], [AV]))
                        if qb < T // 128 - 1:
                            kbl.append(("next", AK[:, kvh, (ka + 1) * 128:(ka + 2) * 128], AV[:, ka + 1, kvh * 64:(kvh + 1) * 64], [AK], [AV]))
                    for cb_ in range(2):
                        kbl.append(("ctx", KC[:, kvh, cb_ * 128:(cb_ + 1) * 128], VC[:, cb_, kvh * 64:(kvh + 1) * 64], [KC], [VC]))
                    jj = (a * 2 + kvh) % 2
                    aps, bps = PB[4], PB[5]
                    for i, (kind, kT, vv, kR, vR) in enumerate(kbl):
                        sp_ = PB[2 + cnt % 2]
                        pts = PTs[cnt % 3]
                        cnt += 1
                        g.mm(v3(sp_[:, :], 4), kT, AQ[:, kvh * 4:(kvh + 1) * 4, sl], True, True, kR + [AQ], [sp_])
                        g.act(pts[:, :, :], v3(sp_[:, :], 4), AF.Exp, [sp_], [pts], scale=0.125)
                        if kind == "prev":
                            g.tt("dve", pts[:, :, :], pts[:, :, :], Mprev[:, :, :], ALU.mult, [pts, Mprev], [pts])
                        elif kind == "next":
                            g.tt("dve", pts[:, :, :], pts[:, :, :], Mnext[:, :, :], ALU.mult, [pts, Mnext], [pts])
                        g.mm(v3(aps[0:64, :], 4), vv, pts[:, :, :], i == 0, i == len(kbl) - 1, vR + [pts], [aps])
                        g.mm(v3(bps[0:64, :], 4), ones_bf[:, :], pts[:, :, :], i == 0, i == len(kbl) - 1, [ones_bf, pts], [bps])
                    dn = den[jj]
                    g.tt("dve", dn[:, :], bps[0:64, :], SINKE[:, kvh, :], ALU.add, [bps, SINKE], [dn])
                    g.recip(dn[:, :], dn[:, :], [dn], [dn])
                    g.tt("dve", matt[:, kvh * 4:(kvh + 1) * 4, sl], v3(aps[0:64, :], 4), v3(dn[:, :], 4), ALU.mult, [aps, dn], [matt])
            for a in range(nsub):
                sl = slice(a * 128, (a + 1) * 128)
                for nh in range(2):
                    yp = PB[6]
                    ns = slice(nh * 512, (nh + 1) * 512)
                    for c in range(2):
                        g.mm(yp[:, :], mconv[:, c, sl], wo_cr[:, c, ns], c == 0, False, [mconv, (WBIG, 0)], [yp])
                    for c in range(2):
                        g.mm(yp[:, :], mret[:, c, sl], wo_cr[:, 2 + c, ns], False, False, [mret, (WBIG, 0)], [yp])
                    for h in range(8):
                        g.mm(yp[:, :], matt[:, h, sl], wo_at[:, h, ns], False, h == 7, [matt, (WBIG, 1)], [yp])
                    ty = tmpy[nh]
                    g.tt("dve", ty[:, :], yp[:, :], G1r[:, ns], ALU.mult, [yp, G1r], [ty])
                    g.tt("pool", xin[:, a, ns], xin[:, a, ns], ty[:, :], ALU.add, [xin, ty], [xin])
                g.ld(xres[t0 + a * 128:t0 + (a + 1) * 128, :], xin[:, a, :], [xin], [xres])
            for a in range(nsub):
                g.act(junk[:, :], xin[:, a, :], AF.Square, [xin], [junk, ss], accum=ss[:, a:a + 1])
                g.ts("dve", rstd[:, a:a + 1], ss[:, a:a + 1], 1.0 / D, EPS, ALU.mult, ALU.add, [ss], [rstd])
                g.act(rstd[:, a:a + 1], rstd[:, a:a + 1], AF.Sqrt, [rstd], [rstd])
                g.recip(rstd[:, a:a + 1], rstd[:, a:a + 1], [rstd], [rstd])
                g.ts("dve", xn2[:, :], xin[:, a, :], rstd[:, a:a + 1], None, ALU.mult, None, [xin, rstd], [xn2])
                g.cp("act", xn2b[:, :], xn2[:, :], [xn2], [xn2b])
                g.ld(XN2d[t0 + a * 128:t0 + (a + 1) * 128, :], xn2b[:, :], [xn2b], [XN2d])
                for rnd in range(2):
                    tps = PB[7]
                    for k4 in range(4):
                        kc = rnd * 4 + k4
                        g.tr(tps[:, k4 * 128:(k4 + 1) * 128], xn2[:, kc * 128:(kc + 1) * 128], identf[:, :], [xn2, identf], [tps])
                    for k4 in range(4):
                        kc = rnd * 4 + k4
                        g.act(h2T[:, kc, :], tps[:, k4 * 128:(k4 + 1) * 128], AF.Identity, [tps, AB], [h2T],
                              bias=AB[:, 3, kc, s:s + 1], scale=AB[:, 2, kc, s:s + 1])
                lps = PB[7]
                for kc in range(8):
                    g.mm(lps[0:16, 0:128], Wr[:, kc, :], h2T[:, kc, :], kc == 0, kc == 7, [Wr, h2T], [lps])
                exb = ex[a % 2]
                g.act(exb[:, :], lps[0:16, 0:128], AF.Exp, [lps], [exb])
                g.ld(EXd[:, t0 + a * 128:t0 + (a + 1) * 128], exb[:, :], [exb], [EXd])

    def phase_route(l, need_ctx):
        g.reset()
        EXT = g.carve([16, TT], name="EXT")
        rden = g.carve([16, 512], name="rden")
        COMB = g.carve([32, T], name="COMB")
        PBc = [g.carve([128, T], name="PBc") for _ in range(2)]
        jA = g.carve([128, T], BF16, name="jA")
        jB = g.carve([128, T], BF16, name="jB")
        PBcc = [g.carve([32, C], name="PBcc") for _ in range(2)]
        jC = g.carve([32, C], BF16, name="jC")
        sc = g.carve([32, 8], name="scr")
        lo, hi, mid, cnt, pp, dd, kvec = (sc[:, i:i + 1] for i in range(7))
        g.ld(EXT[:, :], EXd[:, :], [EXd], [EXT])
        for c0 in range(0, TT, 512):
            w = min(512, TT - c0)
            dps = PB[(c0 // 512) % 2]
            g.mm(dps[0:16, 0:w], ones16[:, :], EXT[:, c0:c0 + w], True, True, [ones16, EXT], [dps])
            g.recip(rden[:, 0:w], dps[0:16, 0:w], [dps], [rden])
            g.tt("dve", EXT[:, c0:c0 + w], EXT[:, c0:c0 + w], rden[:, 0:w], ALU.mult, [EXT, rden], [EXT])
        g.ld(EXd[:, :], EXT[:, :], [EXT], [EXd])
        g.memset("dve", COMB[:, :], 0.0, [COMB])
        g.ld(COMB[0:16, :], EXd[:, 0:T], [EXd], [COMB])
        if need_ctx:
            g.ld(COMB[16:32, 0:C], EXd[:, T:TT], [EXd], [COMB])
        COMB4 = g.carve([128, 1024], name="COMB4")
        sc4 = g.carve([128, 8], name="sc4")
        lo4, hi4, mid4, cnt4, pp4, dd4 = (sc4[:, i:i + 1] for i in range(6))
        g.memset("dve", COMB4[:, :], 0.0, [COMB4])
        for q4 in range(4):
            g.ld(COMB4[q4 * 32:q4 * 32 + 16, :], EXd[:, q4 * 1024:(q4 + 1) * 1024], [EXd], [COMB4])
            if need_ctx:
                g.ld(COMB4[q4 * 32 + 16:q4 * 32 + 32, 0:64], EXd[:, T + q4 * 64:T + (q4 + 1) * 64], [EXd], [COMB4])
        g.memset("dve", sc4[:, :], 0.0, [sc4])
        g.memset("dve", hi4, 1.0, [sc4])
        cps = PB[2]
        for it in range(34):
            g.tt("dve", mid4, lo4, hi4, ALU.add, [sc4], [sc4])
            g.ts("dve", mid4, mid4, 0.5, None, ALU.mult, None, [sc4], [sc4])
            g.ts("dve", jA[:, 0:1024], COMB4[:, :], mid4, 0.0, ALU.is_gt, ALU.add, [COMB4, sc4], [jA, sc4], accum=cnt4)
            g.mm(cps[:, 0:2], SS[:, :], sc4[:, 3:5], True, True, [SS, sc4], [cps])
            g.tt("dve", pp4, cps[:, 0:1], kvec4[:, :], ALU.is_gt, [cps, kvec4], [sc4])
            g.tt("dve", dd4, mid4, lo4, ALU.subtract, [sc4], [sc4])
            g.stt(lo4, dd4, pp4, lo4, ALU.mult, ALU.add, [sc4], [sc4])
            g.tt("dve", dd4, hi4, mid4, ALU.subtract, [sc4], [sc4])
            g.stt(hi4, dd4, pp4, mid4, ALU.mult, ALU.add, [sc4], [sc4])
        g.cp("dve", hi, sc4[0:32, 1:2], [sc4], [sc])
        g.ts("dve", jA[0:32, :], COMB[:, :], hi, None, ALU.is_gt, None, [COMB, sc], [jA])
        zr = PBc[1]
        g.memset("dve", zr[0:32, :], 0.0, [zr])
        kb.op("dve", lambda e: e.tensor_tensor_scan(COMB[:, :], jA[0:32, :], zr[0:32, :], 0.0, ALU.add, ALU.add), [jA, zr], [COMB], cost=9000.0)
        g.ld(POSd[:, :], COMB[:, :], [COMB], [POSd])
        for e in range(NE):
            pb = PBc[e % 2]
            g.ld(pb[:, :], POSd[e:e + 1, :].partition_broadcast(128), [POSd], [pb])
            for jt in range(4):
                col = e * 4 + jt
                if jt < 2:
                    g.ts("dve", jA[:, :], pb[:, :], jval[:, jt:jt + 1], 0.0, ALU.is_le, ALU.add,
                         [pb, jval], [jA, (LISTF, e)], accum=LISTF[:, col:col + 1])
                else:
                    g.act(jB[:, :], pb[:, :], AF.Sign, [pb, jb], [jB, (SACC, e)], bias=jb[:, jt:jt + 1], scale=-1.0,
                          accum=SACC[:, col:col + 1])
            g.ts("dve", LISTF[:, e * 4 + 2:e * 4 + 4], SACC[:, e * 4 + 2:e * 4 + 4], float(T), 0.5, ALU.add, ALU.mult, [(SACC, e)], [(LISTF, e)])
            g.cp("dve", LISTI[:, e * 4:e * 4 + 4], LISTF[:, e * 4:e * 4 + 4], [(LISTF, e)], [(LISTI, e)])
            if need_ctx:
                pc = PBcc[e % 2]
                g.ld(pc[0:32, 0:C], POSd[16 + e:17 + e, 0:C].partition_broadcast(32), [POSd], [pc])
                g.ts("dve", jC[0:32, 0:C], pc[0:32, 0:C], jval[0:32, 0:1], 0.0, ALU.is_le, ALU.add,
                     [pc, jval], [jC, (LISTCF, e)], accum=LISTCF[:, e:e + 1])
                g.ts("dve", LISTCF[:, e:e + 1], LISTCF[:, e:e + 1], float(T), None, ALU.add, None, [(LISTCF, e)], [(LISTCF, e)])
                g.cp("dve", LISTCI[:, e:e + 1], LISTCF[:, e:e + 1], [(LISTCF, e)], [(LISTCI, e)])

    def phase_ffn(l, need_ctx):
        g.reset()
        wparts = [WBIG[:, i * 8192:(i + 1) * 8192].rearrange("p (k n) -> p k n", k=8) for i in range(4)]
        G2r = [g.carve([128, D], name="G2r") for _ in range(2)]
        for s in range(2 if need_ctx else 1):
            g.ld(G2r[s][:, :], MROWd.t.ap()[1, s:s + 1, :].partition_broadcast(128), [MROWd], [G2r[s]])
        XG = [[g.carve([128, D], BF16, name="XG") for _ in range(4)] for _ in range(2)]
        XGc = [g.carve([32, D], BF16, name="XGc") for _ in range(2)]
        GT = [g.carve([128, 4], name="GT") for _ in range(2)]
        GTc = [g.carve([32, 1], name="GTc") for _ in range(2)]
        xsT = g.carve([128, 8, 544], BF16, name="xsT")
        actT = g.carve([128, 8, 544], BF16, name="actT")
        sg = [g.carve([128, 544], name="sg") for _ in range(2)]
        YS = [g.carve([128, D], name="YS") for _ in range(2)]
        NCOL = 544 if need_ctx else 512
        exd_flat = EXd.t.ap().rearrange("e (t o) -> (e t) o", o=1)
        wcount = 0

        def load_w(src):
            nonlocal wcount
            part = wcount % 4
            wcount += 1
            wv = wparts[part]
            for hh in range(2):
                g.ld(wv[:, hh * 4:(hh + 1) * 4, :], src[hh * 512:(hh + 1) * 512, :].rearrange("(k p) n -> p k n", p=128),
                     [w_gate, w_up, w_down], [(WBIG, part)], q="pool")
            return wv, (WBIG, part)

        def gathers(e):
            b = e % 2
            for jt in range(4):
                idx = LISTI[:, e * 4 + jt:e * 4 + jt + 1]
                g.gather(XG[b][jt][:, :], XN2d[:, :], idx, [XN2d, (LISTI, e)], [XG[b][jt]])
                g.gather(GT[b][:, jt:jt + 1], exd_flat, idx, [EXd, (LISTI, e)], [GT[b]], elem_off=e * TT)
            if need_ctx:
                idc = LISTCI[:, e:e + 1]
                g.gather(XGc[b][:, :], XN2d[:, :], idc, [XN2d, (LISTCI, e)], [XGc[b]])
                g.gather(GTc[b][:, :], exd_flat, idc, [EXd, (LISTCI, e)], [GTc[b]], elem_off=e * TT)

        gathers(0)
        for e in range(NE):
            b = e % 2
            wg, wgp = load_w(w_gate.t.ap()[l, e])
            wu, wup = load_w(w_up.t.ap()[l, e])
            wd, wdp = load_w(w_down.t.ap()[l, e])
            if e + 1 < NE:
                gathers(e + 1)
            for kc in range(8):
                tp = PT[kc % 2]
                for jt in range(4):
                    g.tr(tp[:, jt * 128:(jt + 1) * 128], XG[b][jt][:, kc * 128:(kc + 1) * 128], identb[:, :], [XG[b][jt], identb], [tp])
                g.act(xsT[:, kc, 0:512], tp[:, 0:512], AF.Identity, [tp, AB], [xsT], bias=AB[:, 3, kc, 0:1], scale=AB[:, 2, kc, 0:1])
                if need_ctx:
                    g.tr(tp[:, 512:544], XGc[b][:, kc * 128:(kc + 1) * 128], identb[0:32, 0:32], [XGc[b], identb], [tp])
                    g.act(xsT[:, kc, 512:544], tp[:, 512:544], AF.Identity, [tp, AB], [xsT], bias=AB[:, 3, kc, 1:2], scale=AB[:, 2, kc, 1:2])
            for fb in range(8):
                aps, ups = PB[(fb % 2) * 2], PB[(fb % 2) * 2 + 1]
                apc = PB[4]
                fs = slice(fb * 128, (fb + 1) * 128)
                for kc in range(8):
                    g.mm(aps[:, :], wg[:, kc, fs], xsT[:, kc, 0:512], kc == 0, kc == 7, [wgp, xsT], [aps])
                for kc in range(8):
                    g.mm(ups[:, :], wu[:, kc, fs], xsT[:, kc, 0:512], kc == 0, kc == 7, [wup, xsT], [ups])
                sgb = sg[fb % 2]
                g.act(sgb[:, 0:512], aps[:, :], AF.Silu, [aps], [sgb])
                g.tt("dve", actT[:, fb, 0:512], sgb[:, 0:512], ups[:, :], ALU.mult, [sgb, ups], [actT])
                if need_ctx:
                    for kc in range(8):
                        g.mm(apc[:, 0:32], wg[:, kc, fs], xsT[:, kc, 512:544], kc == 0, kc == 7, [wgp, xsT], [apc])
                    for kc in range(8):
                        g.mm(apc[:, 32:64], wu[:, kc, fs], xsT[:, kc, 512:544], kc == 0, kc == 7, [wup, xsT], [apc])
                    g.act(sgb[:, 512:544], apc[:, 0:32], AF.Silu, [apc], [sgb])
                    g.tt("dve", actT[:, fb, 512:544], sgb[:, 512:544], apc[:, 32:64], ALU.mult, [sgb, apc], [actT])
            for st in range(5 if need_ctx else 4):
                ys = YS[st % 2]
                m = 128 if st < 4 else 32
                for nh in range(2):
                    yp = PB[4 + nh]
                    ns = slice(nh * 512, (nh + 1) * 512)
                    cs = slice(st * 128, st * 128 + m)
                    for fb in range(8):
                        g.mm(yp[0:m, :], actT[:, fb, cs], wd[:, fb, ns], fb == 0, fb == 7, [actT, wdp], [yp])
                    if st < 4:
                        g.stt(ys[:, ns], yp[:, :], GT[b][:, st:st + 1], G2r[0][:, ns], ALU.mult, ALU.mult, [yp, GT[b], G2r[0]], [ys])
                    else:
                        g.stt(ys[0:32, ns], yp[0:32, :], GTc[b][:, 0:1], G2r[1][0:32, ns], ALU.mult, ALU.mult, [yp, GTc[b], G2r[1]], [ys])
                if st < 4:
                    g.scatter_add(xres[:, :], ys[:, :], LISTI[:, e * 4 + st:e * 4 + st + 1], [ys, (LISTI, e)], [xres])
                else:
                    g.scatter_add(xres[:, :], ys[0:32, :], LISTCI[:, e:e + 1], [ys, (LISTCI, e)], [xres])

    def phase_final():
        g.reset()
        fgr = g.carve([128, D], name="fgr")
        g.ld(fgr[:, :], fg_d[0:1, :].partition_broadcast(128), [fg_d], [fgr])
        xin = [g.carve([128, 4, D], name="xinf") for _ in range(2)]
        ss = g.carve([128, 4], name="ssf"); rstd = g.carve([128, 4], name="rstdf")
        junk = g.carve([128, D], name="junkf")
        for i, (s, t0, n) in enumerate(tiles(False)):
            xt = xin[i % 2]
            g.ld(xt[:, :, :], xres[t0:t0 + n, :].rearrange("(a p) d -> p a d", p=128), [xres], [xt])
            rms_stats(xt, 4, ss, rstd, junk)
            for a in range(4):
                g.stt(xt[:, a, :], xt[:, a, :], rstd[:, a:a + 1], fgr[:, :], ALU.mult, ALU.mult, [xt, rstd, fgr], [xt])
            g.ld(out_d[t0:t0 + n, :].rearrange("(a p) d -> p a d", p=128), xt[:, :, :], [xt], [out_d])

    setup()
    for l in range(n_layers):
        need_ctx = l < DEPTH - 1
        phase_mod(l)
        if stop_after == ("mod", l):
            break
        phase_proj(l)
        phase_bwd(l)
        if stop_after == ("proj", l):
            break
        phase_mix(l, need_ctx)
        if stop_after == ("mix", l):
            break
        phase_route(l, need_ctx)
        if stop_after == ("route", l):
            break
        phase_ffn(l, need_ctx)
    phase_final()
    if debug:
        g.ld(DBGd[:, 0:64], LISTF[:, :], [LISTF], [DBGd])
        g.ld(DBGd[0:32, 64:80], LISTCF[:, :], [LISTCF], [DBGd])
        g.ld(DBGd[:, 128:192], AB.t.ap().rearrange("p a b c -> p (a b c)") if False else AB[:, :, :, :].rearrange("p a b c -> p (a b c)"), [AB], [DBGd])
    kb.finish()
    kb.emit()
    es.close()
    return nc, kb


def _prep_inputs(x, c, ctx, c_ctx, w_mod, b_mod, norm1_g, norm2_g, w_in, conv_w, ret_decay_logit,
                 attn_sink, w_out, w_router, w_gate, w_up, w_down, final_g):
    f = lambda a: np.ascontiguousarray(np.asarray(a, dtype=np.float32))
    cos, sin = _rope_tables()
    perm = _win_perm()
    shared = {
        "w_mod": f(w_mod),
        "bm_t": f(np.asarray(b_mod).reshape(DEPTH, 48, 128).transpose(2, 0, 1)),
        "b_mod": f(b_mod),
        "g1t": f(np.asarray(norm1_g).reshape(DEPTH, 8, 128).transpose(2, 0, 1)),
        "g2t": f(np.asarray(norm2_g).reshape(DEPTH, 8, 128).transpose(2, 0, 1)),
        "w_in": f(np.asarray(w_in)[:, :, perm]),
        "cw_t": f(np.asarray(conv_w).reshape(DEPTH, 3, 2, 128).transpose(3, 0, 1, 2).reshape(128, DEPTH * 6)),
        "rdl": f(np.asarray(ret_decay_logit).reshape(1, 32)),
        "sink": f(np.asarray(attn_sink).reshape(1, 32)),
        "w_out": f(w_out), "w_router": f(w_router),
        "w_gate": f(w_gate), "w_up": f(w_up), "w_down": f(w_down),
        "final_g": f(np.asarray(final_g).reshape(1, D)),
        "rope_cos": cos, "rope_sin": sin, "rope_perm": _rope_perm(),
        "kvec4": np.where((np.arange(128) % 32) < 16, float(CAP), float(CAPC)).astype(np.float32).reshape(128, 1),
    }
    maps = []
    for core in range(8):
        b = core % 4
        m = dict(shared)
        m["x"] = f(np.asarray(x)[b])
        m["ctx"] = f(np.asarray(ctx)[b])
        m["cvec"] = f(np.stack([np.asarray(c)[b], np.asarray(c_ctx)], 0))
        maps.append(m)
    return maps


_CACHE = {}


def kernel(**inputs):
    maps = _prep_inputs(**inputs)
    if "nc" not in _CACHE:
        _CACHE["nc"] = build()[0]
    nc = _CACHE["nc"]
    res = run_bass_kernel_spmd(nc, maps, core_ids=list(range(8)))
    out = np.stack([np.asarray(res.results[b]["out"], dtype=np.float32) for b in range(4)], 0)
    return out
```

```python
import os
import numpy as np
from contextlib import ExitStack
import concourse.bass as bass
import concourse.mybir as mybir
from concourse.alu_op_type import AluOpType as ALU
from concourse.bass_utils import run_bass_kernel_spmd

F32 = mybir.dt.float32
BF16 = mybir.dt.bfloat16
I32 = mybir.dt.int32
AF = mybir.ActivationFunctionType
AX = mybir.AxisListType

SEM_LIMIT = 4000
NQ = 16

D = 1024
T = 4096
C = 256
TT = T + C
DEPTH = 4
NE = 16
CAP = 512
CAPC = 32
NW = 2816
NFM = 1920
EPS = 1e-6
ZW = T + C + 4
PE2 = "pool"
SKIP = set(os.environ.get("KSKIP", "").split(","))


class Buf:
    def __init__(self, t, nparts=1, name="", excl=False):
        self.t = t
        self.name = name
        self.excl = excl
        self.lw = [None] * nparts
        self.rd = [[] for _ in range(nparts)]
        self.np_ = nparts

    def __getitem__(self, k):
        return self.t[k]


class BV:
    def __init__(self, parent, ap):
        self.parent = parent
        self.t = ap

    def __getitem__(self, k):
        return self.t[k]


def _acc(x):
    if isinstance(x, BV):
        x = x.parent
    if isinstance(x, Buf):
        return x, range(x.np_)
    b, p = x
    if isinstance(b, BV):
        b = b.parent
    if isinstance(p, int):
        p = [p]
    return b, p


class Op:
    __slots__ = ("id", "eng", "fn", "fence", "deps", "cost", "lat", "dma", "start")

    def __init__(self, id, eng, fn, fence, deps, cost, lat, dma):
        self.id = id; self.eng = eng; self.fn = fn; self.fence = fence
        self.deps = deps; self.cost = cost; self.lat = lat; self.dma = dma; self.start = 0.0


class KB:
    def __init__(self, nc, es, same_engine_sync=True):
        self.nc = nc
        self.es = es
        self.eng = {"pe": nc.tensor, "dve": nc.vector, "act": nc.scalar, "pool": nc.gpsimd, "sp": nc.sync}
        self.nsem = 0
        self.ses = same_engine_sync
        self.n_instr = 0
        self.segs = [[]]
        self.nops = 0
        self._fz = nc.alloc_sbuf_tensor("fencez", [128, 8], F32)
        self.fzb = Buf(self._fz, 2, "fencez")
        self.op("dve", lambda e: e.memset(self._fz[:, :], 0.0), (), [self.fzb])

    def _sem(self, name):
        self.nsem += 1
        return self.es.enter_context(self.nc.semaphore(f"{name}_{self.nsem}"))

    def _deps(self, reads, writes):
        deps = set()
        for x in reads:
            b, ps = _acc(x)
            for p in ps:
                if b.lw[p] is not None:
                    deps.add(b.lw[p])
                if b.excl:
                    deps.update(b.rd[p])
        for x in writes:
            b, ps = _acc(x)
            for p in ps:
                if b.lw[p] is not None:
                    deps.add(b.lw[p])
                deps.update(b.rd[p])
        return deps

    def _record(self, oid, reads, writes):
        for x in reads:
            b, ps = _acc(x)
            for p in ps:
                b.rd[p].append(oid)
        for x in writes:
            b, ps = _acc(x)
            for p in ps:
                b.lw[p] = oid
                b.rd[p] = []

    def _add(self, e, fn, reads, writes, fence, cost, lat, dma):
        if fence:
            writes = list(writes) + [(self.fzb, 0 if e == "dve" else 1)]
        oid = self.nops
        self.nops += 1
        o = Op(oid, e, fn, fence, self._deps(reads, writes), float(cost), float(lat), dma)
        self.segs[-1].append(o)
        self._record(oid, reads, writes)
        self.n_instr += 1
        return oid

    def op(self, e, fn, reads=(), writes=(), fence=False, cost=150.0):
        return self._add(e, fn, reads, writes, fence, cost, cost + 80.0, False)

    def dma(self, q, fn, reads=(), writes=(), nbytes=0):
        issue = 120.0 if q == "sp" else 900.0
        lat = 2200.0 + nbytes / 120.0
        return self._add(q, fn, reads, writes, False, issue, lat, True)

    def barrier(self):
        if self.segs[-1]:
            self.segs.append([])

    def finish(self):
        pass

    def sb(self, name, shape, dt=F32, nparts=1):
        return Buf(self.nc.alloc_sbuf_tensor(name, list(shape), dt), nparts, name)

    def ps(self, name, shape, dt=F32, nparts=1):
        return Buf(self.nc.alloc_psum_tensor(name, list(shape), dt), nparts, name, excl=True)

    def dram(self, name, shape, dt=F32, kind="Internal", nparts=1):
        return Buf(self.nc.dram_tensor(name, list(shape), dt, kind=kind), nparts, name)

    def _schedule(self, seg):
        ids = {o.id for o in seg}
        byid = {o.id: o for o in seg}
        succ = {o.id: [] for o in seg}
        indeg = {}
        for o in seg:
            d = [x for x in o.deps if x in ids]
            indeg[o.id] = len(d)
            for x in d:
                succ[x].append(o.id)
        free = {e: 0.0 for e in self.eng}
        rt = {o.id: 0.0 for o in seg}
        ready = {e: [] for e in self.eng}
        for o in seg:
            if indeg[o.id] == 0:
                ready[o.eng].append(o.id)
        order = []
        n = len(seg)
        while len(order) < n:
            best = None
            for e, lst in ready.items():
                if not lst:
                    continue
                f = free[e]
                c = min(lst, key=lambda i: (max(f, rt[i]), i))
                key = (max(f, rt[c]), c)
                if best is None or key < best[0]:
                    best = (key, e, c)
            (st, _), e, c = best
            o = byid[c]
            ready[e].remove(c)
            o.start = st
            free[e] = st + o.cost
            fin = st + o.lat
            order.append(o)
            for sx in succ[c]:
                if rt[sx] < fin:
                    rt[sx] = fin
                indeg[sx] -= 1
                if indeg[sx] == 0:
                    ready[byid[sx].eng].append(sx)
        return order

    def emit(self):
        esem = {}; ecnt = {}; eretired = []
        dsem = {}; dcnt = {}; dretired = []
        for e in self.eng:
            esem[e] = self._sem("e_" + e); ecnt[e] = 0
        for q in ("sp", "pool"):
            dsem[q] = [self._sem(f"d_{q}{i}") for i in range(NQ)]; dcnt[q] = 0
        waited = {e: {} for e in self.eng}
        prog = {e: [] for e in self.eng}
        event = {}

        def wait(e, evs):
            w = waited[e]
            best = {}
            for (sem, val, src) in evs:
                if src == e and (e == "pe" or not self.ses):
                    continue
                k = id(sem)
                if w.get(k, 0) >= val:
                    continue
                if k not in best or best[k][1] < val:
                    best[k] = (sem, val)
            for k, (sem, val) in best.items():
                prog[e].append(("wait", sem, val))
                w[k] = val

        def all_events():
            evs = list(eretired) + list(dretired)
            for e in self.eng:
                if ecnt[e] > 0:
                    evs.append((esem[e], ecnt[e], e))
            for q in dsem:
                n = dcnt[q]
                for j, sm in enumerate(dsem[q]):
                    cnt = (n - j + NQ - 1) // NQ if n > j else 0
                    if cnt > 0:
                        evs.append((sm, 16 * cnt, "dma"))
            return evs

        self.sched_span = []
        for si, seg in enumerate(self.segs):
            if not seg:
                continue
            if si > 0:
                evs = all_events()
                for e in self.eng:
                    wait(e, evs)
            order = self._schedule(seg)
            self.sched_span.append(max(o.start + o.lat for o in order))
            for o in order:
                e = o.eng
                evs = [event[d] for d in o.deps if d in event]
                if o.dma:
                    i = dcnt[e]
                    if 16 * (i // NQ) + 16 > SEM_LIMIT:
                        for j, sm in enumerate(dsem[e]):
                            cnt = (i - j + NQ - 1) // NQ if i > j else 0
                            if cnt > 0:
                                dretired.append((sm, 16 * cnt, "dma"))
                        dsem[e] = [self._sem(f"d_{e}{k}") for k in range(NQ)]
                        dcnt[e] = 0
                        i = 0
                    sem = dsem[e][i % NQ]
                    prev = 16 * (i // NQ)
                    if prev > 0:
                        evs.append((sem, prev, "dma"))
                    wait(e, evs)
                    prog[e].append(("op", o.fn, False, sem, 16))
                    dcnt[e] += 1
                    event[o.id] = (sem, prev + 16, "dma")
                else:
                    if ecnt[e] >= SEM_LIMIT:
                        eretired.append((esem[e], ecnt[e], e))
                        esem[e] = self._sem("e_" + e); ecnt[e] = 0
                    wait(e, evs)
                    prog[e].append(("op", o.fn, o.fence, esem[e], 1))
                    ecnt[e] += 1
                    event[o.id] = (esem[e], ecnt[e], e)
        wait("sp", all_events())
        self.prog = prog
        with self.nc.Block() as block:
            for e, dec in (("pe", block.tensor), ("dve", block.vector), ("act", block.scalar),
                           ("pool", block.gpsimd), ("sp", block.sync)):
                pr = prog[e]
                if not pr:
                    continue

                def body(eng, pr=pr, e=e):
                    for it in pr:
                        if it[0] == "wait":
                            eng.wait_ge(it[1], it[2])
                        else:
                            _, fn, fence, sem, inc = it
                            ins = fn(eng)
                            if fence:
                                ins = self._fence(e)
                            ins.then_inc(sem, inc)

                dec(body)

    def _fence(self, e):
        if e == "dve":
            return self.nc.vector.tensor_copy(self._fz[0:1, 0:1], self._fz[0:1, 1:2])
        if e == "act":
            return self.nc.scalar.copy(self._fz[0:1, 2:3], self._fz[0:1, 3:4])
        raise ValueError(e)


class G:
    def __init__(self, kb, arena_words):
        self.kb = kb
        self.nc = kb.nc
        self.arena = kb.nc.alloc_sbuf_tensor("arena", [128, arena_words], F32)
        self.aw = arena_words
        self.ap_ = 0
        self.uid = 0
        self.live = []
        self.old = []
        self.gen = 0
        self.use_barrier = False

    def reset(self):
        if self.use_barrier:
            self.kb.barrier()
        self.old = self.old + self.live
        self.live = []
        self.gen += 1
        self.ap_ = 0

    def carve(self, shape, dt=F32, parts=128, nparts=1, name="a"):
        n = 1
        for s in shape[1:]:
            n *= s
        words = n if dt in (F32, I32) else (n + 1) // 2
        words = (words + 1) // 2 * 2
        off = self.ap_
        self.ap_ += words
        assert self.ap_ <= self.aw, f"arena overflow {self.ap_} > {self.aw} ({name})"
        v = self.arena[0:shape[0], off:off + words]
        if dt != F32:
            v = v.bitcast(dt)
        if dt == BF16:
            v = v[:, 0:n]
        else:
            v = v[:, 0:n]
        if len(shape) == 3:
            v = v.rearrange("p (a b) -> p a b", a=shape[1])
        elif len(shape) == 4:
            v = v.rearrange("p (a b c) -> p a b c", a=shape[1], b=shape[2])
        self.uid += 1
        nb = Buf(v, nparts, f"{name}{self.uid}")
        nb.gen = self.gen
        inherit = set()
        for (s_, e_, b_) in self.old:
            if s_ < off + words and off < e_:
                for p in range(b_.np_):
                    if b_.lw[p] is not None:
                        inherit.add(b_.lw[p])
                    inherit.update(b_.rd[p])
        if inherit:
            for p in range(nparts):
                nb.rd[p] = list(inherit)
        self.old = [(s_, e_, b_) for (s_, e_, b_) in self.old if not (off <= s_ and e_ <= off + words)]
        self.live.append((off, off + words, nb))
        return nb

    @staticmethod
    def nf(ap):
        n = 1
        for d in list(ap.shape)[1:]:
            n *= d
        return n

    def _ecost(self, eng, n):
        if eng == "dve":
            return 70.0 + 1.1 * n
        if eng == "act":
            return 110.0 + 0.95 * n
        return 120.0 + 2.3 * n

    def mm(self, out, lhsT, rhs, st, sp, R, W):
        n = self.nf(rhs)
        c = 70.0 + 0.4 * n
        if lhsT.shape[0] < 128 or self.nf(lhsT) < 128:
            c = 70.0 + 0.85 * n
        self.kb.op("pe", lambda e: e.matmul(out, lhsT, rhs, start=st, stop=sp), R, W, cost=c)

    def tr(self, out, in_, ident, R, W):
        self.kb.op("pe", lambda e: e.transpose(out, in_, ident), R, W, cost=180.0)

    def act(self, out, in_, func, R, W, bias=None, scale=None, accum=None):
        kw = {}
        if bias is not None:
            kw["bias"] = bias
        if scale is not None:
            kw["scale"] = scale
        if accum is not None:
            kw["accum_out"] = accum
        c = self._ecost("act", self.nf(out)) + (150.0 if accum is not None else 0.0)
        self.kb.op("act", lambda e: e.activation(out, in_, func, **kw), R, W, fence=accum is not None, cost=c)

    def tt(self, eng, out, a, b, op, R, W):
        self.kb.op(eng, lambda e: e.tensor_tensor(out, a, b, op), R, W, cost=self._ecost(eng, self.nf(out)))

    def ts(self, eng, out, a, s1, s2, op0, op1, R, W, accum=None):
        c = self._ecost(eng, self.nf(out))
        if accum is not None:
            self.kb.op(eng, lambda e: e.tensor_scalar(out, a, s1, None, op0, op1, accum_out=accum), R, W, fence=True, cost=c + 150.0)
        elif op1 is None:
            self.kb.op(eng, lambda e: e.tensor_scalar(out, a, s1, None, op0), R, W, cost=c)
        else:
            self.kb.op(eng, lambda e: e.tensor_scalar(out, a, s1, s2, op0, op1), R, W, cost=c)

    def stt(self, out, in0, scalar, in1, op0, op1, R, W):
        self.kb.op("dve", lambda e: e.scalar_tensor_tensor(out, in0, scalar, in1, op0, op1), R, W, cost=70.0 + 2.0 * self.nf(out))

    def cp(self, eng, out, in_, R, W):
        c = self._ecost(eng, self.nf(out))
        if eng == "act":
            self.kb.op("act", lambda e: e.copy(out, in_), R, W, cost=c)
        else:
            self.kb.op(eng, lambda e: e.tensor_copy(out, in_), R, W, cost=c)

    def memset(self, eng, out, val, W):
        self.kb.op(eng, lambda e: e.memset(out, val), (), W, cost=self._ecost(eng, self.nf(out)))

    def recip(self, out, in_, R, W):
        self.kb.op("dve", lambda e: e.reciprocal(out, in_), R, W, cost=70.0 + 3.0 * self.nf(out))

    def red(self, out, in_, R, W):
        self.kb.op("dve", lambda e: e.tensor_reduce(out, in_, AX.X, ALU.add), R, W, cost=70.0 + 1.1 * self.nf(in_))

    @staticmethod
    def _nbytes(ap):
        n = 1
        for d in list(ap.shape):
            n *= d
        return n * 4

    def ld(self, out, in_, R, W, q="sp", slow=False):
        nb = self._nbytes(out)
        if slow:
            self.kb.dma(q, lambda e: e.dma_start(out=out, in_=in_, allow_slow_non_contiguous=True), R, W, nbytes=nb)
        else:
            self.kb.dma(q, lambda e: e.dma_start(out=out, in_=in_), R, W, nbytes=nb)

    def gather(self, out, src, idx, R, W, elem_off=0):
        self.kb.dma("pool", lambda e: e.indirect_dma_start(
            out=out, out_offset=None, in_=src,
            in_offset=bass.IndirectOffsetOnAxis(ap=idx, axis=0), element_offset=elem_off), R, W, nbytes=self._nbytes(out))

    def scatter_add(self, dst, src, idx, R, W):
        self.kb.dma("pool", lambda e: e.indirect_dma_start(
            out=dst, out_offset=bass.IndirectOffsetOnAxis(ap=idx, axis=0),
            in_=src, in_offset=None, compute_op=ALU.add), R, W, nbytes=2 * self._nbytes(src))


def _win_perm():
    names = ['cb', 'cc', 'cx', 'rq', 'rk', 'rv', 'rgf', 'rgb', 'aq', 'ak', 'av']
    sizes = [256] * 3 + [256] * 5 + [512, 128, 128]
    off = {}
    o = 0
    for n, s in zip(names, sizes):
        off[n] = o
        o += s

    def sw(nh):
        idx = []
        for h in range(nh):
            for i in range(64):
                b4, j = i // 16, i % 16
                idx.append(h * 64 + (b4 ^ 1) * 16 + j)
        return np.array(idx)

    cols = []
    for n in ('cb', 'cc', 'cx', 'rq', 'rk'):
        cols += list(off[n] + np.arange(256))
    cols += list(off['aq'] + np.arange(512))
    cols += list(off['ak'] + np.arange(128))
    cols += list(off['rv'] + np.arange(256)) + list(off['rgf'] + np.arange(256))
    cols += list(off['rgb'] + np.arange(256)) + list(off['av'] + np.arange(128))
    cols = np.array(cols)
    assert cols.shape[0] == NW
    return cols


def _rope_perm():
    p = np.zeros((128, 128), np.float32)
    for m in range(128):
        p[m ^ 16, m] = 1.0
    return p


def _rope_tables():
    t = np.arange(T)
    row = (t // 64).astype(np.float32)
    col = (t % 64).astype(np.float32)
    inv = (np.float32(10000.0) ** (-np.arange(0, 32, 2, dtype=np.float32) / np.float32(32))).astype(np.float32)
    ar = (row[:, None] * inv[None, :]).astype(np.float32)
    ac = (col[:, None] * inv[None, :]).astype(np.float32)
    cr, sr, cc_, sc_ = np.cos(ar).T, np.sin(ar).T, np.cos(ac).T, np.sin(ac).T
    cos64 = np.concatenate([cr, cr, cc_, cc_], 0)
    sin64 = np.concatenate([-sr, sr, -sc_, sc_], 0)
    cos = np.ones((128, TT), np.float32)
    sin = np.zeros((128, TT), np.float32)
    cos[:, :T] = np.concatenate([cos64, cos64], 0)
    sin[:, :T] = np.concatenate([sin64, sin64], 0)
    return cos, sin


def build(n_layers=DEPTH, debug=False, stop_after=None, wl=DEPTH, ne_w=NE):
    nc = bass.Bass("TRN2", target_bir_lowering=False)
    es = ExitStack()
    kb = KB(nc, es)
    skind = "ExternalOutput" if debug else "Internal"

    def din(name, shape, dt=F32):
        return kb.dram(name, shape, dt, kind="ExternalInput")

    x_in = din("x", [T, D]); ctx_in = din("ctx", [C, D]); cvec = din("cvec", [2, D])
    w_mod = din("w_mod", [wl, D, 6 * D]); bm_t = din("bm_t", [128, DEPTH, 48]); b_mod = din("b_mod", [DEPTH, 6 * D])
    g1t_d = din("g1t", [128, DEPTH, 8]); g2t_d = din("g2t", [128, DEPTH, 8])
    w_in = din("w_in", [wl, D, NW]); cw_d = din("cw_t", [128, DEPTH * 6])
    rdl_d = din("rdl", [1, 32]); sink_d = din("sink", [1, 32])
    w_out = din("w_out", [wl, D, D]); w_router = din("w_router", [wl, D, NE])
    w_gate = din("w_gate", [wl, ne_w, D, D]); w_up = din("w_up", [wl, ne_w, D, D]); w_down = din("w_down", [wl, ne_w, D, D])
    fg_d = din("final_g", [1, D]); cos_d = din("rope_cos", [128, TT]); sin_d = din("rope_sin", [128, TT])
    perm_d = din("rope_perm", [128, 128]); kvec_d = din("kvec4", [128, 1])
    out_d = kb.dram("out", [T, D], F32, kind="ExternalOutput")

    xres = kb.dram("xres", [TT, D], F32, kind=skind)
    Fd = kb.dram("Fd", [18 * 64, TT], BF16, kind=skind)
    TMd = kb.dram("TMd", [TT, 1152], BF16, kind=skind)
    Zd = kb.dram("Zd", [2, 128, ZW], F32, kind=skind)
    CBd = kb.dram("CBd", [2, 128, TT], BF16, kind=skind)
    SPd = kb.dram("SPd", [2, 34, 64, 256], BF16, kind=skind)
    XN2d = kb.dram("XN2d", [TT, D], BF16, kind=skind)
    EXd = kb.dram("EXd", [NE, TT], F32, kind=skind)
    POSd = kb.dram("POSd", [32, T], F32, kind=skind)
    MROWd = kb.dram("MROWd", [2, 2, D], F32, kind=skind)
    DBGd = kb.dram("DBGd", [128, 256], F32, kind=skind)

    identb = kb.sb("identb", [128, 128], BF16); identf = kb.sb("identf", [128, 128])
    Dm = kb.sb("Dm", [128, 128]); Dpos = kb.sb("Dpos", [128, 128]); Dneg = kb.sb("Dneg", [128, 128])
    Mge8 = kb.sb("Mge8", [128, 128]); Mle8 = kb.sb("Mle8", [128, 128])
    Mprev = kb.sb("Mprev", [128, 4, 128], BF16); Mnext = kb.sb("Mnext", [128, 4, 128], BF16)
    ones_bf = kb.sb("ones_bf", [128, 64], BF16); ones16 = kb.sb("ones16", [16, 16])
    permb = kb.sb("permb", [128, 128], BF16)
    SS = kb.sb("SS", [128, 128]); kvec4 = kb.sb("kvec4s", [128, 1])
    pcol = kb.sb("pcol", [128, 1]); p127 = kb.sb("p127", [128, 1]); jval = kb.sb("jval", [128, 4]); jb = kb.sb("jb", [128, 4])
    ip1 = kb.sb("ip1", [64, 128]); rev = kb.sb("rev", [64, 128])
    scv = kb.sb("scv", [128, 8, 2]); bm_f = kb.sb("bm_f", [128, DEPTH, 48])
    g1t = kb.sb("g1t_s", [128, DEPTH, 8]); g2t = kb.sb("g2t_s", [128, DEPTH, 8]); cw = kb.sb("cw", [128, DEPTH * 6])
    rdl_b = kb.sb("rdl_b", [128, 32]); sink_b = kb.sb("sink_b", [128, 32])
    AB = kb.sb("AB", [128, 4, 8, 2])
    lg = kb.sb("lg", [128, 8]); wkc = kb.sb("wkc", [128, 8]); dcol = kb.sb("dcol", [128, 8]); se = kb.sb("se", [128, 8])
    DMf = kb.sb("DMf", [128, 4, 128]); DMb = kb.sb("DMb", [128, 4, 128])
    QD = kb.sb("QD", [64, 2, 4, 128], BF16); WK = kb.sb("WK", [128, 2, 256]); DEC = kb.sb("DEC", [64, 2, 256])
    SINKE = kb.sb("SINKE", [128, 2, 512]); Wr = kb.sb("Wr", [128, 8, NE])
    KC = kb.sb("KC", [64, 2, 256], BF16); VC = kb.sb("VC", [128, 2, 2, 128], BF16)
    Sf = kb.sb("Sf", [64, 256]); Sb = kb.sb("Sb", [64, 256])
    LISTF = kb.sb("LISTF", [128, 64], nparts=NE); LISTI = kb.sb("LISTI", [128, 64], I32, nparts=NE)
    LISTCF = kb.sb("LISTCF", [32, 16], nparts=NE); LISTCI = kb.sb("LISTCI", [32, 16], I32, nparts=NE)
    SACC = kb.sb("SACC", [128, 64], nparts=NE)
    tmp128 = kb.sb("tmp128", [128, 128])
    WBIG = kb.sb("WBIG", [128, 32768], BF16, nparts=4)
    PB = [kb.ps(f"PB{i}", [128, 512]) for i in range(8)]
    PBF = [BV(PB[i], PB[i][:, :].bitcast(BF16)) for i in range(8)]
    PT = [PBF[6], PBF[7]]

    g = G(kb, 28672)

    def v3(ap, a):
        return ap.rearrange("p (a b) -> p a b", a=a)

    def setup():
        kb.op("pool", lambda e: e.iota(Dm[:, :], [[1, 128]], base=0, channel_multiplier=-1,
                                       allow_small_or_imprecise_dtypes=True), (), [Dm])
        g.ts("dve", identf[:, :], Dm[:, :], 0.0, None, ALU.is_equal, None, [Dm], [identf])
        g.cp("dve", identb[:, :], identf[:, :], [identf], [identb])
        g.ts("dve", Dpos[:, :], Dm[:, :], 0.0, None, ALU.max, None, [Dm], [Dpos])
        g.ts("dve", Dneg[:, :], Dm[:, :], -1.0, 0.0, ALU.mult, ALU.max, [Dm], [Dneg])
        g.ts("dve", Mge8[:, :], Dm[:, :], 0.0, 0.125, ALU.is_ge, ALU.mult, [Dm], [Mge8])
        g.ts("dve", Mle8[:, :], Dm[:, :], 0.0, 0.125, ALU.is_le, ALU.mult, [Dm], [Mle8])
        for gg in range(4):
            g.ts("dve", Mprev[:, gg, :], Dm[:, :], 0.0, None, ALU.is_le, None, [Dm], [Mprev])
            g.ts("dve", Mnext[:, gg, :], Dm[:, :], 0.0, None, ALU.is_ge, None, [Dm], [Mnext])
        g.memset("dve", ones_bf[:, :], 1.0, [ones_bf])
        g.memset("dve", VC[:, :, :, 64:128], 1.0, [VC])
        g.ld(permb[:, :], perm_d[:, :], [perm_d], [permb], q="pool")
        g.ld(kvec4[:, :], kvec_d[:, :], [kvec_d], [kvec4])
        g.ts("dve", SS[:, :], Dm[:, :], 0.0, None, ALU.is_equal, None, [Dm], [SS])
        for kk in (-96.0, -64.0, -32.0, 32.0, 64.0, 96.0):
            g.ts("dve", tmp128[:, :], Dm[:, :], kk, None, ALU.is_equal, None, [Dm], [tmp128])
            g.tt("dve", SS[:, :], SS[:, :], tmp128[:, :], ALU.add, [SS, tmp128], [SS])
        g.memset("dve", ones16[:, :], 1.0, [ones16])
        kb.op("pool", lambda e: e.iota(pcol[:, :], [[0, 1]], base=0, channel_multiplier=1,
                                       allow_small_or_imprecise_dtypes=True), (), [pcol])
        g.ts("dve", p127[:, :], pcol[:, :], -1.0, 127.0, ALU.mult, ALU.add, [pcol], [p127])
        kb.op("pool", lambda e: e.iota(jval[:, :], [[128, 4]], base=0, channel_multiplier=1,
                                       allow_small_or_imprecise_dtypes=True), (), [jval])
        g.ts("dve", jb[:, :], jval[:, :], 0.5, None, ALU.add, None, [jval], [jb])
        kb.op("pool", lambda e: e.iota(ip1[:, :], [[1, 128]], base=1, channel_multiplier=0,
                                       allow_small_or_imprecise_dtypes=True), (), [ip1])
        kb.op("pool", lambda e: e.iota(rev[:, :], [[-1, 128]], base=128, channel_multiplier=0,
                                       allow_small_or_imprecise_dtypes=True), (), [rev])
        g.memset("dve", LISTF[:, :], 0.0, [LISTF])
        g.memset("dve", LISTCF[:, :], 0.0, [LISTCF])
        g.ld(xres[0:T, :], x_in[:, :], [x_in], [xres])
        g.ld(xres[T:TT, :], ctx_in[:, :], [ctx_in], [xres])
        g.memset("dve", tmp128[:, :], 0.0, [tmp128])
        for c0 in (0, T + 1, T + 2, T + C + 3):
            g.ld(Zd.t.ap()[:, :, c0:c0 + 1].rearrange("c p o -> p c o"), v3(tmp128[:, 0:2], 2), [tmp128], [Zd], slow=True)
        for s_ in range(2):
            g.ld(scv[:, :, s_:s_ + 1], cvec.t.ap()[s_:s_ + 1, :].rearrange("s (kc p) -> p kc s", p=128), [cvec], [scv], slow=True)
        g.act(scv[:, :, :], scv[:, :, :], AF.Silu, [scv], [scv])
        g.ld(bm_f[:, :, :], bm_t[:, :, :], [bm_t], [bm_f])
        g.ld(g1t[:, :, :], g1t_d[:, :, :], [g1t_d], [g1t])
        g.ld(g2t[:, :, :], g2t_d[:, :, :], [g2t_d], [g2t])
        g.ld(cw[:, :], cw_d[:, :], [cw_d], [cw])
        g.ld(rdl_b[:, :], rdl_d[0:1, :].partition_broadcast(128), [rdl_d], [rdl_b])
        g.ld(sink_b[:, :], sink_d[0:1, :].partition_broadcast(128), [sink_d], [sink_b])

    def phase_mod(l):
        g.reset()
        modf_ps = v3(PB[0][:, 0:64], 32)
        rows_ps = PB[1]
        rows_sb = g.carve([2, 2048], name="rows")
        bmr = g.carve([2, 2048], name="bmr")
        modf = g.carve([128, 32, 2], name="modf")
        tmpm = g.carve([128, 8, 2], name="tmpm")
        WM = [g.carve([128, 8, 256], name="WM") for _ in range(2)]
        g.ld(bmr[0:2, 0:1024], b_mod[l:l + 1, 2 * D:3 * D].partition_broadcast(2), [b_mod], [bmr])
        g.ld(bmr[0:2, 1024:2048], b_mod[l:l + 1, 5 * D:6 * D].partition_broadcast(2), [b_mod], [bmr])
        wsrc = w_mod.t.ap()[l].rearrange("(kc p) n -> p kc n", p=128)
        for j in range(24):
            grp = j // 4
            wm = WM[j % 2]
            g.ld(wm[:, :, :], wsrc[:, :, j * 256:(j + 1) * 256], [w_mod], [wm])
            if grp in (0, 1, 3, 4):
                gi = {0: 0, 1: 1, 3: 2, 4: 3}[grp]
                for nb in range(2):
                    fm = gi * 8 + (j % 4) * 2 + nb
                    for kc in range(8):
                        g.mm(modf_ps[:, fm, :], wm[:, kc, nb * 128:(nb + 1) * 128], scv[:, kc, :], kc == 0, kc == 7,
                             [wm, scv], [PB[0]])
            else:
                ri = 0 if grp == 2 else 1
                col = ri * 1024 + (j % 4) * 256
                for kc in range(8):
                    g.mm(rows_ps[0:2, 0:256], scv[:, kc, :], wm[:, kc, :], kc == 0, kc == 7, [wm, scv], [PB[1]])
                g.tt("dve", rows_sb[0:2, col:col + 256], rows_ps[0:2, 0:256], bmr[0:2, col:col + 256], ALU.add,
                     [PB[1], bmr], [rows_sb])
        for gi, Gi in enumerate((0, 1, 3, 4)):
            g.tt("dve", modf[:, gi * 8:(gi + 1) * 8, :], modf_ps[:, gi * 8:(gi + 1) * 8, :],
                 bm_f[:, l, Gi * 8:(Gi + 1) * 8].unsqueeze(2).to_broadcast([128, 8, 2]), ALU.add, [PB[0], bm_f], [modf])
        g.ts("dve", tmpm[:, :, :], modf[:, 8:16, :], 1.0, None, ALU.add, None, [modf], [tmpm])
        g.tt("dve", AB[:, 0, :, :], tmpm[:, :, :], g1t[:, l, :].unsqueeze(2).to_broadcast([128, 8, 2]), ALU.mult,
             [tmpm, g1t], [AB])
        g.cp("dve", AB[:, 1, :, :], modf[:, 0:8, :], [modf], [AB])
        g.ts("dve", tmpm[:, :, :], modf[:, 24:32, :], 1.0, None, ALU.add, None, [modf], [tmpm])
        g.tt("dve", AB[:, 2, :, :], tmpm[:, :, :], g2t[:, l, :].unsqueeze(2).to_broadcast([128, 8, 2]), ALU.mult,
             [tmpm, g2t], [AB])
        g.cp("dve", AB[:, 3, :, :], modf[:, 16:24, :], [modf], [AB])
        g.ld(MROWd.t.ap()[0], rows_sb[0:2, 0:1024], [rows_sb], [MROWd])
        g.ld(MROWd.t.ap()[1], rows_sb[0:2, 1024:2048], [rows_sb], [MROWd])
        g.act(lg[:, :], rdl_b[:, l * 8:(l + 1) * 8], AF.Exp, [rdl_b], [lg], scale=-1.0)
        g.ts("dve", lg[:, :], lg[:, :], 1.0, None, ALU.add, None, [lg], [lg])
        g.act(lg[:, :], lg[:, :], AF.Ln, [lg], [lg])
        g.ts("dve", lg[:, :], lg[:, :], -1.0, None, ALU.mult, None, [lg], [lg])
        for d in range(2):
            for h in range(4):
                dh = d * 4 + h
                g.act(tmp128[:, :], (Dpos if d == 0 else Dneg)[:, :], AF.Exp, [Dpos, Dneg, lg], [tmp128], scale=lg[:, dh:dh + 1])
                g.tt("dve", (DMf if d == 0 else DMb)[:, h, :], tmp128[:, :], (Mge8 if d == 0 else Mle8)[:, :], ALU.mult,
                     [tmp128, Mge8, Mle8], [DMf if d == 0 else DMb])
                g.act(QD[:, d, h, :], (ip1 if d == 0 else rev)[:, :], AF.Exp, [ip1, rev, lg], [QD], scale=lg[0:64, dh:dh + 1])
                g.act(wkc[:, dh:dh + 1], (p127 if d == 0 else pcol)[:, :], AF.Exp, [p127, pcol, lg], [wkc], scale=lg[:, dh:dh + 1])
        g.ts("dve", wkc[:, :], wkc[:, :], 0.125, None, ALU.mult, None, [wkc], [wkc])
        g.act(dcol[:, :], lg[:, :], AF.Exp, [lg], [dcol], scale=128.0)
        g.act(se[:, :], sink_b[:, l * 8:(l + 1) * 8], AF.Exp, [sink_b], [se])
        for d in range(2):
            g.cp("dve", v3(WK[:, d, :], 4), wkc[:, d * 4:(d + 1) * 4].unsqueeze(2).to_broadcast([128, 4, 64]), [wkc], [WK])
            g.cp("dve", v3(DEC[:, d, :], 4), dcol[0:64, d * 4:(d + 1) * 4].unsqueeze(2).to_broadcast([64, 4, 64]), [dcol], [DEC])
            g.cp("dve", v3(SINKE[:, d, :], 4), se[:, d * 4:(d + 1) * 4].unsqueeze(2).to_broadcast([128, 4, 128]), [se], [SINKE])
        g.ld(Wr[:, :, :], w_router.t.ap()[l].rearrange("(kc p) e -> p kc e", p=128), [w_router], [Wr])

    FM_PAIRS = [
        (6, 0, False), (7, 2, False), (8, 4, True), (9, 6, True),
        (10, 8, False), (11, 10, False), (12, 12, False), (13, 14, False), (14, 16, False),
    ]

    def zcol(tt):
        return tt + 1 if tt < T else tt + 3

    def chunk_of(tt):
        return tt // 128 if tt < T else 32 + (tt - T) // 128

    def tiles(with_ctx=True):
        ts_ = []
        if with_ctx:
            ts_.append((1, T, C))
        for i in range(T // 512):
            ts_.append((0, i * 512, 512))
        return ts_

    def rms_stats(xin, nsub, ss, rstd, junk):
        for a in range(nsub):
            g.act(junk[:, :], xin[:, a, :], AF.Square, [xin], [junk, ss], accum=ss[:, a:a + 1])
        g.ts("dve", rstd[:, 0:nsub], ss[:, 0:nsub], 1.0 / D, EPS, ALU.mult, ALU.add, [ss], [rstd])
        g.act(rstd[:, 0:nsub], rstd[:, 0:nsub], AF.Sqrt, [rstd], [rstd])
        g.recip(rstd[:, 0:nsub], rstd[:, 0:nsub], [rstd], [rstd])

    def phase_proj(l):
        g.reset()
        win = WBIG[:, 0:8 * NW].rearrange("p (k n) -> p k n", k=8)
        for kc in range(8):
            for hh in range(2):
                g.ld(win[:, kc, hh * 1408:(hh + 1) * 1408],
                     w_in.t.ap()[l, kc * 128:(kc + 1) * 128, hh * 1408:(hh + 1) * 1408], [w_in], [WBIG], q="pool")
        xins = [g.carve([128, 4, D], name="xin") for _ in range(2)]
        xns = [g.carve([128, 4, D], BF16, name="xn") for _ in range(2)]
        hTs = [g.carve([128, 8, 512], BF16, name="hT") for _ in range(2)]
        cosb = g.carve([128, 512], name="cosb"); sinb = g.carve([128, 512], name="sinb")
        ss = g.carve([128, 4], name="ss"); rstd = g.carve([128, 4], name="rstd")
        junk = g.carve([128, D], name="junk")
        tmsts = [g.carve([128, 4, 1152], BF16, name="tmst") for _ in range(2)]
        t1 = g.carve([128, 512], name="t1"); t2 = g.carve([128, 512], name="t2")
        fst = [g.carve([128, 512], BF16, name="fst") for _ in range(2)]
        kst = [g.carve([128, 512], BF16, name="kst") for _ in range(2)]
        xbs = [g.carve([128, 512], BF16, name="xbs") for _ in range(2)]
        ccs = g.carve([128, 512], name="ccs")
        zst = g.carve([128, 2, 512], name="zst")
        cbst = g.carve([128, 2, 512], BF16, name="cbst")
        kw = g.carve([128, 256], BF16, name="kw")
        spst = [g.carve([64, 256], BF16, name="spst") for _ in range(2)]
        g.memset("dve", Sf[:, :], 0.0, [Sf])
        fcount = 0
        pbi = 0
        Ffull = Fd.t.ap()
        for ti_, (s, t0, n) in enumerate(tiles(True)):
            nsub = n // 128
            xin = xins[ti_ % 2]; xn = xns[ti_ % 2]; hT = hTs[ti_ % 2]; tmst = tmsts[ti_ % 2]
            g.ld(xin[:, 0:nsub, :], xres[t0:t0 + n, :].rearrange("(a p) d -> p a d", p=128), [xres], [xin])
            g.ld(cosb[:, 0:n], cos_d[:, t0:t0 + n], [cos_d], [cosb])
            g.ld(sinb[:, 0:n], sin_d[:, t0:t0 + n], [sin_d], [sinb])
            rms_stats(xin, nsub, ss, rstd, junk)
            for a in range(nsub):
                g.ts("dve", xn[:, a, :], xin[:, a, :], rstd[:, a:a + 1], None, ALU.mult, None, [xin, rstd], [xn])
            for kc in range(8):
                tp = PT[kc % 2]
                for a in range(nsub):
                    g.tr(tp[:, a * 128:(a + 1) * 128], xn[:, a, kc * 128:(kc + 1) * 128], identb[:, :], [xn, identb], [tp])
                g.act(hT[:, kc, 0:n], tp[:, 0:n], AF.Identity, [tp, AB], [hT],
                      bias=AB[:, 1, kc, s:s + 1], scale=AB[:, 0, kc, s:s + 1])

            def fm_block(fb):
                nonlocal pbi
                pb = PB[pbi % 4]
                pbi += 1
                for kc in range(8):
                    g.mm(pb[:, 0:n], win[:, kc, fb * 128:(fb + 1) * 128], hT[:, kc, 0:n], kc == 0, kc == 7, [WBIG, hT], [pb])
                return pb

            for c in range(2):
                pb = fm_block(c)
                g.cp("act", cbst[:, c, 0:n], pb[:, 0:n], [pb], [cbst])
            for c in range(2):
                pcc = fm_block(2 + c)
                g.cp("act", ccs[:, 0:n], pcc[:, 0:n], [pcc], [ccs])
                pcx = fm_block(4 + c)
                g.tt("dve", zst[:, c, 0:n], pcx[:, 0:n], ccs[:, 0:n], ALU.mult, [pcx, ccs], [zst])
            g.ld(CBd.t.ap()[:, :, t0:t0 + n].rearrange("c p t -> p c t"), cbst[:, :, 0:n], [cbst], [CBd])
            g.ld(Zd.t.ap()[:, :, zcol(t0):zcol(t0) + n].rearrange("c p t -> p c t"), zst[:, :, 0:n], [zst], [Zd])
            for (fb, hidx, isk) in FM_PAIRS:
                px = fm_block(fb)
                xb = xbs[fcount % 2]
                g.cp("act", xb[:, 0:n], px[:, 0:n], [px], [xb])
                g.tt("dve", t1[:, 0:n], px[:, 0:n], cosb[:, 0:n], ALU.mult, [px, cosb], [t1])
                psw = PB[pbi % 4]
                pbi += 1
                g.mm(psw[:, 0:n], permb[:, :], xb[:, 0:n], True, True, [permb, xb], [psw])
                g.tt("dve", t2[:, 0:n], psw[:, 0:n], sinb[:, 0:n], ALU.mult, [psw, sinb], [t2])
                if isk:
                    dst = kst[(hidx - 4) // 2]
                else:
                    dst = fst[fcount % 2]
                fcount += 1
                g.tt("pool", dst[:, 0:n], t1[:, 0:n], t2[:, 0:n], ALU.add, [t1, t2], [dst])
                g.ld(Ffull[hidx * 64:hidx * 64 + 128, t0:t0 + n], dst[:, 0:n], [dst], [Fd])
            ktp = PT[0]
            for a in range(nsub):
                for hp in range(2):
                    g.tr(ktp[:, a * 256 + hp * 128:a * 256 + (hp + 1) * 128], kst[hp][:, a * 128:(a + 1) * 128], identb[:, :],
                         [kst[hp], identb], [ktp])
            g.cp("act", tmst[:, 0:nsub, 0:256], v3(ktp[:, 0:nsub * 256], nsub), [ktp], [tmst])
            for a in range(nsub):
                pa, pbb = PB[4], PB[5]
                for kc in range(8):
                    g.mm(pa[:, 0:512], hT[:, kc, a * 128:(a + 1) * 128], win[:, kc, NFM:NFM + 512], kc == 0, kc == 7, [WBIG, hT], [pa])
                for kc in range(8):
                    g.mm(pbb[:, 0:384], hT[:, kc, a * 128:(a + 1) * 128], win[:, kc, NFM + 512:NFM + 896], kc == 0, kc == 7, [WBIG, hT], [pbb])
                g.cp("dve", tmst[:, a, 256:512], pa[:, 0:256], [pa], [tmst])
                g.act(tmst[:, a, 512:768], pa[:, 256:512], AF.Silu, [pa], [tmst])
                g.act(tmst[:, a, 768:1024], pbb[:, 0:256], AF.Silu, [pbb], [tmst])
                g.cp("dve", tmst[:, a, 1024:1152], pbb[:, 256:384], [pbb], [tmst])
            g.ld(TMd[t0:t0 + n, :].rearrange("(a p) c -> p a c", p=128), tmst[:, 0:nsub, :], [tmst], [TMd])
            for a in range(nsub):
                ch = chunk_of(t0 + a * 128)
                g.tt("dve", kw[:, :], tmst[:, a, 0:256], WK[:, 0, :], ALU.mult, [tmst, WK], [kw])
                ups = PB[4]
                for h in range(4):
                    g.mm(ups[0:64, h * 64:(h + 1) * 64], kw[:, h * 64:(h + 1) * 64], tmst[:, a, 256 + h * 64:256 + (h + 1) * 64],
                         True, True, [kw, tmst], [ups])
                sp = spst[ch % 2]
                g.cp("act", sp[:, :], Sf[:, :], [Sf], [sp])
                g.ld(SPd.t.ap()[0, ch], sp[:, :], [sp], [SPd])
                g.tt("pool", Sf[:, :], Sf[:, :], DEC[:, 0, :], ALU.mult, [Sf, DEC], [Sf])
                g.tt("dve", Sf[:, :], Sf[:, :], ups[0:64, 0:256], ALU.add, [Sf, ups], [Sf])

    def phase_bwd(l):
        g.reset()
        kvb = [g.carve([128, 512], BF16, name="kvb") for _ in range(2)]
        kw = g.carve([128, 256], BF16, name="kwb")
        spst = [g.carve([64, 256], BF16, name="spstb") for _ in range(2)]
        g.memset("dve", Sb[:, :], 0.0, [Sb])
        order = [33, 32] + list(range(31, -1, -1))
        for i, ch in enumerate(order):
            r0 = ch * 128 if ch < 32 else T + (ch - 32) * 128
            kv = kvb[i % 2]
            g.ld(kv[:, :], TMd[r0:r0 + 128, 0:512], [TMd], [kv])
            g.tt("dve", kw[:, :], kv[:, 0:256], WK[:, 1, :], ALU.mult, [kv, WK], [kw])
            ups = PB[i % 2]
            for h in range(4):
                g.mm(ups[0:64, h * 64:(h + 1) * 64], kw[:, h * 64:(h + 1) * 64], kv[:, 256 + h * 64:256 + (h + 1) * 64],
                     True, True, [kw, kv], [ups])
            sp = spst[i % 2]
            g.cp("act", sp[:, :], Sb[:, :], [Sb], [sp])
            g.ld(SPd.t.ap()[1, ch], sp[:, :], [sp], [SPd])
            g.tt("pool", Sb[:, :], Sb[:, :], DEC[:, 1, :], ALU.mult, [Sb, DEC], [Sb])
            g.tt("dve", Sb[:, :], Sb[:, :], ups[0:64, 0:256], ALU.add, [Sb, ups], [Sb])

    def mix_tiles(with_ctx):
        ts_ = []
        if with_ctx:
            ts_.append((1, T, 256))
        for i in range(T // 256):
            ts_.append((0, i * 256, 256))
        return ts_

    def phase_mix(l, need_ctx):
        g.reset()
        wo_cr = WBIG[:, 0:4096].rearrange("p (c n) -> p c n", c=4)
        wo_at = WBIG[0:64, 8192:16384].rearrange("p (h n) -> p h n", h=8)
        g.ld(wo_cr, w_out.t.ap()[l, 0:512, :].rearrange("(c p) n -> p c n", p=128), [w_out], [(WBIG, 0)], q="pool")
        g.ld(wo_at, w_out.t.ap()[l, 512:1024, :].rearrange("(h p) n -> p h n", p=64), [w_out], [(WBIG, 1)], q="pool")
        Fv = Fd.t.ap().rearrange("(h p) t -> p h t", p=64)
        g.ld(KC[:, :, :], Fv[:, 16:18, T:TT], [Fd], [KC])
        for kv_ in range(2):
            g.ld(VC[:, :, kv_, 0:64], TMd[T:TT, 1024 + kv_ * 64:1088 + kv_ * 64].rearrange("(a p) c -> p a c", p=128), [TMd], [VC])
        NT = 256

        def tset():
            return dict(RQK=g.carve([64, 8, NT], BF16, name="RQK"), AQ=g.carve([64, 8, NT], BF16, name="AQ"),
                        AK=g.carve([64, 2, NT + 256], BF16, name="AK"), TMt=g.carve([128, 2, 768], BF16, name="TMt"),
                        AV=g.carve([128, 4, 2, 128], BF16, name="AV"), Zt=g.carve([128, 2, NT + 2], name="Zt"),
                        CBt=g.carve([128, 2, NT], BF16, name="CBt"), SPt=g.carve([64, 2, 2, 256], BF16, name="SPt"),
                        xin=g.carve([128, 2, D], name="xin2"), mconv=g.carve([128, 2, NT], BF16, name="mconv"),
                        matt=g.carve([64, 8, NT], BF16, name="matt"), mret=g.carve([128, 2, NT], BF16, name="mret"))
        TB = [tset(), tset()]
        for B_ in TB:
            g.memset("dve", B_["AV"][:, :, :, 64:128], 1.0, [B_["AV"]])
        G1r = g.carve([128, D], name="G1r")
        ctmp = g.carve([128, NT], name="ctmp")
        STf = [g.carve([128, 4, 128], BF16, name="STf") for _ in range(2)]
        STb = [g.carve([128, 4, 128], BF16, name="STb") for _ in range(2)]
        qd = [g.carve([64, 2, 4, 128], BF16, name="qd") for _ in range(2)]
        t1 = [g.carve([128, 512], name="t1m") for _ in range(2)]
        sq = g.carve([128, 512], name="sq")
        st8 = [g.carve([128, 6, 8], name="st8") for _ in range(2)]
        ret = [g.carve([128, 256], BF16, name="ret") for _ in range(2)]
        PTs = [g.carve([128, 4, 128], BF16, name="PTs") for _ in range(2)]
        den = [g.carve([128, 512], name="den") for _ in range(2)]
        tmpy = [g.carve([128, 512], name="tmpy") for _ in range(2)]
        junk = g.carve([128, D], BF16, name="junk2")
        ss = g.carve([128, 4], name="ss2"); rstd = g.carve([128, 4], name="rstd2")
        xn2 = g.carve([128, D], name="xn2"); xn2b = g.carve([128, D], BF16, name="xn2b")
        h2T = g.carve([128, 8, 128], name="h2T")
        ex = [g.carve([16, 128], name="ex") for _ in range(2)]
        tl = mix_tiles(need_ctx)
        info = {}

        def load_tile(ti):
            (s, t0, n) = tl[ti]
            B = TB[ti % 2]
            nsub = n // 128
            g.ld(B["RQK"][:, :, 0:n], Fv[:, 0:8, t0:t0 + n], [Fd], [B["RQK"]])
            g.ld(B["AQ"][:, :, 0:n], Fv[:, 8:16, t0:t0 + n], [Fd], [B["AQ"]])
            koff = 0
            if s == 0:
                lo = max(t0 - 128, 0); hi = min(t0 + n + 128, T)
                koff = t0 - lo
                g.ld(B["AK"][:, :, 0:hi - lo], Fv[:, 16:18, lo:hi], [Fd], [B["AK"]])
                for kv_ in range(2):
                    g.ld(B["AV"][:, 0:(hi - lo) // 128, kv_, 0:64], TMd[lo:hi, 1024 + kv_ * 64:1088 + kv_ * 64].rearrange("(a p) c -> p a c", p=128), [TMd], [B["AV"]])
            g.ld(B["TMt"][:, 0:nsub, :], TMd[t0:t0 + n, 256:1024].rearrange("(a p) c -> p a c", p=128), [TMd], [B["TMt"]])
            zc = zcol(t0)
            g.ld(B["Zt"][:, :, 0:n + 2], Zd.t.ap()[:, :, zc - 1:zc + n + 1].rearrange("c p t -> p c t"), [Zd], [B["Zt"]])
            g.ld(B["CBt"][:, :, 0:n], CBd.t.ap()[:, :, t0:t0 + n].rearrange("c p t -> p c t"), [CBd], [B["CBt"]])
            g.ld(B["xin"][:, 0:nsub, :], xres[t0:t0 + n, :].rearrange("(a p) d -> p a d", p=128), [xres], [B["xin"]])
            ch0 = chunk_of(t0)
            for d_ in range(2):
                g.ld(B["SPt"][:, d_, 0:nsub, :], SPd.t.ap()[d_, ch0:ch0 + nsub].rearrange("a p f -> p a f"), [SPd], [B["SPt"]])
            info[ti] = koff

        load_tile(0)
        cur_s = None
        cnt = 0
        for ti, (s, t0, n) in enumerate(tl):
            if ti + 1 < len(tl):
                load_tile(ti + 1)
            if s != cur_s:
                g.ld(G1r[:, :], MROWd.t.ap()[0, s:s + 1, :].partition_broadcast(128), [MROWd], [G1r])
                cur_s = s
            B = TB[ti % 2]
            RQK, AQ, AK, TMt, AV, Zt, CBt, SPt, xin, mconv, matt, mret = (B[k] for k in ("RQK", "AQ", "AK", "TMt", "AV", "Zt", "CBt", "SPt", "xin", "mconv", "matt", "mret"))
            koff = info[ti]
            nsub = n // 128
            lat = s == 0
            for c in range(2):
                wb = l * 6
                g.ts("dve", ctmp[:, 0:n], Zt[:, c, 0:n], cw[:, wb + 0 * 2 + c:wb + 0 * 2 + c + 1], None, ALU.mult, None, [Zt, cw], [ctmp])
                g.stt(ctmp[:, 0:n], Zt[:, c, 1:n + 1], cw[:, wb + 1 * 2 + c:wb + 1 * 2 + c + 1], ctmp[:, 0:n], ALU.mult, ALU.add, [Zt, cw, ctmp], [ctmp])
                g.stt(ctmp[:, 0:n], Zt[:, c, 2:n + 2], cw[:, wb + 2 * 2 + c:wb + 2 * 2 + c + 1], ctmp[:, 0:n], ALU.mult, ALU.add, [Zt, cw, ctmp], [ctmp])
                g.tt("dve", mconv[:, c, 0:n], ctmp[:, 0:n], CBt[:, c, 0:n], ALU.mult, [ctmp, CBt], [mconv])
            for a in range(nsub):
                sl = slice(a * 128, (a + 1) * 128)
                stp = PB[0]
                for h in range(4):
                    g.mm(stp[:, h * 128:(h + 1) * 128], RQK[:, 4 + h, sl], RQK[:, h, sl], True, True, [RQK], [stp])
                g.tt("dve", STf[a][:, :, :], v3(stp[:, :], 4), DMf[:, :, :], ALU.mult, [stp, DMf], [STf[a]])
                g.tt("dve", STb[a][:, :, :], v3(stp[:, :], 4), DMb[:, :, :], ALU.mult, [stp, DMb], [STb[a]])
                for d in range(2):
                    g.tt("pool", qd[a][:, d, :, :], RQK[:, 0:4, sl], QD[:, d, :, :], ALU.mult, [RQK, QD], [qd[a]])
                op_ = PB[1]
                for d in range(2):
                    STd = STf[a] if d == 0 else STb[a]
                    for h in range(4):
                        o_ap = op_[:, d * 256 + h * 64:d * 256 + (h + 1) * 64]
                        g.mm(o_ap, STd[:, h, :], TMt[:, a, h * 64:(h + 1) * 64], True, False, [STd, TMt], [op_])
                        g.mm(o_ap, qd[a][:, d, h, :], SPt[:, d, a, h * 64:(h + 1) * 64], False, True, [qd[a], SPt], [op_])
                o3 = v3(op_[:, :], 8)
                s8 = st8[a]
                g.red(s8[:, 0, :], o3, [op_], [s8])
                g.act(sq[:, :], op_[:, :], AF.Square, [op_], [sq])
                g.red(s8[:, 1, :], v3(sq[:, :], 8), [sq], [s8])
                g.ts("dve", s8[:, 2, :], s8[:, 0, :], 1.0 / 64, None, ALU.mult, None, [s8], [s8])
                g.tt("dve", s8[:, 5, :], s8[:, 2, :], s8[:, 2, :], ALU.mult, [s8], [s8])
                g.stt(s8[:, 3, :], s8[:, 1, :], 1.0 / 64, s8[:, 5, :], ALU.mult, ALU.subtract, [s8], [s8])
                g.ts("dve", s8[:, 3, :], s8[:, 3, :], EPS, None, ALU.add, None, [s8], [s8])
                g.act(s8[:, 3, :], s8[:, 3, :], AF.Sqrt, [s8], [s8])
                g.recip(s8[:, 3, :], s8[:, 3, :], [s8], [s8])
                g.stt(s8[:, 4, :], s8[:, 2, :], -1.0, s8[:, 3, :], ALU.mult, ALU.mult, [s8], [s8])
                t13 = v3(t1[a][:, :], 8)
                g.tt("dve", t13, o3, s8[:, 3, :].unsqueeze(2).to_broadcast([128, 8, 64]), ALU.mult, [op_, s8], [t1[a]])
                g.tt("pool", t13, t13, s8[:, 4, :].unsqueeze(2).to_broadcast([128, 8, 64]), ALU.add, [t1[a], s8], [t1[a]])
                g.tt("pool", t1[a][:, :], t1[a][:, :], TMt[:, a, 256:768], ALU.mult, [t1[a], TMt], [t1[a]])
                g.tt("pool", ret[a][:, :], t1[a][:, 0:256], t1[a][:, 256:512], ALU.add, [t1[a]], [ret[a]])
                rtp = PBF[0]
                for c in range(2):
                    g.tr(rtp[:, c * 128:(c + 1) * 128], ret[a][:, c * 128:(c + 1) * 128], identb[:, :], [ret[a], identb], [rtp])
                g.cp("act", mret[:, :, sl], v3(rtp[:, 0:256], 2), [rtp], [mret])
            for a in range(nsub):
                sl = slice(a * 128, (a + 1) * 128)
                qb = t0 // 128 + a
                for kvh in range(2):
                    kbl = []
                    if lat:
                        ka = koff // 128 + a
                        if qb > 0:
                            kbl.append(("prev", AK[:, kvh, (ka - 1) * 128:ka * 128], AV[:, ka - 1, kvh, :], [AK], [AV]))
                        kbl.append(("same", AK[:, kvh, ka * 128:(ka + 1) * 128], AV[:, ka, kvh, :], [AK], [AV]))
                        if qb < T // 128 - 1:
                            kbl.append(("next", AK[:, kvh, (ka + 1) * 128:(ka + 2) * 128], AV[:, ka + 1, kvh, :], [AK], [AV]))
                    for cb_ in range(2):
                        kbl.append(("ctx", KC[:, kvh, cb_ * 128:(cb_ + 1) * 128], VC[:, cb_, kvh, :], [KC], [VC]))
                    jj = (a * 2 + kvh) % 2
                    abp = PB[4 + jj]
                    for i, (kind, kT, vv, kR, vR) in enumerate(kbl):
                        sp_ = PB[2 + cnt % 2]
                        pts = PTs[cnt % 2]
                        cnt += 1
                        g.mm(v3(sp_[:, :], 4), kT, AQ[:, kvh * 4:(kvh + 1) * 4, sl], True, True, kR + [AQ], [sp_])
                        g.act(pts[:, :, :], v3(sp_[:, :], 4), AF.Exp, [sp_], [pts], scale=0.125)
                        if kind == "prev":
                            g.tt("dve", pts[:, :, :], pts[:, :, :], Mprev[:, :, :], ALU.mult, [pts, Mprev], [pts])
                        elif kind == "next":
                            g.tt("dve", pts[:, :, :], pts[:, :, :], Mnext[:, :, :], ALU.mult, [pts, Mnext], [pts])
                        g.mm(v3(abp[:, :], 4), vv, pts[:, :, :], i == 0, i == len(kbl) - 1, vR + [pts], [abp])
                    dn = den[jj]
                    g.tt("dve", dn[64:128, :], abp[64:128, :], SINKE[64:128, kvh, :], ALU.add, [abp, SINKE], [dn])
                    g.recip(dn[64:128, :], dn[64:128, :], [dn], [dn])
                    g.ld(dn[0:64, :], dn[64:128, :], [dn], [dn])
                    g.tt("dve", matt[:, kvh * 4:(kvh + 1) * 4, sl], v3(abp[0:64, :], 4), v3(dn[0:64, :], 4), ALU.mult, [abp, dn], [matt])
            for a in range(nsub):
                sl = slice(a * 128, (a + 1) * 128)
                for nh in range(2):
                    yp = PB[6]
                    ns = slice(nh * 512, (nh + 1) * 512)
                    for c in range(2):
                        g.mm(yp[:, :], mconv[:, c, sl], wo_cr[:, c, ns], c == 0, False, [mconv, (WBIG, 0)], [yp])
                    for c in range(2):
                        g.mm(yp[:, :], mret[:, c, sl], wo_cr[:, 2 + c, ns], False, False, [mret, (WBIG, 0)], [yp])
                    for h in range(8):
                        g.mm(yp[:, :], matt[:, h, sl], wo_at[:, h, ns], False, h == 7, [matt, (WBIG, 1)], [yp])
                    ty = tmpy[nh]
                    g.tt("dve", ty[:, :], yp[:, :], G1r[:, ns], ALU.mult, [yp, G1r], [ty])
                    g.tt("pool", xin[:, a, ns], xin[:, a, ns], ty[:, :], ALU.add, [xin, ty], [xin])
                g.ld(xres[t0 + a * 128:t0 + (a + 1) * 128, :], xin[:, a, :], [xin], [xres])
            for a in range(nsub):
                g.act(junk[:, :], xin[:, a, :], AF.Square, [xin], [junk, ss], accum=ss[:, a:a + 1])
                g.ts("dve", rstd[:, a:a + 1], ss[:, a:a + 1], 1.0 / D, EPS, ALU.mult, ALU.add, [ss], [rstd])
                g.act(rstd[:, a:a + 1], rstd[:, a:a + 1], AF.Sqrt, [rstd], [rstd])
                g.recip(rstd[:, a:a + 1], rstd[:, a:a + 1], [rstd], [rstd])
                g.ts("dve", xn2[:, :], xin[:, a, :], rstd[:, a:a + 1], None, ALU.mult, None, [xin, rstd], [xn2])
                g.cp("act", xn2b[:, :], xn2[:, :], [xn2], [xn2b])
                g.ld(XN2d[t0 + a * 128:t0 + (a + 1) * 128, :], xn2b[:, :], [xn2b], [XN2d])
                for rnd in range(2):
                    tps = PB[7]
                    for k4 in range(4):
                        kc = rnd * 4 + k4
                        g.tr(tps[:, k4 * 128:(k4 + 1) * 128], xn2[:, kc * 128:(kc + 1) * 128], identf[:, :], [xn2, identf], [tps])
                    for k4 in range(4):
                        kc = rnd * 4 + k4
                        g.act(h2T[:, kc, :], tps[:, k4 * 128:(k4 + 1) * 128], AF.Identity, [tps, AB], [h2T],
                              bias=AB[:, 3, kc, s:s + 1], scale=AB[:, 2, kc, s:s + 1])
                lps = PB[7]
                for kc in range(8):
                    g.mm(lps[0:16, 0:128], Wr[:, kc, :], h2T[:, kc, :], kc == 0, kc == 7, [Wr, h2T], [lps])
                exb = ex[a % 2]
                g.act(exb[:, :], lps[0:16, 0:128], AF.Exp, [lps], [exb])
                g.ld(EXd[:, t0 + a * 128:t0 + (a + 1) * 128], exb[:, :], [exb], [EXd])

    def phase_route(l, need_ctx):
        g.reset()
        EXT = g.carve([16, TT], name="EXT")
        rden = g.carve([16, 512], name="rden")
        COMB = g.carve([32, T], name="COMB")
        PBc = [g.carve([128, T], name="PBc") for _ in range(2)]
        jA = g.carve([128, T], BF16, name="jA")
        jB = g.carve([128, T], BF16, name="jB")
        PBcc = [g.carve([32, C], name="PBcc") for _ in range(2)]
        jC = g.carve([32, C], BF16, name="jC")
        sc = g.carve([32, 8], name="scr")
        lo, hi, mid, cnt, pp, dd, kvec = (sc[:, i:i + 1] for i in range(7))
        g.ld(EXT[:, :], EXd[:, :], [EXd], [EXT])
        for c0 in range(0, TT, 512):
            w = min(512, TT - c0)
            dps = PB[(c0 // 512) % 2]
            g.mm(dps[0:16, 0:w], ones16[:, :], EXT[:, c0:c0 + w], True, True, [ones16, EXT], [dps])
            g.recip(rden[:, 0:w], dps[0:16, 0:w], [dps], [rden])
            g.tt("dve", EXT[:, c0:c0 + w], EXT[:, c0:c0 + w], rden[:, 0:w], ALU.mult, [EXT, rden], [EXT])
        g.ld(EXd[:, :], EXT[:, :], [EXT], [EXd])
        g.memset("dve", COMB[:, :], 0.0, [COMB])
        g.ld(COMB[0:16, :], EXd[:, 0:T], [EXd], [COMB])
        if need_ctx:
            g.ld(COMB[16:32, 0:C], EXd[:, T:TT], [EXd], [COMB])
        COMB4 = g.carve([128, 1024], name="COMB4")
        sc4 = g.carve([128, 8], name="sc4")
        lo4, hi4, mid4, cnt4, pp4, dd4 = (sc4[:, i:i + 1] for i in range(6))
        g.memset("dve", COMB4[:, :], 0.0, [COMB4])
        for q4 in range(4):
            g.ld(COMB4[q4 * 32:q4 * 32 + 16, :], EXd[:, q4 * 1024:(q4 + 1) * 1024], [EXd], [COMB4])
            if need_ctx:
                g.ld(COMB4[q4 * 32 + 16:q4 * 32 + 32, 0:64], EXd[:, T + q4 * 64:T + (q4 + 1) * 64], [EXd], [COMB4])
        g.memset("dve", sc4[:, :], 0.0, [sc4])
        g.memset("dve", hi4, 1.0, [sc4])
        cps = PB[2]
        for it in range(34):
            g.tt("dve", mid4, lo4, hi4, ALU.add, [sc4], [sc4])
            g.ts("dve", mid4, mid4, 0.5, None, ALU.mult, None, [sc4], [sc4])
            g.ts("dve", jA[:, 0:1024], COMB4[:, :], mid4, 0.0, ALU.is_gt, ALU.add, [COMB4, sc4], [jA, sc4], accum=cnt4)
            g.mm(cps[:, 0:2], SS[:, :], sc4[:, 3:5], True, True, [SS, sc4], [cps])
            g.tt("dve", pp4, cps[:, 0:1], kvec4[:, :], ALU.is_gt, [cps, kvec4], [sc4])
            g.tt("dve", dd4, mid4, lo4, ALU.subtract, [sc4], [sc4])
            g.stt(lo4, dd4, pp4, lo4, ALU.mult, ALU.add, [sc4], [sc4])
            g.tt("dve", dd4, hi4, mid4, ALU.subtract, [sc4], [sc4])
            g.stt(hi4, dd4, pp4, mid4, ALU.mult, ALU.add, [sc4], [sc4])
        g.cp("dve", hi, sc4[0:32, 1:2], [sc4], [sc])
        g.ts("dve", jA[0:32, :], COMB[:, :], hi, None, ALU.is_gt, None, [COMB, sc], [jA])
        zr = PBc[1]
        g.memset("dve", zr[0:32, :], 0.0, [zr])
        kb.op("dve", lambda e: e.tensor_tensor_scan(COMB[:, :], jA[0:32, :], zr[0:32, :], 0.0, ALU.add, ALU.add), [jA, zr], [COMB], cost=9000.0)
        g.ld(POSd[:, :], COMB[:, :], [COMB], [POSd])
        for e in range(NE):
            pb = PBc[e % 2]
            g.ld(pb[:, :], POSd[e:e + 1, :].partition_broadcast(128), [POSd], [pb])
            for jt in range(4):
                col = e * 4 + jt
                if jt < 2:
                    g.ts("dve", jA[:, :], pb[:, :], jval[:, jt:jt + 1], 0.0, ALU.is_le, ALU.add,
                         [pb, jval], [jA, (LISTF, e)], accum=LISTF[:, col:col + 1])
                else:
                    g.act(jB[:, :], pb[:, :], AF.Sign, [pb, jb], [jB, (SACC, e)], bias=jb[:, jt:jt + 1], scale=-1.0,
                          accum=SACC[:, col:col + 1])
            g.ts("dve", LISTF[:, e * 4 + 2:e * 4 + 4], SACC[:, e * 4 + 2:e * 4 + 4], float(T), 0.5, ALU.add, ALU.mult, [(SACC, e)], [(LISTF, e)])
            g.cp("dve", LISTI[:, e * 4:e * 4 + 4], LISTF[:, e * 4:e * 4 + 4], [(LISTF, e)], [(LISTI, e)])
            if need_ctx:
                pc = PBcc[e % 2]
                g.ld(pc[0:32, 0:C], POSd[16 + e:17 + e, 0:C].partition_broadcast(32), [POSd], [pc])
                g.ts("dve", jC[0:32, 0:C], pc[0:32, 0:C], jval[0:32, 0:1], 0.0, ALU.is_le, ALU.add,
                     [pc, jval], [jC, (LISTCF, e)], accum=LISTCF[:, e:e + 1])
                g.ts("dve", LISTCF[:, e:e + 1], LISTCF[:, e:e + 1], float(T), None, ALU.add, None, [(LISTCF, e)], [(LISTCF, e)])
                g.cp("dve", LISTCI[:, e:e + 1], LISTCF[:, e:e + 1], [(LISTCF, e)], [(LISTCI, e)])

    def phase_ffn(l, need_ctx):
        g.reset()
        wparts = [WBIG[:, i * 8192:(i + 1) * 8192].rearrange("p (k n) -> p k n", k=8) for i in range(4)]
        G2r = [g.carve([128, D], name="G2r") for _ in range(2)]
        for s in range(2 if need_ctx else 1):
            g.ld(G2r[s][:, :], MROWd.t.ap()[1, s:s + 1, :].partition_broadcast(128), [MROWd], [G2r[s]])
        XG = [[g.carve([128, D], BF16, name="XG") for _ in range(4)] for _ in range(2)]
        XGc = [g.carve([32, D], BF16, name="XGc") for _ in range(2)]
        GT = [g.carve([128, 4], name="GT") for _ in range(2)]
        GTc = [g.carve([32, 1], name="GTc") for _ in range(2)]
        xsTs = [g.carve([128, 8, 544], BF16, name="xsT") for _ in range(2)]
        actTs = [g.carve([128, 8, 544], BF16, name="actT") for _ in range(2)]
        sg = [g.carve([128, 544], name="sg") for _ in range(2)]
        YS = [g.carve([128, D], name="YS") for _ in range(2)]
        NCOL = 544 if need_ctx else 512
        exd_flat = EXd.t.ap().rearrange("e (t o) -> (e t) o", o=1)
        wcount = 0

        def load_w(src):
            nonlocal wcount
            part = wcount % 4
            wcount += 1
            wv = wparts[part]
            for hh in range(2):
                g.ld(wv[:, hh * 4:(hh + 1) * 4, :], src[hh * 512:(hh + 1) * 512, :].rearrange("(k p) n -> p k n", p=128),
                     [w_gate, w_up, w_down], [(WBIG, part)], q="pool")
            return wv, (WBIG, part)

        def gathers(e):
            b = e % 2
            for jt in range(4):
                idx = LISTI[:, e * 4 + jt:e * 4 + jt + 1]
                g.gather(XG[b][jt][:, :], XN2d[:, :], idx, [XN2d, (LISTI, e)], [XG[b][jt]])
                g.gather(GT[b][:, jt:jt + 1], exd_flat, idx, [EXd, (LISTI, e)], [GT[b]], elem_off=e * TT)
            if need_ctx:
                idc = LISTCI[:, e:e + 1]
                g.gather(XGc[b][:, :], XN2d[:, :], idc, [XN2d, (LISTCI, e)], [XGc[b]])
                g.gather(GTc[b][:, :], exd_flat, idc, [EXd, (LISTCI, e)], [GTc[b]], elem_off=e * TT)

        gathers(0)
        for e in range(NE):
            b = e % 2
            xsT = xsTs[b]; actT = actTs[b]
            wg, wgp = load_w(w_gate.t.ap()[l, e])
            wu, wup = load_w(w_up.t.ap()[l, e])
            wd, wdp = load_w(w_down.t.ap()[l, e])
            if e + 1 < NE:
                gathers(e + 1)
            for kc in range(8):
                tp = PT[kc % 2]
                for jt in range(4):
                    g.tr(tp[:, jt * 128:(jt + 1) * 128], XG[b][jt][:, kc * 128:(kc + 1) * 128], identb[:, :], [XG[b][jt], identb], [tp])
                g.act(xsT[:, kc, 0:512], tp[:, 0:512], AF.Identity, [tp, AB], [xsT], bias=AB[:, 3, kc, 0:1], scale=AB[:, 2, kc, 0:1])
                if need_ctx:
                    g.tr(tp[:, 512:544], XGc[b][:, kc * 128:(kc + 1) * 128], identb[0:32, 0:32], [XGc[b], identb], [tp])
                    g.act(xsT[:, kc, 512:544], tp[:, 512:544], AF.Identity, [tp, AB], [xsT], bias=AB[:, 3, kc, 1:2], scale=AB[:, 2, kc, 1:2])
            for fb in range(8):
                aps, ups = PB[(fb % 2) * 2], PB[(fb % 2) * 2 + 1]
                apc = PB[4]
                fs = slice(fb * 128, (fb + 1) * 128)
                for kc in range(8):
                    g.mm(aps[:, :], wg[:, kc, fs], xsT[:, kc, 0:512], kc == 0, kc == 7, [wgp, xsT], [aps])
                for kc in range(8):
                    g.mm(ups[:, :], wu[:, kc, fs], xsT[:, kc, 0:512], kc == 0, kc == 7, [wup, xsT], [ups])
                sgb = sg[fb % 2]
                g.act(sgb[:, 0:512], aps[:, :], AF.Silu, [aps], [sgb])
                g.tt("dve", actT[:, fb, 0:512], sgb[:, 0:512], ups[:, :], ALU.mult, [sgb, ups], [actT])
                if need_ctx:
                    for kc in range(8):
                        g.mm(apc[:, 0:32], wg[:, kc, fs], xsT[:, kc, 512:544], kc == 0, kc == 7, [wgp, xsT], [apc])
                    for kc in range(8):
                        g.mm(apc[:, 32:64], wu[:, kc, fs], xsT[:, kc, 512:544], kc == 0, kc == 7, [wup, xsT], [apc])
                    g.act(sgb[:, 512:544], apc[:, 0:32], AF.Silu, [apc], [sgb])
                    g.tt("dve", actT[:, fb, 512:544], sgb[:, 512:544], apc[:, 32:64], ALU.mult, [sgb, apc], [actT])
            for st in range(5 if need_ctx else 4):
                ys = YS[st % 2]
                m = 128 if st < 4 else 32
                for nh in range(2):
                    yp = PB[4 + nh]
                    ns = slice(nh * 512, (nh + 1) * 512)
                    cs = slice(st * 128, st * 128 + m)
                    for fb in range(8):
                        g.mm(yp[0:m, :], actT[:, fb, cs], wd[:, fb, ns], fb == 0, fb == 7, [actT, wdp], [yp])
                    if st < 4:
                        g.stt(ys[:, ns], yp[:, :], GT[b][:, st:st + 1], G2r[0][:, ns], ALU.mult, ALU.mult, [yp, GT[b], G2r[0]], [ys])
                    else:
                        g.stt(ys[0:32, ns], yp[0:32, :], GTc[b][:, 0:1], G2r[1][0:32, ns], ALU.mult, ALU.mult, [yp, GTc[b], G2r[1]], [ys])
                if st < 4:
                    g.scatter_add(xres[:, :], ys[:, :], LISTI[:, e * 4 + st:e * 4 + st + 1], [ys, (LISTI, e)], [xres])
                else:
                    g.scatter_add(xres[:, :], ys[0:32, :], LISTCI[:, e:e + 1], [ys, (LISTCI, e)], [xres])

    def phase_final():
        g.reset()
        fgr = g.carve([128, D], name="fgr")
        g.ld(fgr[:, :], fg_d[0:1, :].partition_broadcast(128), [fg_d], [fgr])
        xin = [g.carve([128, 4, D], name="xinf") for _ in range(2)]
        ss = g.carve([128, 4], name="ssf"); rstd = g.carve([128, 4], name="rstdf")
        junk = g.carve([128, D], name="junkf")
        for i, (s, t0, n) in enumerate(tiles(False)):
            xt = xin[i % 2]
            g.ld(xt[:, :, :], xres[t0:t0 + n, :].rearrange("(a p) d -> p a d", p=128), [xres], [xt])
            rms_stats(xt, 4, ss, rstd, junk)
            for a in range(4):
                g.stt(xt[:, a, :], xt[:, a, :], rstd[:, a:a + 1], fgr[:, :], ALU.mult, ALU.mult, [xt, rstd, fgr], [xt])
            g.ld(out_d[t0:t0 + n, :].rearrange("(a p) d -> p a d", p=128), xt[:, :, :], [xt], [out_d])

    setup()
    for l in range(n_layers):
        need_ctx = l < DEPTH - 1
        phase_mod(l)
        if stop_after == ("mod", l):
            break
        phase_proj(l)
        phase_bwd(l)
        if stop_after == ("proj", l):
            break
        phase_mix(l, need_ctx)
        if stop_after == ("mix", l):
            break
        phase_route(l, need_ctx)
        if stop_after == ("route", l):
            break
        phase_ffn(l, need_ctx)
    phase_final()
    if debug:
        g.ld(DBGd[:, 0:64], LISTF[:, :], [LISTF], [DBGd])
        g.ld(DBGd[0:32, 64:80], LISTCF[:, :], [LISTCF], [DBGd])
        g.ld(DBGd[:, 128:192], AB.t.ap().rearrange("p a b c -> p (a b c)") if False else AB[:, :, :, :].rearrange("p a b c -> p (a b c)"), [AB], [DBGd])
    kb.finish()
    kb.emit()
    es.close()
    return nc, kb


def _prep_inputs(x, c, ctx, c_ctx, w_mod, b_mod, norm1_g, norm2_g, w_in, conv_w, ret_decay_logit,
                 attn_sink, w_out, w_router, w_gate, w_up, w_down, final_g):
    f = lambda a: np.ascontiguousarray(np.asarray(a, dtype=np.float32))
    cos, sin = _rope_tables()
    perm = _win_perm()
    shared = {
        "w_mod": f(w_mod),
        "bm_t": f(np.asarray(b_mod).reshape(DEPTH, 48, 128).transpose(2, 0, 1)),
        "b_mod": f(b_mod),
        "g1t": f(np.asarray(norm1_g).reshape(DEPTH, 8, 128).transpose(2, 0, 1)),
        "g2t": f(np.asarray(norm2_g).reshape(DEPTH, 8, 128).transpose(2, 0, 1)),
        "w_in": f(np.asarray(w_in)[:, :, perm]),
        "cw_t": f(np.asarray(conv_w).reshape(DEPTH, 3, 2, 128).transpose(3, 0, 1, 2).reshape(128, DEPTH * 6)),
        "rdl": f(np.asarray(ret_decay_logit).reshape(1, 32)),
        "sink": f(np.asarray(attn_sink).reshape(1, 32)),
        "w_out": f(w_out), "w_router": f(w_router),
        "w_gate": f(w_gate), "w_up": f(w_up), "w_down": f(w_down),
        "final_g": f(np.asarray(final_g).reshape(1, D)),
        "rope_cos": cos, "rope_sin": sin, "rope_perm": _rope_perm(),
        "kvec4": np.where((np.arange(128) % 32) < 16, float(CAP), float(CAPC)).astype(np.float32).reshape(128, 1),
    }
    maps = []
    for core in range(8):
        b = core % 4
        m = dict(shared)
        m["x"] = f(np.asarray(x)[b])
        m["ctx"] = f(np.asarray(ctx)[b])
        m["cvec"] = f(np.stack([np.asarray(c)[b], np.asarray(c_ctx)], 0))
        maps.append(m)
    return maps


_CACHE = {}


def kernel(**inputs):
    maps = _prep_inputs(**inputs)
    if "nc" not in _CACHE:
        _CACHE["nc"] = build()[0]
    nc = _CACHE["nc"]
    res = run_bass_kernel_spmd(nc, maps, core_ids=list(range(8)))
    out = np.stack([np.asarray(res.results[b]["out"], dtype=np.float32) for b in range(4)], 0)
    return out
```
